# Optimizing a Trainium2 kernel written in Bass

```python
import math
import jax, jax.numpy as jnp
from jax import lax
import numpy as np

D_MODEL = 1024
BATCH = 4
SEQ = 8192
DEPTH = 1

CHUNK = 64
QBLK = 128
A_QBLK = CHUNK
HEAD_DIM = 64
A_HEADS = 8
A_DIM = A_HEADS * HEAD_DIM
IDX_HEADS = 8
IDX_DIM = 64
TOPK_MAX = 256
B_HEADS = 8
B_DIM = B_HEADS * HEAD_DIM
C_HEADS = 4
C_HEAD_DIM = 128
C_DIM = C_HEADS * C_HEAD_DIM
N_MEM = 256
N_BRANCH = 3
D_FF = ((8 * D_MODEL // 3 + 255) // 256) * 256
REL_BUCKETS = 32
REL_MAX_DIST = 128
EPS = 1e-6
SPLIT_SIZES = (A_DIM, A_DIM, A_DIM, IDX_HEADS * IDX_DIM, IDX_DIM, IDX_HEADS,
               B_DIM, B_DIM, B_DIM, C_DIM, N_BRANCH * D_MODEL)
N_IN = sum(SPLIT_SIZES)

kernel_name = 'hybrid_dsa_stickbreak_memory_block'


def rmsnorm(x, g):
    xf = x.astype(jnp.float32)
    y = xf * lax.rsqrt(jnp.mean(xf * xf, axis=-1, keepdims=True) + EPS)
    return (y * g.astype(jnp.float32)).astype(x.dtype)


def rel_bucket(rel):
    nb = REL_BUCKETS // 2
    max_exact = nb // 2
    n = jnp.abs(rel)
    large = max_exact + (jnp.log(jnp.maximum(n, 1).astype(jnp.float32) / max_exact)
                         / math.log(REL_MAX_DIST / max_exact) * (nb - max_exact)).astype(jnp.int32)
    large = jnp.minimum(large, nb - 1)
    return jnp.where(rel > 0, nb, 0) + jnp.where(n < max_exact, n, large)


def dsa_attention(aq, ak, av, iq, ik, iw, rel_bias):
    bsz, seq = aq.shape[0], aq.shape[1]
    k_sel = min(TOPK_MAX, seq // 4)
    key_chunk = jnp.arange(seq) // CHUNK
    gather = jax.vmap(lambda t, j: t[j])

    def block(i):
        q0 = i * A_QBLK
        qpos = q0 + jnp.arange(A_QBLK)
        qchunk = qpos // CHUNK
        iq_b = lax.dynamic_slice_in_dim(iq, q0, A_QBLK, axis=1)
        iw_b = lax.dynamic_slice_in_dim(iw, q0, A_QBLK, axis=1).astype(jnp.float32)
        aq_b = lax.dynamic_slice_in_dim(aq, q0, A_QBLK, axis=1)
        s = jnp.einsum('bqhd,bkd->bqhk', iq_b, ik).astype(jnp.float32) * IDX_DIM ** -0.5
        score = jnp.einsum('bqh,bqhk->bqk', iw_b, jax.nn.relu(s))
        admissible = key_chunk[None, :] <= qchunk[:, None]
        score = jnp.where(admissible[None], score, -jnp.inf)
        _, idx = lax.top_k(score, k_sel)
        valid = (idx // CHUNK) <= qchunk[None, :, None]
        kg = gather(ak, idx)
        vg = gather(av, idx)
        logits = jnp.einsum('bqhd,bqkhd->bhqk', aq_b, kg).astype(jnp.float32) * HEAD_DIM ** -0.5
        bias = rel_bias[rel_bucket(idx - qpos[None, :, None])]
        logits = logits + jnp.transpose(bias, (0, 3, 1, 2)).astype(jnp.float32)
        logits = jnp.where(valid[:, None], logits, -jnp.inf)
        p = jax.nn.softmax(logits, axis=-1).astype(vg.dtype)
        return jnp.einsum('bhqk,bqkhd->bqhd', p, vg)

    out = lax.map(block, jnp.arange(seq // A_QBLK))
    return jnp.transpose(out, (1, 0, 2, 3, 4)).reshape(bsz, seq, A_DIM)


def stick_breaking_attention(bq, bk, bv):
    bsz, seq = bq.shape[0], bq.shape[1]
    kpos = jnp.arange(seq)

    def block(i):
        q0 = i * QBLK
        qpos = q0 + jnp.arange(QBLK)
        q_b = lax.dynamic_slice_in_dim(bq, q0, QBLK, axis=1)
        z = jnp.einsum('bqhd,bkhd->bhqk', q_b, bk).astype(jnp.float32) * HEAD_DIM ** -0.5
        causal = kpos[None, :] < qpos[:, None]
        log_beta = jax.nn.log_sigmoid(z)
        log_one_minus = jnp.where(causal, jax.nn.log_sigmoid(-z), 0.0)
        tail = lax.cumsum(log_one_minus, axis=log_one_minus.ndim - 1, reverse=True) - log_one_minus
        a = jnp.where(causal, jnp.exp(log_beta + tail), 0.0).astype(bv.dtype)
        return jnp.einsum('bhqk,bkhd->bqhd', a, bv)

    out = lax.map(block, jnp.arange(seq // QBLK))
    return jnp.transpose(out, (1, 0, 2, 3, 4)).reshape(bsz, seq, B_DIM)


def memory_attention(cq, mk, mv):
    bsz, seq = cq.shape[0], cq.shape[1]
    logits = jnp.einsum('bshd,bmhd->bhsm', cq, mk).astype(jnp.float32) * C_HEAD_DIM ** -0.5
    p = jax.nn.softmax(logits, axis=-1).astype(mv.dtype)
    return jnp.einsum('bhsm,bmhd->bshd', p, mv).reshape(bsz, seq, C_DIM)


def setup_inputs(seed: int = 0) -> dict:
    key = jax.random.key(seed)
    ks = jax.random.split(key, 20)
    f32 = jnp.float32

    def w(k, shape, fan_in):
        return jax.random.normal(k, shape, f32) * fan_in ** -0.5

    def gain(k, shape):
        return 1.0 + 0.02 * jax.random.normal(k, shape, f32)

    return {
        'x': jax.random.normal(ks[0], (BATCH, SEQ, D_MODEL), f32),
        'mem': jax.random.normal(ks[1], (BATCH, N_MEM, D_MODEL), f32),
        'rel_bias': 0.5 * jax.random.normal(ks[2], (REL_BUCKETS, A_HEADS), f32),
        'g_mix_pre': gain(ks[3], (DEPTH, D_MODEL)),
        'w_in': w(ks[4], (DEPTH, D_MODEL, N_IN), D_MODEL),
        'b_gate': 0.02 * jax.random.normal(ks[5], (DEPTH, N_BRANCH * D_MODEL), f32),
        'g_mem': gain(ks[6], (DEPTH, D_MODEL)),
        'w_mem_kv': w(ks[7], (DEPTH, D_MODEL, 2 * C_DIM), D_MODEL),
        'w_up_a': w(ks[8], (DEPTH, A_DIM, D_MODEL), A_DIM),
        'w_up_b': w(ks[9], (DEPTH, B_DIM, D_MODEL), B_DIM),
        'w_up_c': w(ks[10], (DEPTH, C_DIM, D_MODEL), C_DIM),
        'w_out': w(ks[11], (DEPTH, D_MODEL, D_MODEL), D_MODEL),
        'g_mix_post': gain(ks[12], (DEPTH, D_MODEL)),
        'g_ffn_pre': gain(ks[13], (DEPTH, D_MODEL)),
        'w_ffn_in': w(ks[14], (DEPTH, D_MODEL, 2 * D_FF), D_MODEL),
        'w_ffn_out': w(ks[15], (DEPTH, D_FF, D_MODEL), D_FF),
        'g_ffn_post': gain(ks[16], (DEPTH, D_MODEL)),
    }


def reference(x, mem, rel_bias, g_mix_pre, w_in, b_gate, g_mem, w_mem_kv, w_up_a, w_up_b,
              w_up_c, w_out, g_mix_post, g_ffn_pre, w_ffn_in, w_ffn_out, g_ffn_post):
    bsz, seq = x.shape[0], x.shape[1]
    split_points = [int(v) for v in np.cumsum(SPLIT_SIZES)[:-1]]
    for l in range(DEPTH):
        h = rmsnorm(x, g_mix_pre[l])
        proj = h @ w_in[l]
        aq, ak, av, iq, ik, iw, bq, bk, bv, cq, gate_logits = jnp.split(proj, split_points, axis=-1)
        aq = aq.reshape(bsz, seq, A_HEADS, HEAD_DIM)
        ak = ak.reshape(bsz, seq, A_HEADS, HEAD_DIM)
        av = av.reshape(bsz, seq, A_HEADS, HEAD_DIM)
        iq = iq.reshape(bsz, seq, IDX_HEADS, IDX_DIM)
        iw = iw * IDX_HEADS ** -0.5
        bq = bq.reshape(bsz, seq, B_HEADS, HEAD_DIM)
        bk = bk.reshape(bsz, seq, B_HEADS, HEAD_DIM)
        bv = bv.reshape(bsz, seq, B_HEADS, HEAD_DIM)
        cq = cq.reshape(bsz, seq, C_HEADS, C_HEAD_DIM)
        mkv = rmsnorm(mem, g_mem[l]) @ w_mem_kv[l]
        mk = mkv[..., :C_DIM].reshape(bsz, N_MEM, C_HEADS, C_HEAD_DIM)
        mv = mkv[..., C_DIM:].reshape(bsz, N_MEM, C_HEADS, C_HEAD_DIM)

        ya = dsa_attention(aq, ak, av, iq, ik, iw, rel_bias) @ w_up_a[l]
        yb = stick_breaking_attention(bq, bk, bv) @ w_up_b[l]
        yc = memory_attention(cq, mk, mv) @ w_up_c[l]
        gates = jax.nn.sigmoid((gate_logits + b_gate[l]).astype(jnp.float32)).astype(x.dtype)
        gates = gates.reshape(bsz, seq, N_BRANCH, D_MODEL)
        merged = gates[:, :, 0] * ya + gates[:, :, 1] * yb + gates[:, :, 2] * yc
        x = x + rmsnorm(merged @ w_out[l], g_mix_post[l])

        h = rmsnorm(x, g_ffn_pre[l])
        gu = h @ w_ffn_in[l]
        f = (jax.nn.silu(gu[..., :D_FF]) * gu[..., D_FF:]) @ w_ffn_out[l]
        x = x + rmsnorm(f, g_ffn_post[l])
    return x
```

```python
import numpy as np
import ml_dtypes
from contextlib import ExitStack
import concourse.bass as bass
import concourse.mybir as mybir
from concourse.bass_utils import run_bass_kernel_spmd

F32 = mybir.dt.float32
BF16 = mybir.dt.bfloat16
AF = mybir.ActivationFunctionType
ALU = mybir.AluOpType
AX = mybir.AxisListType

D = 1024
KC = 8
NMEM = 256
DFF = 2816
FC = DFF // 128
EPS = 1e-6
KSEL = 256.0
NBIS = 18
O_AQ, O_AK, O_AV, O_IQ, O_IK, O_IW, O_BQ, O_BK, O_BV, O_CQ, O_G = (
    0, 512, 1024, 1536, 2048, 2112, 2120, 2632, 3144, 3656, 4168)


class Buf:
    __slots__ = ("name", "w", "r", "sem")

    def __init__(self, name):
        self.name = name
        self.w = {}
        self.r = {}
        self.sem = "d_" + name


class _Rec:
    def __getattr__(self, name):
        def mk(*a, **kw):
            return (name, a, kw)
        return mk


R = _Rec()


def _put(d, tok):
    if tok is not None and d.get(tok[0], 0) < tok[1]:
        d[tok[0]] = tok[1]


class Prog:
    ENG = ("pe", "act", "dve", "pool", "sp")

    def __init__(self):
        self.q = {e: [] for e in self.ENG}
        self.cnt = {}
        self.seen = {e: {} for e in self.ENG}
        self.dma_sems = []
        self.pending = {e: ([], [], []) for e in self.ENG}

    def _emit(self, eng, fn, deps, inc, dma_sem):
        waits = []
        for d in deps:
            if d is None:
                continue
            k, v = d
            if self.seen[eng].get(k, 0) >= v:
                continue
            self.seen[eng][k] = v
            waits.append((k, v))
        tok = None
        if dma_sem is not None:
            if dma_sem not in self.cnt:
                self.dma_sems.append(dma_sem)
            self.cnt[dma_sem] = self.cnt.get(dma_sem, 0) + 16
            tok = (dma_sem, self.cnt[dma_sem])
            self.q[eng].append((fn, waits, (dma_sem, 16)))
        elif inc:
            self.cnt[eng] = self.cnt.get(eng, 0) + 1
            tok = (eng, self.cnt[eng])
            self.q[eng].append((fn, waits, (eng, 1)))
        else:
            self.q[eng].append((fn, waits, None))
        return tok

    @staticmethod
    def _deps(reads, writes, acc, extra, eng=None):
        deps = list(extra)
        for b in reads:
            deps.extend(b.w.items())
        for b in writes:
            deps.extend(b.r.items())
            deps.extend(b.w.items())
        for b in acc:
            deps.extend(b.r.items())
            deps.extend((k, v) for k, v in b.w.items() if k != eng)
        return deps

    @staticmethod
    def _commit(tok, reads, writes, acc):
        for b in reads:
            _put(b.r, tok)
        for b in writes:
            b.w = {tok[0]: tok[1]}
            b.r = {}
        for b in acc:
            _put(b.w, tok)

    def op(self, eng, fn, reads=(), writes=(), inc=True, acc=(), extra=()):
        tok = self._emit(eng, fn, self._deps(reads, writes, acc, extra, eng), inc, None)
        pr, pw, pa = self.pending[eng]
        if tok is None:
            pr.extend(reads); pw.extend(writes); pa.extend(acc)
        else:
            self._commit(tok, list(reads) + pr, list(writes) + pw, list(acc) + pa)
            self.pending[eng] = ([], [], [])
        return tok

    def dma(self, out_ap, in_ap, reads=(), writes=(), acc=(), sem=None, eng="sp", extra=()):
        tok = self._emit(eng, ("dma_start", (), dict(out=out_ap, in_=in_ap)),
                         self._deps(reads, writes, acc, extra, eng), False, sem)
        self._commit(tok, reads, writes, acc)
        return tok

    def barrier(self):
        deps = list(self.cnt.items())
        for e in self.ENG:
            self._emit(e, None, deps, False, None)

    def run(self, nc, final_waits=()):
        with ExitStack() as es:
            sems = {}
            for k in list(self.ENG) + self.dma_sems:
                sems[k] = es.enter_context(nc.semaphore("s_" + k))
            block = es.enter_context(nc.Block())

            def replay(engname):
                def f(e):
                    for fn, waits, inc in self.q[engname]:
                        for k, v in waits:
                            e.wait_ge(sems[k], v)
                        if fn is None:
                            continue
                        ins = getattr(e, fn[0])(*fn[1], **fn[2])
                        if inc is not None:
                            ins.then_inc(sems[inc[0]], inc[1])
                    if engname == "sp":
                        for k, v in final_waits:
                            e.wait_ge(sems[k], v)
                return f

            block.sync(replay("sp"))
            block.tensor(replay("pe"))
            block.scalar(replay("act"))
            block.vector(replay("dve"))
            block.gpsimd(replay("pool"))


def build(S, stop_after=99, debug=False):
    SO = S // 2
    NSB = SO // 512
    NT = S // 128
    NGF = S // 512
    nc = bass.Bass("TRN2", target_bir_lowering=False)
    P = Prog()
    ES = [ExitStack()]

    def din(name, shape, dt=F32):
        return nc.dram_tensor(name, list(shape), dt, kind="ExternalInput").ap()

    dbg_kind = "ExternalOutput" if debug else "Internal"

    def dscr(name, shape, dt=BF16):
        return nc.dram_tensor(name, list(shape), dt, kind=dbg_kind).ap()

    xf = din("xf", [S, D])
    xq = din("xq", [SO, D])
    mem = din("mem", [NMEM, D])
    w_in = din("w_in", [D, 7240])
    b_gate = din("b_gate", [128, 24])
    gcols = din("gcols", [128, 4 * KC])
    grows = din("grows", [128, 2, D])
    w_mem_kv = din("w_mem_kv", [D, 1024])
    w_up = din("w_up", [3, 512, D])
    w_out = din("w_out", [D, D])
    w_ffn_in = din("w_ffn_in", [D, 2 * DFF])
    w_ffn_out = din("w_ffn_out", [DFF, D])
    biasT_in = din("biasT", [8, 128, 9, 512])
    biasc_in = din("biasc", [128, 8])
    maskc_in = din("maskc", [128, 8, 512], BF16)
    negadm_in = din("negadm", [128, 4, 1024], BF16)
    consts_in = din("consts", [128, 512], BF16)
    out = nc.dram_tensor("out", [SO, D], F32, kind="ExternalOutput").ap()

    akT = dscr("akT", [512, S]); bkT = dscr("bkT", [512, S]); ikT = dscr("ikT", [64, S])
    avh = dscr("avh", [8, 128, NT, 65]); bvh = dscr("bvh", [8, 128, NT, 64])
    aqT = dscr("aqT", [512, SO]); iqT = dscr("iqT", [512, SO]); bqT = dscr("bqT", [512, SO])
    gT = dscr("gT", [24, 128, SO])
    OaT = dscr("OaT", [512, SO]); ObT = dscr("ObT", [512, SO]); OcT = dscr("OcT", [512, SO])
    mskT = dscr("mskT", [NSB, 128, NT, 512])
    biasTb = dscr("biasTb", [8, 128, 9, 512])
    x1 = dscr("x1", [SO, D], F32)

    sbc = [0]

    def sb(name, shape, dt):
        sbc[0] += 1
        return ES[-1].enter_context(nc.sbuf_tensor("%s_%d" % (name, sbc[0]), list(shape), dt))

    def ps(name, shape, dt=F32):
        return ES[-1].enter_context(nc.psum_tensor(name, list(shape), dt))

    def store(dst_ap, src_ap, srcbuf):
        return P.dma(dst_ap, src_ap, reads=[srcbuf], sem=srcbuf.sem)

    cst = sb("cst", [128, 512], BF16)
    B_cst = Buf("cst")
    ident = cst[:, 0:128]
    negtri = cst[:, 128:256]
    ones_b = cst[:, 256:384]
    negones = cst[:, 384:512]
    P.dma(cst[:], consts_in[:, :], writes=[B_cst], sem=B_cst.sem)
    gcol = sb("gcol", [128, 4 * KC], F32); B_gcol = Buf("gcol")
    P.dma(gcol[:], gcols[:, :], writes=[B_gcol], sem=B_gcol.sem)
    bgate = sb("bgate", [128, 24], F32); B_bgate = Buf("bgate")
    P.dma(bgate[:], b_gate[:, :], writes=[B_bgate], sem=B_bgate.sem)
    iwabs = sb("iwabs", [128, SO // 128, 8], F32); B_iwabs = Buf("iwabs")
    iwsgn = sb("iwsgn", [128, SO // 128, 8], F32); B_iwsgn = Buf("iwsgn")
    mkT = sb("mkT", [128, 4, NMEM], BF16); B_mkT = Buf("mkT")
    mvS = sb("mvS", [128, 2, 512], BF16); B_mvS = Buf("mvS")
    EPS_T = sb("eps_t", [128, 1], F32); B_eps = Buf("eps")
    P.op("pool", R.memset(EPS_T[:], EPS), writes=[B_eps])

    psb = [ps("psb%d" % i, [128, 512], F32) for i in range(6)]
    B_ps = [Buf("ps%d" % i) for i in range(6)]
    psT = [ps("psT%d" % i, [128, 1024], BF16) for i in range(2)]
    B_psT = [Buf("psT%d" % i) for i in range(2)]
    NPS = 6
    ps_rr = [0]

    def next_ps():
        i = ps_rr[0] % NPS
        ps_rr[0] += 1
        return psb[i], B_ps[i]

    evac_rr = [0]

    def evac(out_ap, in_ap, B_in, B_out, eng=None):
        if eng is None:
            eng = "act" if evac_rr[0] % 2 == 0 else "dve"
            evac_rr[0] += 1
        if eng == "act":
            return P.op("act", R.copy(out=out_ap, in_=in_ap), reads=[B_in], acc=[B_out])
        return P.op("dve", R.tensor_copy(out=out_ap, in_=in_ap), reads=[B_in], acc=[B_out])

    def prep_weight(dst, B_dst, src, nk, segs, gidx, stg, B_stg):
        i = 0
        for kc in range(nk):
            for (s0, n, d0, scale) in segs:
                for c in range(0, n, 2048):
                    m = min(2048, n - c)
                    k = i % 2
                    i += 1
                    P.dma(stg[k][:, 0:m], src[kc * 128:(kc + 1) * 128, s0 + c:s0 + c + m],
                          writes=[B_stg[k]], sem=B_stg[k].sem)
                    eng = "pool" if (i % 2) else "dve"
                    if gidx is None:
                        P.op(eng, R.tensor_scalar(
                            out=dst[:, kc, d0 + c:d0 + c + m], in0=stg[k][:, 0:m], scalar1=float(scale),
                            scalar2=1.0, op0=ALU.mult, op1=ALU.mult),
                            reads=[B_stg[k]], acc=[B_dst])
                    else:
                        P.op(eng, R.tensor_scalar(
                            out=dst[:, kc, d0 + c:d0 + c + m], in0=stg[k][:, 0:m],
                            scalar1=gcol[:, gidx * KC + kc:gidx * KC + kc + 1],
                            scalar2=float(scale), op0=ALU.mult, op1=ALU.mult),
                            reads=[B_stg[k], B_gcol], acc=[B_dst])

    def make_front(sfx, nbm=4):
        fe = {}
        fe["xt"] = [sb("xt%s%d" % (sfx, i), [128, nbm, D], F32) for i in range(2)]
        fe["B_xt"] = [Buf("xt%s%d" % (sfx, i)) for i in range(2)]
        fe["xn"] = sb("xn" + sfx, [128, nbm, D], BF16); fe["B_xn"] = Buf("xn" + sfx)
        fe["hT"] = [sb("hT%s%d" % (sfx, i), [128, KC, nbm * 128], BF16) for i in range(2)]
        fe["B_hT"] = [Buf("hT%s%d" % (sfx, i)) for i in range(2)]
        fe["ss"] = sb("ss" + sfx, [128, 8], F32); fe["B_ss"] = [Buf("ss%s%d" % (sfx, i)) for i in range(2)]
        fe["rs"] = sb("rs" + sfx, [128, 8], F32); fe["B_rs"] = [Buf("rs%s%d" % (sfx, i)) for i in range(2)]
        fe["junk"] = sb("junk" + sfx, [128, D], BF16); fe["B_junk"] = Buf("junk" + sfx)
        return fe

    def front_load(fe, g, src_rows, nb=4):
        b = g % 2
        xt, B_xt = fe["xt"][b], fe["B_xt"][b]
        P.dma(xt[:, 0:nb, :], src_rows.rearrange("(n p) d -> p n d", p=128), writes=[B_xt], sem=B_xt.sem)

    def front(fe, g, src_rows, nb=4, loaded=False):
        b = g % 2
        xt, B_xt = fe["xt"][b], fe["B_xt"][b]
        if not loaded:
            front_load(fe, g, src_rows, nb)
        ss, rs = fe["ss"], fe["rs"]
        for j in range(nb):
            P.op("act", R.activation(out=fe["junk"][:], in_=xt[:, j, :], func=AF.Square,
                                                    accum_out=ss[:, b * 4 + j:b * 4 + j + 1]),
                 reads=[B_xt], writes=[fe["B_junk"]], acc=[fe["B_ss"][b]])
        P.op("act", R.activation(out=rs[:, b * 4:b * 4 + nb], in_=ss[:, b * 4:b * 4 + nb], func=AF.Sqrt,
                                           bias=EPS_T[:, 0:1], scale=1.0 / D),
             reads=[fe["B_ss"][b], B_eps], writes=[fe["B_rs"][b]])
        P.op("dve", R.reciprocal(out=rs[:, b * 4:b * 4 + nb], in_=rs[:, b * 4:b * 4 + nb]),
             reads=[fe["B_rs"][b]], writes=[fe["B_rs"][b]])
        xn = fe["xn"]
        for j in range(nb):
            if j % 2 == 0:
                P.op("act", R.activation(out=xn[:, j, :], in_=xt[:, j, :], func=AF.Copy,
                                                        scale=rs[:, b * 4 + j:b * 4 + j + 1]),
                     reads=[B_xt, fe["B_rs"][b]], acc=[fe["B_xn"]])
            else:
                P.op("dve", R.tensor_scalar(out=xn[:, j, :], in0=xt[:, j, :],
                                                           scalar1=rs[:, b * 4 + j:b * 4 + j + 1], scalar2=None,
                                                           op0=ALU.mult),
                     reads=[B_xt, fe["B_rs"][b]], acc=[fe["B_xn"]])
        hT, B_hT = fe["hT"][b], fe["B_hT"][b]
        for kc in range(0, KC, 2):
            pb = (kc // 2) % 2
            for k2 in range(2):
                for j in range(nb):
                    last = (k2 == 1 and j == nb - 1)
                    P.op("pe", R.transpose(
                        out=psT[pb][:, k2 * 512 + j * 128:k2 * 512 + (j + 1) * 128],
                        in_=xn[:, j, (kc + k2) * 128:(kc + k2 + 1) * 128], identity=ident),
                        reads=[fe["B_xn"], B_cst], acc=[B_psT[pb]], inc=last)
            src = psT[pb][:, :].rearrange("p (k t) -> p k t", k=2)[:, :, 0:nb * 128]
            if (kc // 2) % 2 == 0:
                P.op("dve", R.tensor_copy(out=hT[:, kc:kc + 2, 0:nb * 128], in_=src),
                     reads=[B_psT[pb]], acc=[B_hT])
            else:
                P.op("act", R.copy(out=hT[:, kc:kc + 2, 0:nb * 128], in_=src),
                     reads=[B_psT[pb]], acc=[B_hT])
        return hT, B_hT

    def proj_fm(W, B_W, hT, B_hT, c0, m, n=512):
        pt, B_pt = next_ps()
        for kc in range(KC):
            P.op("pe", R.matmul(pt[0:m, 0:n], lhsT=W[:, kc, c0:c0 + m], rhs=hT[:, kc, 0:n],
                                                 start=(kc == 0), stop=(kc == KC - 1)),
                 reads=[B_W, B_hT], acc=[B_pt], inc=(kc == KC - 1))
        return pt, B_pt

    def proj_tm(W, B_W, hT, B_hT, j, c0, n):
        pt, B_pt = next_ps()
        for kc in range(KC):
            P.op("pe", R.matmul(pt[:, 0:n], lhsT=hT[:, kc, j * 128:(j + 1) * 128],
                                                 rhs=W[:, kc, c0:c0 + n], start=(kc == 0), stop=(kc == KC - 1)),
                 reads=[B_W, B_hT], acc=[B_pt], inc=(kc == KC - 1))
        return pt, B_pt

    if stop_after >= 1:
        ES.append(ExitStack())
        Wk = sb("Wk", [128, KC, 2112], BF16); B_Wk = Buf("Wk")
        stg = [sb("wstg%d" % i, [128, 2048], F32) for i in range(2)]
        B_stg = [Buf("wstg%d" % i) for i in range(2)]
        prep_weight(Wk, B_Wk, w_in, KC,
                    [(O_AK, 1024, 0, 1.0), (O_IK, 64, 1024, 1.0), (O_BK, 1024, 1088, 1.0)], 0, stg, B_stg)
        fe = make_front("a")
        fst = [sb("fst%d" % i, [128, 9, 512], BF16) for i in range(2)]
        B_fst = [Buf("fst%d" % i) for i in range(2)]
        avst = [sb("avst%d" % i, [128, 8, 4, 65], BF16) for i in range(2)]
        B_avst = [Buf("avst%d" % i) for i in range(2)]
        bvst = [sb("bvst%d" % i, [128, 8, 4, 64], BF16) for i in range(2)]
        B_bvst = [Buf("bvst%d" % i) for i in range(2)]
        for i in range(2):
            P.op("pool", R.memset(avst[i][:], 1.0), writes=[B_avst[i]])
        front_load(fe, 0, xf[0:512, :])
        for g in range(NGF):
            b = g % 2
            if g + 1 < NGF:
                front_load(fe, g + 1, xf[(g + 1) * 512:(g + 2) * 512, :])
            hT, B_hT = front(fe, g, None, loaded=True)
            for oc in range(9):
                if oc < 4:
                    c0, m = oc * 128, 128
                elif oc < 8:
                    c0, m = 1088 + (oc - 4) * 128, 128
                else:
                    c0, m = 1024, 64
                pt, B_pt = proj_fm(Wk, B_Wk, hT, B_hT, c0, m)
                evac(fst[b][0:m, oc, :], pt[0:m, :], B_pt, B_fst[b])
            store(akT[:, g * 512:(g + 1) * 512].rearrange("(c p) t -> p c t", p=128), fst[b][:, 0:4, :], B_fst[b])
            store(bkT[:, g * 512:(g + 1) * 512].rearrange("(c p) t -> p c t", p=128), fst[b][:, 4:8, :], B_fst[b])
            store(ikT[:, g * 512:(g + 1) * 512], fst[b][0:64, 8, :], B_fst[b])
            for j in range(4):
                pt, B_pt = proj_tm(Wk, B_Wk, hT, B_hT, j, 512, 512)
                evac(avst[b][:, :, j, 0:64], pt[:, :].rearrange("p (h d) -> p h d", h=8), B_pt, B_avst[b])
                pt, B_pt = proj_tm(Wk, B_Wk, hT, B_hT, j, 1600, 512)
                evac(bvst[b][:, :, j, :], pt[:, :].rearrange("p (h d) -> p h d", h=8), B_pt, B_bvst[b])
            for h in range(8):
                store(avh[h, :, g * 4:(g + 1) * 4, :], avst[b][:, h, :, :], B_avst[b])
                store(bvh[h, :, g * 4:(g + 1) * 4, :], bvst[b][:, h, :, :], B_bvst[b])
        P.barrier()
        ES.pop().close()

    if stop_after >= 2:
        ES.append(ExitStack())
        Wq = sb("Wq", [128, KC, 5128], BF16); B_Wq = Buf("Wq")
        Wm = sb("Wm", [128, KC, 1024], BF16); B_Wm = Buf("Wm")
        ES.append(ExitStack())
        stg = [sb("wstg%d" % i, [128, 2048], F32) for i in range(2)]
        B_stg = [Buf("wstgq%d" % i) for i in range(2)]
        prep_weight(Wm, B_Wm, w_mem_kv, KC, [(0, 1024, 0, 1.0)], 1, stg, B_stg)
        prep_weight(Wq, B_Wq, w_in, KC,
                    [(O_AQ, 512, 0, 0.125), (O_IQ, 512, 512, 1.0), (O_BQ, 512, 1024, 0.125),
                     (O_CQ, 512, 1536, 128 ** -0.5), (O_IW, 8, 2048, 1.0), (O_G, 3072, 2056, 1.0)], 0, stg, B_stg)
        P.barrier()
        ES.pop().close()
        fe = make_front("q")
        hT, B_hT = front(fe, 1, mem[:, :], nb=2)
        for h in range(4):
            pt, B_pt = proj_fm(Wm, B_Wm, hT, B_hT, h * 128, 128, n=NMEM)
            evac(mkT[:, h, :], pt[:, 0:NMEM], B_pt, B_mkT)
        for j in range(2):
            pt, B_pt = proj_tm(Wm, B_Wm, hT, B_hT, j, 512, 512)
            evac(mvS[:, j, :], pt[:, :], B_pt, B_mvS)
        qst = [sb("qst%d" % i, [128, 12, 512], BF16) for i in range(1)] * 2
        B_qst = [Buf("qst%d" % i) for i in range(1)] * 2
        cqs = [sb("cqs%d" % i, [128, 4, 512], BF16) for i in range(1)] * 2
        B_cqs = [Buf("cqs%d" % i) for i in range(1)] * 2
        gst = [sb("gst%d" % i, [128, 512], BF16) for i in range(4)]
        B_gst = [Buf("gst%d" % i) for i in range(4)]
        pm = [sb("pm%d" % i, [128, 512], BF16) for i in range(4)]
        B_pm = [Buf("pm%d" % i) for i in range(4)]
        rD = [sb("rD%d" % i, [128, 512], F32) for i in range(2)]
        B_rD = [Buf("rD%d" % i) for i in range(2)]
        ocst = [sb("ocst%d" % i, [128, 4, 512], BF16) for i in range(2)]
        B_ocst = [Buf("ocst%d" % i) for i in range(2)]
        gi = 0
        pmi = 0
        front_load(fe, 0, xq[0:512, :])
        for g in range(NSB):
            b = g % 2
            tsl = slice(g * 512, (g + 1) * 512)
            if g + 1 < NSB:
                front_load(fe, g + 1, xq[(g + 1) * 512:(g + 2) * 512, :])
            hT, B_hT = front(fe, g, None, loaded=True)
            for oc in range(12):
                pt, B_pt = proj_fm(Wq, B_Wq, hT, B_hT, oc * 128, 128)
                evac(qst[b][:, oc, :], pt[:, :], B_pt, B_qst[b])
            store(aqT[:, tsl].rearrange("(c p) t -> p c t", p=128), qst[b][:, 0:4, :], B_qst[b])
            store(iqT[:, tsl].rearrange("(c p) t -> p c t", p=128), qst[b][:, 4:8, :], B_qst[b])
            store(bqT[:, tsl].rearrange("(c p) t -> p c t", p=128), qst[b][:, 8:12, :], B_qst[b])
            for h in range(4):
                pt, B_pt = proj_fm(Wq, B_Wq, hT, B_hT, 1536 + h * 128, 128)
                evac(cqs[b][:, h, :], pt[:, :], B_pt, B_cqs[b])
            for j in range(4):
                pt, B_pt = proj_tm(Wq, B_Wq, hT, B_hT, j, 2048, 8)
                P.op("dve", R.tensor_scalar(
                    out=iwsgn[:, g * 4 + j, :], in0=pt[:, 0:8], scalar1=0.0, scalar2=0.5,
                    op0=ALU.is_ge, op1=ALU.subtract),
                    reads=[B_pt], acc=[B_iwsgn])
                P.op("dve", R.scalar_tensor_tensor(
                    out=iwabs[:, g * 4 + j, :], in0=pt[:, 0:8], scalar=2.0, in1=iwsgn[:, g * 4 + j, :],
                    op0=ALU.mult, op1=ALU.mult),
                    reads=[B_pt, B_iwsgn], acc=[B_iwabs])
            for h in range(4):
                pms = []
                for mt in range(2):
                    pz, B_pz = next_ps()
                    P.op("pe", R.matmul(
                        pz[:, :], lhsT=mkT[:, h, mt * 128:(mt + 1) * 128], rhs=cqs[b][:, h, :],
                        start=True, stop=True), reads=[B_mkT, B_cqs[b]], writes=[B_pz])
                    k = pmi % 4
                    pmi += 1
                    P.op("act", R.activation(out=pm[k][:], in_=pz[:, :], func=AF.Exp),
                         reads=[B_pz], writes=[B_pm[k]])
                    pms.append(k)
                po, B_po = next_ps()
                pd, B_pd = next_ps()
                for mt in range(2):
                    k = pms[mt]
                    P.op("pe", R.matmul(
                        po[:, :], lhsT=mvS[:, mt, h * 128:(h + 1) * 128], rhs=pm[k][:],
                        start=(mt == 0), stop=(mt == 1)), reads=[B_mvS, B_pm[k]], acc=[B_po], inc=(mt == 1))
                for mt in range(2):
                    k = pms[mt]
                    P.op("pe", R.matmul(
                        pd[:, :], lhsT=ones_b, rhs=pm[k][:], start=(mt == 0), stop=(mt == 1)),
                        reads=[B_cst, B_pm[k]], acc=[B_pd], inc=(mt == 1))
                r = h % 2
                P.op("dve", R.reciprocal(out=rD[r][:], in_=pd[:, :]),
                     reads=[B_pd], writes=[B_rD[r]])
                P.op("dve", R.tensor_tensor(
                    out=ocst[b][:, h, :], in0=po[:, :], in1=rD[r][:], op=ALU.mult),
                    reads=[B_po, B_rD[r]], acc=[B_ocst[b]])
            store(OcT[:, tsl].rearrange("(c p) t -> p c t", p=128), ocst[b][:, :, :], B_ocst[b])
            for oc in range(24):
                pt, B_pt = proj_fm(Wq, B_Wq, hT, B_hT, 2056 + oc * 128, 128)
                k = gi % 4
                gi += 1
                P.op("act", R.activation(
                    out=gst[k][:], in_=pt[:, :], func=AF.Sigmoid, bias=bgate[:, oc:oc + 1]),
                    reads=[B_pt, B_bgate], writes=[B_gst[k]])
                store(gT[oc, :, tsl], gst[k][:], B_gst[k])
        P.barrier()
        ES.pop().close()

    ONE_T = sb("one_t", [128, 1], F32); B_one = Buf("one")
    P.op("pool", R.memset(ONE_T[:], 1.0), writes=[B_one])
    if stop_after >= 3:
        ES.append(ExitStack())
        kTs = [sb("kTs%d" % i, [64, S], BF16) for i in range(2)]; B_kTs = [Buf("kTs%d" % i) for i in range(2)]
        Vs = [sb("Vs%d" % i, [128, NT, 64], BF16) for i in range(2)]; B_Vs = [Buf("Vs%d" % i) for i in range(2)]
        qs = [sb("qs%d" % i, [64, 512], BF16) for i in range(2)]; B_qs = [Buf("qs%d" % i) for i in range(2)]
        maskc = sb("maskc", [128, 8, 512], BF16); B_maskc = Buf("maskc")
        P.dma(maskc[:], maskc_in[:, :, :], writes=[B_maskc], sem=B_maskc.sem)
        e_sb = [sb("e_sb%d" % i, [128, 512], F32) for i in range(2)]; B_e = [Buf("e%d" % i) for i in range(2)]
        sp_sb = [sb("sp_sb%d" % i, [128, 512], BF16) for i in range(4)]; B_sp = [Buf("sp%d" % i) for i in range(4)]
        spacc = [sb("spacc%d" % i, [128, 512], BF16) for i in range(2)]; B_spacc = [Buf("spacc%d" % i) for i in range(2)]
        a_sb = [sb("a_sb%d" % i, [128, 512], BF16) for i in range(3)]; B_a = [Buf("a%d" % i) for i in range(3)]
        obst = [sb("obst%d" % i, [64, 512], BF16) for i in range(2)]; B_obst = [Buf("obst%d" % i) for i in range(2)]
        items = [(G, h) for G in range(NSB) for h in range(8)]

        def sb_load(idx):
            G, h = items[idx]
            kb = idx % 2
            KL = (G + 1) * 1024
            P.dma(kTs[kb][:, 0:KL], bkT[h * 64:(h + 1) * 64, 0:KL], writes=[B_kTs[kb]], sem=B_kTs[kb].sem)
            P.dma(Vs[kb][:, 0:KL // 128, :], bvh[h, :, 0:KL // 128, :], writes=[B_Vs[kb]], sem=B_Vs[kb].sem)
            P.dma(qs[kb][:], bqT[h * 64:(h + 1) * 64, G * 512:(G + 1) * 512], writes=[B_qs[kb]], sem=B_qs[kb].sem)

        cz = [0]; csp = [0]; cc = [0]; ca = [0]
        sb_load(0)
        for idx, (G, h) in enumerate(items):
            kb = idx % 2
            if idx + 1 < len(items):
                sb_load(idx + 1)
            n = (G + 1) * 8
            psO, B_psO = psb[4 + kb], B_ps[4 + kb]
            st = {}
            st2 = {}

            def stage1(i):
                kt = n - 1 - i
                j = kt - 8 * G
                zi = cz[0] % 2; cz[0] += 1
                si = csp[0] % 4; csp[0] += 1
                st[i] = (zi, si)
                P.op("pe", R.matmul(psb[zi][:, :], lhsT=kTs[kb][:, kt * 128:(kt + 1) * 128], rhs=qs[kb][:],
                                    start=True, stop=True),
                     reads=[B_kTs[kb], B_qs[kb]], writes=[B_ps[zi]])
                P.op("act", R.activation(out=e_sb[zi][:], in_=psb[zi][:, :], func=AF.Exp),
                     reads=[B_ps[zi]], writes=[B_e[zi]])
                P.op("act", R.activation(out=sp_sb[si][:], in_=e_sb[zi][:], func=AF.Ln, bias=ONE_T[:, 0:1]),
                     reads=[B_e[zi], B_one], writes=[B_sp[si]])
                if j >= 0:
                    P.op("dve", R.tensor_tensor(out=sp_sb[si][:], in0=sp_sb[si][:], in1=maskc[:, j, :], op=ALU.mult),
                         reads=[B_sp[si], B_maskc], writes=[B_sp[si]])

            accprev = [None, None]

            def stage2(i):
                kt = n - 1 - i
                j = kt - 8 * G
                zi, si = st.pop(i)
                ci = 2 + (cc[0] % 2); cc[0] += 1
                ai = ca[0] % 3; ca[0] += 1
                st2[i] = ai
                P.op("pe", R.matmul(psb[ci][:, :], lhsT=kTs[kb][:, kt * 128:(kt + 1) * 128], rhs=qs[kb][:],
                                    start=True, stop=False),
                     reads=[B_kTs[kb], B_qs[kb]], writes=[B_ps[ci]], inc=False)
                P.op("pe", R.matmul(psb[ci][:, :], lhsT=negtri, rhs=sp_sb[si][:], start=False, stop=(i == 0)),
                     reads=[B_cst, B_sp[si]], acc=[B_ps[ci]], inc=(i == 0))
                if i > 0:
                    ap_prev, B_prev = accprev
                    P.op("pe", R.matmul(psb[ci][:, :], lhsT=negones, rhs=ap_prev, start=False, stop=True),
                         reads=[B_cst, B_prev], acc=[B_ps[ci]])
                if i < n - 1:
                    if i == 0:
                        accprev[0], accprev[1] = sp_sb[si][:], B_sp[si]
                    else:
                        ap_prev, B_prev = accprev
                        k = i % 2
                        P.op("pool", R.tensor_tensor(out=spacc[k][:], in0=ap_prev, in1=sp_sb[si][:], op=ALU.add),
                             reads=[B_prev, B_sp[si]], writes=[B_spacc[k]])
                        accprev[0], accprev[1] = spacc[k][:], B_spacc[k]
                P.op("act", R.activation(out=a_sb[ai][:], in_=psb[ci][:, :], func=AF.Exp),
                     reads=[B_ps[ci]], writes=[B_a[ai]])
                if j >= 0:
                    P.op("dve", R.tensor_tensor(out=a_sb[ai][:], in0=a_sb[ai][:], in1=maskc[:, j, :], op=ALU.mult),
                         reads=[B_a[ai], B_maskc], writes=[B_a[ai]])

            def stage3(i):
                kt = n - 1 - i
                ai = st2.pop(i)
                if i == 0:
                    P.op("pe", R.matmul(psO[0:64, :], lhsT=Vs[kb][:, kt, :], rhs=a_sb[ai][:],
                                        start=True, stop=(n == 1)),
                         reads=[B_Vs[kb], B_a[ai]], writes=[B_psO])
                else:
                    P.op("pe", R.matmul(psO[0:64, :], lhsT=Vs[kb][:, kt, :], rhs=a_sb[ai][:],
                                        start=False, stop=(i == n - 1)),
                         reads=[B_Vs[kb], B_a[ai]], acc=[B_psO])

            for step in range(n + 2):
                if step < n:
                    stage1(step)
                if 0 <= step - 1 < n:
                    stage2(step - 1)
                if 0 <= step - 2 < n:
                    stage3(step - 2)
            evac(obst[kb][:], psO[0:64, :], B_psO, B_obst[kb])
            store(ObT[h * 64:(h + 1) * 64, G * 512:(G + 1) * 512], obst[kb][:], B_obst[kb])
        P.barrier()
        ES.pop().close()

    if stop_after >= 4:
        ES.append(ExitStack())
        ikTs = sb("ikTs", [64, S], BF16); B_ikTs = Buf("ikTs")
        P.dma(ikTs[:], ikT[:, :], writes=[B_ikTs], sem=B_ikTs.sem)
        negadm = sb("negadm", [128, 4, 1024], BF16); B_negadm = Buf("negadm")
        P.dma(negadm[:], negadm_in[:, :, :], writes=[B_negadm], sem=B_negadm.sem)
        iqs = [sb("iqs%d" % i, [64, 8, 512], BF16) for i in range(2)]; B_iqs = [Buf("iqs%d" % i) for i in range(2)]
        sc = [sb("sc%d" % i, [128, S], F32) for i in range(2)]
        B_scc = [[Buf("sc%d_%d" % (i, c)) for c in range(S // 512)] for i in range(2)]
        B_sc = [Buf("scw%d" % i) for i in range(2)]
        r_sb = [sb("r_sb%d" % i, [128, 512], BF16) for i in range(4)]; B_r = [Buf("r%d" % i) for i in range(4)]
        dgs = [sb("dgs%d" % i, [128, 8, 128], BF16) for i in range(2)]; B_dgs = [Buf("dgs%d" % i) for i in range(2)]
        szi = [0]
        selt = [sb("selt%d" % i, [128, S], BF16) for i in range(2)]; B_sel = [Buf("sel%d" % i) for i in range(2)]
        junk4 = sb("junk4", [128, S], BF16); B_junk4 = Buf("junk4")
        junk5 = sb("junk5", [128, S], BF16); B_junk5 = Buf("junk5")
        sm = [sb("sm%d" % i, [128, 8], F32) for i in range(2)]
        B_sm = [[Buf("sm%d_%d" % (i, k)) for k in range(8)] for i in range(2)]
        mst = [sb("mst%d" % i, [128, 8, 128], BF16) for i in range(3)]; B_mst = [Buf("mst%d" % i) for i in range(3)]
        ri = 0
        msi = 0
        pti = 0
        P.dma(iqs[0][:], iqT.rearrange("(h d) t -> d h t", d=64)[:, :, 0:512], writes=[B_iqs[0]], sem=B_iqs[0].sem)
        for G in range(NSB):
            gb = G % 2
            if G + 1 < NSB:
                P.dma(iqs[1 - gb][:], iqT.rearrange("(h d) t -> d h t", d=64)[:, :, (G + 1) * 512:(G + 2) * 512],
                      writes=[B_iqs[1 - gb]], sem=B_iqs[1 - gb].sem)
            KL = (G + 1) * 1024
            nch = KL // 512
            for qp in range(2):
                chains = []
                for qi in range(2):
                    qb = 2 * qp + qi
                    blk = G * 4 + qb
                    cb = qi
                    scb = sc[cb]
                    smb, B_smb = sm[cb], B_sm[cb]
                    for ih in range(8):
                        P.op("pool", R.tensor_scalar(out=dgs[cb][:, ih, :], in0=ident, scalar1=iwabs[:, blk, ih:ih + 1],
                                                     scalar2=iwsgn[:, blk, ih:ih + 1], op0=ALU.mult, op1=ALU.mult),
                             reads=[B_cst, B_iwabs, B_iwsgn], acc=[B_dgs[cb]],
                             extra=(list(B_dgs[cb].r.items()) if ih == 0 else []))
                    its = [(c, ih) for c in range(nch) for ih in range(8)]
                    pend = []
                    for n_it in range(len(its) + 2):
                        if n_it < len(its):
                            c, ih = its[n_it]
                            z = szi[0] % 4; szi[0] += 1
                            P.op("pe", R.matmul(psb[z][:, :], lhsT=iqs[gb][:, ih, qb * 128:(qb + 1) * 128],
                                                rhs=ikTs[:, c * 512:(c + 1) * 512], start=True, stop=True),
                                 reads=[B_iqs[gb], B_ikTs], writes=[B_ps[z]])
                            k = ri % 4
                            ri += 1
                            P.op("act", R.activation(out=r_sb[k][:], in_=psb[z][:, :], func=AF.Relu),
                                 reads=[B_ps[z]], writes=[B_r[k]])
                            pend.append((c, ih, k))
                        if n_it >= 2:
                            c, ih, k = pend.pop(0)
                            ab = 4 + (c % 2)
                            if ih == 0:
                                P.op("pe", R.matmul(psb[ab][:, :], lhsT=dgs[cb][:, ih, :], rhs=r_sb[k][:],
                                                    start=True, stop=False),
                                     reads=[B_dgs[cb], B_r[k]], writes=[B_ps[ab]], inc=True)
                            else:
                                P.op("pe", R.matmul(psb[ab][:, :], lhsT=dgs[cb][:, ih, :], rhs=r_sb[k][:],
                                                    start=False, stop=(ih == 7)),
                                     reads=[B_dgs[cb], B_r[k]], acc=[B_ps[ab]], inc=True)
                            if ih == 7:
                                dst = scb[:, c * 512:(c + 1) * 512]
                                if c % 2 == 0:
                                    P.op("dve", R.tensor_copy(out=dst, in_=psb[ab][:, :]),
                                         reads=[B_ps[ab]], writes=[B_scc[cb][c]], extra=list(B_sc[cb].r.items()))
                                else:
                                    P.op("act", R.copy(out=dst, in_=psb[ab][:, :]),
                                         reads=[B_ps[ab]], writes=[B_scc[cb][c]], extra=list(B_sc[cb].r.items()))
                    chunks = [B_scc[cb][c] for c in range(nch)]
                    P.op("dve", R.tensor_reduce(out=smb[:, 0:1], in_=scb[:, 0:KL], axis=AX.X, op=ALU.max),
                         reads=chunks, writes=[B_smb[0]])
                    P.op("dve", R.tensor_reduce(out=smb[:, 1:2], in_=scb[:, 0:KL], axis=AX.X, op=ALU.min),
                         reads=chunks, writes=[B_smb[1]])
                    P.op("dve", R.tensor_tensor(out=scb[:, KL - 1024:KL], in0=scb[:, KL - 1024:KL],
                                                in1=negadm[:, qb, :], op=ALU.add),
                         reads=chunks + [B_negadm], writes=[B_sc[cb]] + chunks[-2:])
                    P.op("dve", R.tensor_tensor(out=smb[:, 2:3], in0=smb[:, 0:1], in1=smb[:, 1:2], op=ALU.subtract),
                         reads=[B_smb[0], B_smb[1]], writes=[B_smb[2]])
                    P.op("dve", R.tensor_scalar(out=smb[:, 3:4], in0=smb[:, 2:3], scalar1=1.0 + 2.0 ** -9,
                                                scalar2=2e-20, op0=ALU.mult, op1=ALU.add),
                         reads=[B_smb[2]], writes=[B_smb[3]])
                    P.op("dve", R.scalar_tensor_tensor(out=smb[:, 4:5], in0=smb[:, 2:3], scalar=-(2.0 ** -10),
                                                       in1=smb[:, 1:2], op0=ALU.mult, op1=ALU.add),
                         reads=[B_smb[2], B_smb[1]], writes=[B_smb[4]])
                    chains.append((qb, cb, scb, smb, B_smb))
                for k in range(NBIS):
                    hk = 2.0 ** -(k + 1)
                    for (qb, cb, scb, smb, B_smb) in chains:
                        if cb == 0:
                            P.op("dve", R.scalar_tensor_tensor(out=smb[:, 5:6], in0=smb[:, 3:4], scalar=hk,
                                                               in1=smb[:, 4:5], op0=ALU.mult, op1=ALU.add),
                                 reads=[B_smb[3], B_smb[4]], writes=[B_smb[5]])
                            P.op("dve", R.tensor_scalar(out=junk4[:, 0:KL], in0=scb[:, 0:KL], scalar1=smb[:, 5:6],
                                                        scalar2=None, op0=ALU.is_ge, op1=ALU.add,
                                                        accum_out=smb[:, 6:7]),
                                 reads=[B_sc[cb], B_smb[5]], writes=[B_junk4, B_smb[6]])
                        else:
                            P.op("dve", R.scalar_tensor_tensor(out=smb[:, 5:6], in0=smb[:, 3:4], scalar=-hk,
                                                               in1=smb[:, 4:5], op0=ALU.mult, op1=ALU.subtract),
                                 reads=[B_smb[3], B_smb[4]], writes=[B_smb[5]])
                            P.op("act", R.activation(out=junk5[:, 0:KL], in_=scb[:, 0:KL], func=AF.Sign,
                                                     bias=smb[:, 5:6], accum_out=smb[:, 6:7]),
                                 reads=[B_sc[cb], B_smb[5]], writes=[B_junk5, B_smb[6]])
                    for (qb, cb, scb, smb, B_smb) in chains:
                        thr = (KSEL - 0.5) if cb == 0 else (2.0 * KSEL - 1.0 - KL)
                        P.op("dve", R.tensor_scalar(out=smb[:, 7:8], in0=smb[:, 6:7], scalar1=thr,
                                                    scalar2=hk, op0=ALU.is_ge, op1=ALU.mult),
                             reads=[B_smb[6]], writes=[B_smb[7]])
                        P.op("dve", R.scalar_tensor_tensor(out=smb[:, 4:5], in0=smb[:, 7:8], scalar=smb[:, 3:4],
                                                           in1=smb[:, 4:5], op0=ALU.mult, op1=ALU.add),
                             reads=[B_smb[7], B_smb[3], B_smb[4]], writes=[B_smb[4]])
                for (qb, cb, scb, smb, B_smb) in chains:
                    P.op("dve", R.tensor_scalar(out=selt[cb][:, 0:KL], in0=scb[:, 0:KL], scalar1=smb[:, 4:5],
                                                scalar2=None, op0=ALU.is_ge),
                         reads=[B_sc[cb], B_smb[4]], writes=[B_sel[cb]])
                    for k0 in range(0, KL // 128, 8):
                        pb = pti % 2
                        pti += 1
                        for kk in range(8):
                            kt = k0 + kk
                            P.op("pe", R.transpose(
                                out=psT[pb][:, kk * 128:(kk + 1) * 128], in_=selt[cb][:, kt * 128:(kt + 1) * 128],
                                identity=ident), reads=[B_sel[cb], B_cst], acc=[B_psT[pb]], inc=(kk == 7))
                        m = msi % 3
                        msi += 1
                        P.op("act", R.copy(out=mst[m][:], in_=psT[pb][:, :].rearrange(
                            "p (k t) -> p k t", k=8)), reads=[B_psT[pb]], writes=[B_mst[m]])
                        store(mskT[G, :, k0:k0 + 8, qb * 128:(qb + 1) * 128], mst[m][:], B_mst[m])
        P.barrier()
        ES.pop().close()

    if stop_after >= 5:
        ES.append(ExitStack())
        kTs = [sb("akTs%d" % i, [64, S], BF16) for i in range(2)]; B_kTs = [Buf("akTs%d" % i) for i in range(2)]
        Vs = [sb("aVs%d" % i, [128, NT, 65], BF16) for i in range(2)]; B_Vs = [Buf("aVs%d" % i) for i in range(2)]
        qs = [sb("aqs%d" % i, [64, 512], BF16) for i in range(2)]; B_qs = [Buf("aqs%d" % i) for i in range(2)]
        bsf = [sb("bsf%d" % i, [128, 9, 512], F32) for i in range(2)]; B_bsf = [Buf("bsf%d" % i) for i in range(2)]
        bsb = [sb("bsb%d" % i, [128, 9, 512], BF16) for i in range(2)]; B_bsb = [Buf("bsb%d" % i) for i in range(2)]
        biasc = sb("biasc", [128, 8], F32); B_biasc = Buf("biasc")
        P.dma(biasc[:], biasc_in[:, :], writes=[B_biasc], sem=B_biasc.sem)
        mk_sb = sb("mk_sb", [128, NT, 512], BF16); B_mk = [Buf("mk%d" % i) for i in range(NT // 8)]
        p_sb = [sb("p_sb%d" % i, [128, 512], BF16) for i in range(3)]; B_p = [Buf("p%d" % i) for i in range(3)]
        pm_sb = [sb("pm_sb%d" % i, [128, 512], BF16) for i in range(4)]; B_pmm = [Buf("pmm%d" % i) for i in range(4)]
        Osb = [sb("Osb%d" % i, [65, 512], F32) for i in range(2)]; B_Osb = [Buf("Osb%d" % i) for i in range(2)]
        oast = [sb("oast%d" % i, [64, 512], BF16) for i in range(2)]; B_oast = [Buf("oast%d" % i) for i in range(2)]
        sel65 = sb("sel65", [65, 64], F32); B_sel65 = Buf("sel65")
        P.op("pool", R.memset(sel65[:], 0.0), writes=[B_sel65])
        P.op("pool", R.memset(sel65[64:65, :], 1.0), reads=[B_sel65], writes=[B_sel65])
        items = [(G, h) for G in range(NSB) for h in range(8)]

        def dsa_load(idx):
            G, h = items[idx]
            kb = idx % 2
            KL = (G + 1) * 1024
            P.dma(kTs[kb][:, 0:KL], akT[h * 64:(h + 1) * 64, 0:KL], writes=[B_kTs[kb]], sem=B_kTs[kb].sem)
            P.dma(Vs[kb][:, 0:KL // 128, :], avh[h, :, 0:KL // 128, :], writes=[B_Vs[kb]], sem=B_Vs[kb].sem)
            P.dma(qs[kb][:], aqT[h * 64:(h + 1) * 64, G * 512:(G + 1) * 512], writes=[B_qs[kb]], sem=B_qs[kb].sem)
            P.dma(bsf[kb][:], biasT_in[h, :, :, :], writes=[B_bsf[kb]], sem=B_bsf[kb].sem)
            P.op("pool", R.tensor_copy(out=bsb[kb][:], in_=bsf[kb][:]), reads=[B_bsf[kb]], writes=[B_bsb[kb]])

        pi = 0
        zi = 0
        dsa_load(0)
        for idx, (G, h) in enumerate(items):
            kb = idx % 2
            if h == 0:
                for sg in range(G + 1):
                    P.dma(mk_sb[:, sg * 8:(sg + 1) * 8, :], mskT[G, :, sg * 8:(sg + 1) * 8, :],
                          writes=[B_mk[sg]], sem=B_mk[sg].sem)
            if idx + 1 < len(items):
                dsa_load(idx + 1)
            n = (G + 1) * 8
            psO, B_psO = psb[4 + kb], B_ps[4 + kb]
            LA = 2
            pmk = {}
            for step in range(n + LA):
                if step < n:
                    kt = step
                    j = kt - 8 * G
                    near = j >= -1
                    z = zi % 4
                    zi += 1
                    k = pi % 3
                    km = pi % 4
                    pi += 1
                    pmk[kt] = km
                    P.op("pe", R.matmul(
                        psb[z][:, :], lhsT=kTs[kb][:, kt * 128:(kt + 1) * 128], rhs=qs[kb][:], start=True, stop=not near),
                        reads=[B_kTs[kb], B_qs[kb]], writes=[B_ps[z]], inc=not near)
                    if near:
                        P.op("pe", R.matmul(psb[z][:, :], lhsT=ident, rhs=bsb[kb][:, j + 1, :], start=False, stop=True),
                             reads=[B_cst, B_bsb[kb]], acc=[B_ps[z]])
                        P.op("act", R.activation(out=p_sb[k][:], in_=psb[z][:, :], func=AF.Exp),
                             reads=[B_ps[z]], writes=[B_p[k]])
                    else:
                        P.op("act", R.activation(out=p_sb[k][:], in_=psb[z][:, :], func=AF.Exp, bias=biasc[:, h:h + 1]),
                             reads=[B_ps[z], B_biasc], writes=[B_p[k]])
                    P.op("dve", R.tensor_tensor(out=pm_sb[km][:], in0=p_sb[k][:], in1=mk_sb[:, kt, :], op=ALU.mult),
                         reads=[B_p[k], B_mk[kt // 8]], writes=[B_pmm[km]])
                kt = step - LA
                if kt >= 0:
                    km = pmk.pop(kt)
                    if kt == 0:
                        P.op("pe", R.matmul(psO[0:65, :], lhsT=Vs[kb][:, kt, :], rhs=pm_sb[km][:],
                                            start=True, stop=(n == 1)),
                             reads=[B_Vs[kb], B_pmm[km]], writes=[B_psO])
                    else:
                        P.op("pe", R.matmul(psO[0:65, :], lhsT=Vs[kb][:, kt, :], rhs=pm_sb[km][:],
                                            start=False, stop=(kt == n - 1)),
                             reads=[B_Vs[kb], B_pmm[km]], acc=[B_psO])
            P.op("act", R.copy(out=Osb[kb][:], in_=psO[0:65, :]), reads=[B_psO], writes=[B_Osb[kb]])
            P.op("dve", R.reciprocal(out=Osb[kb][64:65, :], in_=Osb[kb][64:65, :]),
                 reads=[B_Osb[kb]], writes=[B_Osb[kb]])
            pt, B_pt = psb[zi % 4], B_ps[zi % 4]
            zi += 1
            P.op("pe", R.matmul(pt[0:64, :], lhsT=sel65[:, :], rhs=Osb[kb][:, :], start=True, stop=True),
                 reads=[B_sel65, B_Osb[kb]], writes=[B_pt])
            P.op("dve", R.tensor_tensor(out=oast[kb][:], in0=Osb[kb][0:64, :], in1=pt[0:64, :],
                                                         op=ALU.mult),
                 reads=[B_Osb[kb], B_pt], writes=[B_oast[kb]])
            store(OaT[h * 64:(h + 1) * 64, G * 512:(G + 1) * 512], oast[kb][:], B_oast[kb])
        P.barrier()
        ES.pop().close()

    gpost = sb("gpost", [128, 2, D], F32); B_gpost = Buf("gpost")
    P.dma(gpost[:], grows[:, :, :], writes=[B_gpost], sem=B_gpost.sem)

    def post_norm(psA, B_psA, psB_, B_psB, gi, res_ap, B_res, out_ap, B_out, tmp, B_tmp, ss2, B_ss2, junk, B_junk):
        P.op("act", R.activation(out=junk[:, 0:512], in_=psA[:, :], func=AF.Square, accum_out=ss2[:, 0:1]),
             reads=[B_psA], writes=[B_junk], acc=[B_ss2])
        P.op("act", R.activation(out=junk[:, 0:512], in_=psB_[:, :], func=AF.Square, accum_out=ss2[:, 1:2]),
             reads=[B_psB], writes=[B_junk], acc=[B_ss2])
        P.op("dve", R.tensor_tensor(out=ss2[:, 2:3], in0=ss2[:, 0:1], in1=ss2[:, 1:2], op=ALU.add),
             reads=[B_ss2], writes=[B_ss2])
        P.op("act", R.activation(out=ss2[:, 3:4], in_=ss2[:, 2:3], func=AF.Sqrt, bias=EPS_T[:, 0:1],
                                           scale=1.0 / D), reads=[B_ss2, B_eps], writes=[B_ss2])
        P.op("dve", R.reciprocal(out=ss2[:, 3:4], in_=ss2[:, 3:4]), reads=[B_ss2], writes=[B_ss2])
        for half, (pp, B_pp) in enumerate(((psA, B_psA), (psB_, B_psB))):
            hs = slice(half * 512, (half + 1) * 512)
            P.op("dve", R.scalar_tensor_tensor(
                out=tmp[:, hs], in0=pp[:, :], scalar=ss2[:, 3:4], in1=gpost[:, gi, hs], op0=ALU.mult, op1=ALU.mult),
                reads=[B_pp, B_ss2, B_gpost], acc=[B_tmp])
        P.op("pool", R.tensor_tensor(out=out_ap, in0=tmp[:, :], in1=res_ap, op=ALU.add),
             reads=[B_tmp, B_res], acc=[B_out])

    if stop_after >= 6:
        ES.append(ExitStack())
        Wup = sb("Wup", [128, 12, D], BF16); B_Wup = Buf("Wup")
        Wo = sb("Wo", [128, KC, D], BF16); B_Wo = Buf("Wo")
        stg = [sb("wstg6%d" % i, [128, 2048], F32) for i in range(2)]
        B_stg = [Buf("wstg6%d" % i) for i in range(2)]
        prep_weight(Wup, B_Wup, w_up.rearrange("r k n -> (r k) n"), 12, [(0, 1024, 0, 1.0)], None, stg, B_stg)
        prep_weight(Wo, B_Wo, w_out, KC, [(0, 1024, 0, 1.0)], None, stg, B_stg)
        OT = [sb("OT%d" % r, [128, 4, 512], BF16) for r in range(3)]; B_OT = [Buf("OT%d" % r) for r in range(3)]
        gts = sb("gts", [128, 24, 512], BF16); B_gts = Buf("gts")
        xo = sb("xo", [128, 4, D], F32); B_xo = Buf("xo")
        mT = sb("mT", [128, KC, 512], BF16); B_mT = Buf("mT")
        mt_ = [sb("mtmp%d" % i, [128, 512], F32) for i in range(4)]; B_mt = [Buf("mtmp%d" % i) for i in range(4)]
        tmp6 = sb("tmp6", [128, D], F32); B_tmp6 = Buf("tmp6")
        ss6 = sb("ss6", [128, 4], F32); B_ss6 = Buf("ss6")
        junk6 = sb("junk6", [128, 512], BF16); B_junk6 = Buf("junk6")
        x1st = sb("x1st", [128, 4, D], F32); B_x1st = Buf("x1st")
        srcs = [OaT, ObT, OcT]
        for G in range(NSB):
            tsl = slice(G * 512, (G + 1) * 512)
            for r in range(3):
                P.dma(OT[r][:], srcs[r][:, tsl].rearrange("(c p) t -> p c t", p=128), writes=[B_OT[r]], sem=B_OT[r].sem)
            P.dma(gts[:], gT[:, :, tsl].rearrange("c p t -> p c t"), writes=[B_gts], sem=B_gts.sem)
            P.dma(xo[:], xq[tsl, :].rearrange("(n p) d -> p n d", p=128), writes=[B_xo], sem=B_xo.sem)
            for oc in range(KC):
                pys = []
                for r in range(3):
                    pt, B_pt = next_ps()
                    for kc in range(4):
                        P.op("pe", R.matmul(
                            pt[:, :], lhsT=Wup[:, r * 4 + kc, oc * 128:(oc + 1) * 128], rhs=OT[r][:, kc, :],
                            start=(kc == 0), stop=(kc == 3)), reads=[B_Wup, B_OT[r]], acc=[B_pt], inc=(kc == 3))
                    pys.append((pt, B_pt))
                for r in range(3):
                    pt, B_pt = pys[r]
                    P.op("dve", R.tensor_tensor(
                        out=mt_[r][:], in0=pt[:, :], in1=gts[:, r * 8 + oc, :], op=ALU.mult),
                        reads=[B_pt, B_gts], writes=[B_mt[r]])
                P.op("pool", R.tensor_tensor(out=mt_[3][:], in0=mt_[0][:], in1=mt_[1][:], op=ALU.add),
                     reads=[B_mt[0], B_mt[1]], writes=[B_mt[3]])
                P.op("pool", R.tensor_tensor(out=mT[:, oc, :], in0=mt_[3][:], in1=mt_[2][:], op=ALU.add),
                     reads=[B_mt[3], B_mt[2]], acc=[B_mT])
            for j in range(4):
                pp = []
                for half in range(2):
                    pt, B_pt = next_ps()
                    for kc in range(KC):
                        P.op("pe", R.matmul(
                            pt[:, :], lhsT=mT[:, kc, j * 128:(j + 1) * 128], rhs=Wo[:, kc, half * 512:(half + 1) * 512],
                            start=(kc == 0), stop=(kc == KC - 1)), reads=[B_mT, B_Wo], acc=[B_pt], inc=(kc == KC - 1))
                    pp.append((pt, B_pt))
                post_norm(pp[0][0], pp[0][1], pp[1][0], pp[1][1], 0, xo[:, j, :], B_xo, x1st[:, j, :], B_x1st,
                          tmp6, B_tmp6, ss6, B_ss6, junk6, B_junk6)
            store(x1[tsl, :].rearrange("(n p) d -> p n d", p=128), x1st[:], B_x1st)
        P.barrier()
        ES.pop().close()

    if stop_after >= 7:
        ES.append(ExitStack())
        Wfi = sb("Wfi", [128, KC, 2 * DFF], BF16); B_Wfi = Buf("Wfi")
        Wfo = sb("Wfo", [128, FC, D], BF16); B_Wfo = Buf("Wfo")
        ES.append(ExitStack())
        stg = [sb("wstg7%d" % i, [128, 2048], F32) for i in range(2)]
        B_stg = [Buf("wstg7%d" % i) for i in range(2)]
        prep_weight(Wfi, B_Wfi, w_ffn_in, KC, [(0, 2 * DFF, 0, 1.0)], 2, stg, B_stg)
        prep_weight(Wfo, B_Wfo, w_ffn_out, FC, [(0, D, 0, 1.0)], None, stg, B_stg)
        P.barrier()
        ES.pop().close()
        fe = make_front("f", nbm=2)
        aT = sb("aT", [128, FC, 256], BF16); B_aT = Buf("aT")
        sg_ = [sb("sg%d" % i, [128, 256], F32) for i in range(2)]; B_sg = [Buf("sg%d" % i) for i in range(2)]
        tmp7 = sb("tmp7", [128, D], F32); B_tmp7 = Buf("tmp7")
        ss7 = sb("ss7", [128, 4], F32); B_ss7 = Buf("ss7")
        junk7 = sb("junk7", [128, 512], BF16); B_junk7 = Buf("junk7")
        ost = [sb("ost%d" % i, [128, 2, D], F32) for i in range(1)] * 2; B_ost = [Buf("ost%d" % i) for i in range(1)] * 2
        NG7 = SO // 256
        front_load(fe, 0, x1[0:256, :], nb=2)
        for g in range(NG7):
            b = g % 2
            if g + 1 < NG7:
                front_load(fe, g + 1, x1[(g + 1) * 256:(g + 2) * 256, :], nb=2)
            hT, B_hT = front(fe, g, None, nb=2, loaded=True)
            for fc in range(FC):
                pg, B_pg = proj_fm(Wfi, B_Wfi, hT, B_hT, fc * 128, 128, n=256)
                pu, B_pu = proj_fm(Wfi, B_Wfi, hT, B_hT, DFF + fc * 128, 128, n=256)
                k = fc % 2
                P.op("act", R.activation(out=sg_[k][:], in_=pg[:, 0:256], func=AF.Silu),
                     reads=[B_pg], writes=[B_sg[k]])
                P.op("dve", R.tensor_tensor(out=aT[:, fc, :], in0=sg_[k][:], in1=pu[:, 0:256],
                                                                         op=ALU.mult),
                     reads=[B_sg[k], B_pu], acc=[B_aT])
            for j in range(2):
                pp = []
                for half in range(2):
                    pt, B_pt = next_ps()
                    for fc in range(FC):
                        P.op("pe", R.matmul(
                            pt[:, :], lhsT=aT[:, fc, j * 128:(j + 1) * 128], rhs=Wfo[:, fc, half * 512:(half + 1) * 512],
                            start=(fc == 0), stop=(fc == FC - 1)), reads=[B_aT, B_Wfo], acc=[B_pt], inc=(fc == FC - 1))
                    pp.append((pt, B_pt))
                post_norm(pp[0][0], pp[0][1], pp[1][0], pp[1][1], 1, fe["xt"][b][:, j, :], fe["B_xt"][b],
                          ost[b][:, j, :], B_ost[b], tmp7, B_tmp7, ss7, B_ss7, junk7, B_junk7)
            store(out[g * 256:(g + 1) * 256, :].rearrange("(n p) d -> p n d", p=128), ost[b][:], B_ost[b])
        P.barrier()
        ES.pop().close()


    final = [(k, v) for k, v in P.cnt.items() if k not in P.ENG]
    P.run(nc, final_waits=final)
    while ES:
        ES.pop().close()
    return nc


def _rel_bucket(rel):
    nb = 16
    max_exact = 8
    n = np.abs(rel)
    large = max_exact + (np.log(np.maximum(n, 1).astype(np.float32) / max_exact)
                         / np.float32(np.log(128 / max_exact)) * (nb - max_exact)).astype(np.int32)
    large = np.minimum(large, nb - 1)
    return np.where(rel > 0, nb, 0) + np.where(n < max_exact, n, large)


def make_consts():
    c = np.zeros((128, 512), np.float32)
    c[:, 0:128] = np.eye(128)
    j = np.arange(128)[:, None]
    s = np.arange(128)[None, :]
    c[:, 128:256] = -(j >= s).astype(np.float32)
    c[:, 256:384] = 1.0
    c[:, 384:512] = -1.0
    return c.astype(ml_dtypes.bfloat16)


def core_consts(hf, rel_bias):
    p = np.arange(128)[:, None, None]
    j = np.arange(8)[None, :, None]
    t = np.arange(512)[None, None, :]
    krel = j * 128 + p
    qrel = hf * 512 + t
    maskc = (krel < qrel).astype(np.float32).astype(ml_dtypes.bfloat16)
    qp = np.arange(128)[:, None, None]
    qb = np.arange(4)[None, :, None]
    kr = np.arange(1024)[None, None, :]
    qpos = hf * 512 + qb * 128 + qp
    adm = (kr // 64) <= (qpos // 64)
    negadm = np.where(adm, 0.0, -1e30).astype(np.float32).astype(ml_dtypes.bfloat16)
    jj = np.arange(9)[None, :, None]
    rel = ((jj - 1) * 128 + p) - qrel
    bidx = _rel_bucket(rel)
    biasT = np.ascontiguousarray(np.transpose(rel_bias[bidx], (3, 0, 1, 2))).astype(np.float32)
    biasc = np.ascontiguousarray(np.broadcast_to(rel_bias[15][None, :], (128, 8))).astype(np.float32)
    return maskc, negadm, biasT, biasc


def make_in_maps(S, x, mem, rel_bias, g_mix_pre, w_in, b_gate, g_mem, w_mem_kv, w_up_a, w_up_b,
                 w_up_c, w_out, g_mix_post, g_ffn_pre, w_ffn_in, w_ffn_out, g_ffn_post):
    f = lambda a: np.ascontiguousarray(np.asarray(a, dtype=np.float32))
    x = f(x); mem = f(mem); rel_bias = f(rel_bias)
    B = x.shape[0]
    SO = S // 2
    NSB = SO // 512
    col = lambda g: f(g)[0].reshape(KC, 128).T
    gcols = np.ascontiguousarray(np.concatenate([col(g_mix_pre), col(g_mem), col(g_ffn_pre), col(g_ffn_pre)], axis=1))
    grows = np.ascontiguousarray(np.broadcast_to(np.stack([f(g_mix_post)[0], f(g_ffn_post)[0]])[None], (128, 2, D)))
    bg = np.ascontiguousarray(f(b_gate)[0].reshape(24, 128).T)
    w_up = np.ascontiguousarray(np.stack([f(w_up_a)[0], f(w_up_b)[0], f(w_up_c)[0]]))
    consts = make_consts()
    cc = [core_consts(hf, rel_bias) for hf in range(2)]
    shared = dict(w_in=f(w_in)[0], b_gate=bg, gcols=gcols, grows=grows, w_mem_kv=f(w_mem_kv)[0], w_up=w_up,
                  w_out=f(w_out)[0], w_ffn_in=f(w_ffn_in)[0], w_ffn_out=f(w_ffn_out)[0], consts=consts)
    in_maps = []
    for c in range(2 * B):
        b, hf = c // 2, c % 2
        xb = x[b]
        xq = np.ascontiguousarray(xb.reshape(NSB, 2, 512, D)[:, hf].reshape(SO, D))
        maskc, negadm, biasT, biasc = cc[hf]
        m = dict(shared)
        m.update(xf=xb, xq=xq, mem=mem[b], maskc=maskc, negadm=negadm, biasT=biasT, biasc=biasc)
        in_maps.append(m)
    return in_maps


_NC_CACHE = {}


def kernel(**inputs):
    x = np.asarray(inputs["x"])
    B, S, _ = x.shape
    SO = S // 2
    NSB = SO // 512
    if S not in _NC_CACHE:
        _NC_CACHE[S] = build(S)
    nc = _NC_CACHE[S]
    in_maps = make_in_maps(S, **inputs)
    res = run_bass_kernel_spmd(nc, in_maps, core_ids=list(range(2 * B)))
    outp = np.empty((B, S, D), np.float32)
    for c in range(2 * B):
        b, hf = c // 2, c % 2
        outp[b].reshape(NSB, 2, 512, D)[:, hf] = np.asarray(res.results[c]["out"]).reshape(NSB, 512, D)
    return outp
```

```python
import numpy as np
import ml_dtypes
from contextlib import ExitStack
import concourse.bass as bass
import concourse.mybir as mybir
from concourse.bass_utils import run_bass_kernel_spmd

F32 = mybir.dt.float32
BF16 = mybir.dt.bfloat16
AF = mybir.ActivationFunctionType
ALU = mybir.AluOpType
AX = mybir.AxisListType

D = 1024
KC = 8
NMEM = 256
DFF = 2816
FC = DFF // 128
EPS = 1e-6
KSEL = 256.0
NBIS = 18
O_AQ, O_AK, O_AV, O_IQ, O_IK, O_IW, O_BQ, O_BK, O_BV, O_CQ, O_G = (
    0, 512, 1024, 1536, 2048, 2112, 2120, 2632, 3144, 3656, 4168)


class Buf:
    __slots__ = ("name", "w", "r", "sem")

    def __init__(self, name):
        self.name = name
        self.w = {}
        self.r = {}
        self.sem = "d_" + name


class _Rec:
    def __getattr__(self, name):
        def mk(*a, **kw):
            return (name, a, kw)
        return mk


R = _Rec()


def _put(d, tok):
    if tok is not None and d.get(tok[0], 0) < tok[1]:
        d[tok[0]] = tok[1]


class Prog:
    ENG = ("pe", "act", "dve", "pool", "sp")

    def __init__(self):
        self.q = {e: [] for e in self.ENG}
        self.cnt = {}
        self.seen = {e: {} for e in self.ENG}
        self.dma_sems = []
        self.pending = {e: ([], [], []) for e in self.ENG}

    def _emit(self, eng, fn, deps, inc, dma_sem):
        waits = []
        for d in deps:
            if d is None:
                continue
            k, v = d
            if self.seen[eng].get(k, 0) >= v:
                continue
            self.seen[eng][k] = v
            waits.append((k, v))
        tok = None
        if dma_sem is not None:
            if dma_sem not in self.cnt:
                self.dma_sems.append(dma_sem)
            self.cnt[dma_sem] = self.cnt.get(dma_sem, 0) + 16
            tok = (dma_sem, self.cnt[dma_sem])
            self.q[eng].append((fn, waits, (dma_sem, 16)))
        elif inc:
            self.cnt[eng] = self.cnt.get(eng, 0) + 1
            tok = (eng, self.cnt[eng])
            self.q[eng].append((fn, waits, (eng, 1)))
        else:
            self.q[eng].append((fn, waits, None))
        return tok

    @staticmethod
    def _deps(reads, writes, acc, extra, eng=None):
        deps = list(extra)
        for b in reads:
            deps.extend(b.w.items())
        for b in writes:
            deps.extend(b.r.items())
            deps.extend(b.w.items())
        for b in acc:
            deps.extend(b.r.items())
            deps.extend((k, v) for k, v in b.w.items() if k != eng)
        return deps

    @staticmethod
    def _commit(tok, reads, writes, acc):
        for b in reads:
            _put(b.r, tok)
        for b in writes:
            b.w = {tok[0]: tok[1]}
            b.r = {}
        for b in acc:
            _put(b.w, tok)

    def op(self, eng, fn, reads=(), writes=(), inc=True, acc=(), extra=()):
        tok = self._emit(eng, fn, self._deps(reads, writes, acc, extra, eng), inc, None)
        pr, pw, pa = self.pending[eng]
        if tok is None:
            pr.extend(reads); pw.extend(writes); pa.extend(acc)
        else:
            self._commit(tok, list(reads) + pr, list(writes) + pw, list(acc) + pa)
            self.pending[eng] = ([], [], [])
        return tok

    def dma(self, out_ap, in_ap, reads=(), writes=(), acc=(), sem=None, eng="sp", extra=()):
        tok = self._emit(eng, ("dma_start", (), dict(out=out_ap, in_=in_ap)),
                         self._deps(reads, writes, acc, extra, eng), False, sem)
        self._commit(tok, reads, writes, acc)
        return tok

    def barrier(self):
        deps = list(self.cnt.items())
        for e in self.ENG:
            self._emit(e, None, deps, False, None)

    def run(self, nc, final_waits=()):
        with ExitStack() as es:
            sems = {}
            for k in list(self.ENG) + self.dma_sems:
                sems[k] = es.enter_context(nc.semaphore("s_" + k))
            block = es.enter_context(nc.Block())

            def replay(engname):
                def f(e):
                    for fn, waits, inc in self.q[engname]:
                        for k, v in waits:
                            e.wait_ge(sems[k], v)
                        if fn is None:
                            continue
                        ins = getattr(e, fn[0])(*fn[1], **fn[2])
                        if inc is not None:
                            ins.then_inc(sems[inc[0]], inc[1])
                    if engname == "sp":
                        for k, v in final_waits:
                            e.wait_ge(sems[k], v)
                return f

            block.sync(replay("sp"))
            block.tensor(replay("pe"))
            block.scalar(replay("act"))
            block.vector(replay("dve"))
            block.gpsimd(replay("pool"))


def build(S, stop_after=99, debug=False):
    SO = S // 2
    NSB = SO // 512
    NT = S // 128
    NGF = S // 512
    nc = bass.Bass("TRN2", target_bir_lowering=False)
    P = Prog()
    ES = [ExitStack()]

    def din(name, shape, dt=F32):
        return nc.dram_tensor(name, list(shape), dt, kind="ExternalInput").ap()

    dbg_kind = "ExternalOutput" if debug else "Internal"

    def dscr(name, shape, dt=BF16):
        return nc.dram_tensor(name, list(shape), dt, kind=dbg_kind).ap()

    xf = din("xf", [S, D])
    xq = din("xq", [SO, D])
    mem = din("mem", [NMEM, D])
    w_in = din("w_in", [D, 7240])
    b_gate = din("b_gate", [128, 24])
    gcols = din("gcols", [128, 4 * KC])
    grows = din("grows", [128, 2, D])
    w_mem_kv = din("w_mem_kv", [D, 1024])
    w_up = din("w_up", [3, 512, D])
    w_out = din("w_out", [D, D])
    w_ffn_in = din("w_ffn_in", [D, 2 * DFF])
    w_ffn_out = din("w_ffn_out", [DFF, D])
    biasT_in = din("biasT", [8, 128, 9, 512])
    biasc_in = din("biasc", [128, 8])
    maskc_in = din("maskc", [128, 8, 512], BF16)
    negadm_in = din("negadm", [128, 4, 1024], BF16)
    consts_in = din("consts", [128, 512], BF16)
    out = nc.dram_tensor("out", [SO, D], F32, kind="ExternalOutput").ap()

    akT = dscr("akT", [512, S]); bkT = dscr("bkT", [512, S]); ikT = dscr("ikT", [64, S])
    avh = dscr("avh", [8, 128, NT, 65]); bvh = dscr("bvh", [8, 128, NT, 64])
    aqT = dscr("aqT", [512, SO]); iqT = dscr("iqT", [512, SO]); bqT = dscr("bqT", [512, SO])
    gT = dscr("gT", [24, 128, SO])
    OaT = dscr("OaT", [512, SO]); ObT = dscr("ObT", [512, SO]); OcT = dscr("OcT", [512, SO])
    mskT = dscr("mskT", [NSB, 128, NT, 512])
    biasTb = dscr("biasTb", [8, 128, 9, 512])
    x1 = dscr("x1", [SO, D], F32)

    sbc = [0]

    def sb(name, shape, dt):
        sbc[0] += 1
        return ES[-1].enter_context(nc.sbuf_tensor("%s_%d" % (name, sbc[0]), list(shape), dt))

    def ps(name, shape, dt=F32):
        return ES[-1].enter_context(nc.psum_tensor(name, list(shape), dt))

    def store(dst_ap, src_ap, srcbuf):
        return P.dma(dst_ap, src_ap, reads=[srcbuf], sem=srcbuf.sem)

    cst = sb("cst", [128, 512], BF16)
    B_cst = Buf("cst")
    ident = cst[:, 0:128]
    negtri = cst[:, 128:256]
    ones_b = cst[:, 256:384]
    negones = cst[:, 384:512]
    P.dma(cst[:], consts_in[:, :], writes=[B_cst], sem=B_cst.sem)
    gcol = sb("gcol", [128, 4 * KC], F32); B_gcol = Buf("gcol")
    P.dma(gcol[:], gcols[:, :], writes=[B_gcol], sem=B_gcol.sem)
    bgate = sb("bgate", [128, 24], F32); B_bgate = Buf("bgate")
    P.dma(bgate[:], b_gate[:, :], writes=[B_bgate], sem=B_bgate.sem)
    iwabs = sb("iwabs", [128, SO // 128, 8], F32); B_iwabs = Buf("iwabs")
    iwsgn = sb("iwsgn", [128, SO // 128, 8], F32); B_iwsgn = Buf("iwsgn")
    mkT = sb("mkT", [128, 4, NMEM], BF16); B_mkT = Buf("mkT")
    mvS = sb("mvS", [128, 2, 512], BF16); B_mvS = Buf("mvS")
    EPS_T = sb("eps_t", [128, 1], F32); B_eps = Buf("eps")
    P.op("pool", R.memset(EPS_T[:], EPS), writes=[B_eps])

    psb = [ps("psb%d" % i, [128, 512], F32) for i in range(6)]
    B_ps = [Buf("ps%d" % i) for i in range(6)]
    psT = [ps("psT%d" % i, [128, 1024], BF16) for i in range(2)]
    B_psT = [Buf("psT%d" % i) for i in range(2)]
    NPS = 6
    ps_rr = [0]

    def next_ps():
        i = ps_rr[0] % NPS
        ps_rr[0] += 1
        return psb[i], B_ps[i]

    evac_rr = [0]

    def evac(out_ap, in_ap, B_in, B_out, eng=None):
        if eng is None:
            eng = "act" if evac_rr[0] % 2 == 0 else "dve"
            evac_rr[0] += 1
        if eng == "act":
            return P.op("act", R.copy(out=out_ap, in_=in_ap), reads=[B_in], acc=[B_out])
        return P.op("dve", R.tensor_copy(out=out_ap, in_=in_ap), reads=[B_in], acc=[B_out])

    def prep_weight(dst, B_dst, src, nk, segs, gidx, stg, B_stg):
        i = 0
        for kc in range(nk):
            for (s0, n, d0, scale) in segs:
                for c in range(0, n, 2048):
                    m = min(2048, n - c)
                    k = i % 2
                    i += 1
                    P.dma(stg[k][:, 0:m], src[kc * 128:(kc + 1) * 128, s0 + c:s0 + c + m],
                          writes=[B_stg[k]], sem=B_stg[k].sem)
                    eng = "pool" if (i % 2) else "dve"
                    if gidx is None:
                        P.op(eng, R.tensor_scalar(
                            out=dst[:, kc, d0 + c:d0 + c + m], in0=stg[k][:, 0:m], scalar1=float(scale),
                            scalar2=1.0, op0=ALU.mult, op1=ALU.mult),
                            reads=[B_stg[k]], acc=[B_dst])
                    else:
                        P.op(eng, R.tensor_scalar(
                            out=dst[:, kc, d0 + c:d0 + c + m], in0=stg[k][:, 0:m],
                            scalar1=gcol[:, gidx * KC + kc:gidx * KC + kc + 1],
                            scalar2=float(scale), op0=ALU.mult, op1=ALU.mult),
                            reads=[B_stg[k], B_gcol], acc=[B_dst])

    def make_front(sfx, nbm=4):
        fe = {}
        fe["xt"] = [sb("xt%s%d" % (sfx, i), [128, nbm, D], F32) for i in range(2)]
        fe["B_xt"] = [Buf("xt%s%d" % (sfx, i)) for i in range(2)]
        fe["xn"] = sb("xn" + sfx, [128, nbm, D], BF16); fe["B_xn"] = Buf("xn" + sfx)
        fe["hT"] = [sb("hT%s%d" % (sfx, i), [128, KC, nbm * 128], BF16) for i in range(2)]
        fe["B_hT"] = [Buf("hT%s%d" % (sfx, i)) for i in range(2)]
        fe["ss"] = sb("ss" + sfx, [128, 8], F32); fe["B_ss"] = [Buf("ss%s%d" % (sfx, i)) for i in range(2)]
        fe["rs"] = sb("rs" + sfx, [128, 8], F32); fe["B_rs"] = [Buf("rs%s%d" % (sfx, i)) for i in range(2)]
        fe["junk"] = sb("junk" + sfx, [128, D], BF16); fe["B_junk"] = Buf("junk" + sfx)
        return fe

    def front_load(fe, g, src_rows, nb=4):
        b = g % 2
        xt, B_xt = fe["xt"][b], fe["B_xt"][b]
        P.dma(xt[:, 0:nb, :], src_rows.rearrange("(n p) d -> p n d", p=128), writes=[B_xt], sem=B_xt.sem)

    def front(fe, g, src_rows, nb=4, loaded=False):
        b = g % 2
        xt, B_xt = fe["xt"][b], fe["B_xt"][b]
        if not loaded:
            front_load(fe, g, src_rows, nb)
        ss, rs = fe["ss"], fe["rs"]
        for j in range(nb):
            P.op("act", R.activation(out=fe["junk"][:], in_=xt[:, j, :], func=AF.Square,
                                                    accum_out=ss[:, b * 4 + j:b * 4 + j + 1]),
                 reads=[B_xt], writes=[fe["B_junk"]], acc=[fe["B_ss"][b]])
        P.op("act", R.activation(out=rs[:, b * 4:b * 4 + nb], in_=ss[:, b * 4:b * 4 + nb], func=AF.Sqrt,
                                           bias=EPS_T[:, 0:1], scale=1.0 / D),
             reads=[fe["B_ss"][b], B_eps], writes=[fe["B_rs"][b]])
        P.op("dve", R.reciprocal(out=rs[:, b * 4:b * 4 + nb], in_=rs[:, b * 4:b * 4 + nb]),
             reads=[fe["B_rs"][b]], writes=[fe["B_rs"][b]])
        xn = fe["xn"]
        for j in range(nb):
            if j % 2 == 0:
                P.op("act", R.activation(out=xn[:, j, :], in_=xt[:, j, :], func=AF.Copy,
                                                        scale=rs[:, b * 4 + j:b * 4 + j + 1]),
                     reads=[B_xt, fe["B_rs"][b]], acc=[fe["B_xn"]])
            else:
                P.op("dve", R.tensor_scalar(out=xn[:, j, :], in0=xt[:, j, :],
                                                           scalar1=rs[:, b * 4 + j:b * 4 + j + 1], scalar2=None,
                                                           op0=ALU.mult),
                     reads=[B_xt, fe["B_rs"][b]], acc=[fe["B_xn"]])
        hT, B_hT = fe["hT"][b], fe["B_hT"][b]
        for kc in range(0, KC, 2):
            pb = (kc // 2) % 2
            for k2 in range(2):
                for j in range(nb):
                    last = (k2 == 1 and j == nb - 1)
                    P.op("pe", R.transpose(
                        out=psT[pb][:, k2 * 512 + j * 128:k2 * 512 + (j + 1) * 128],
                        in_=xn[:, j, (kc + k2) * 128:(kc + k2 + 1) * 128], identity=ident),
                        reads=[fe["B_xn"], B_cst], acc=[B_psT[pb]], inc=last)
            src = psT[pb][:, :].rearrange("p (k t) -> p k t", k=2)[:, :, 0:nb * 128]
            if (kc // 2) % 2 == 0:
                P.op("dve", R.tensor_copy(out=hT[:, kc:kc + 2, 0:nb * 128], in_=src),
                     reads=[B_psT[pb]], acc=[B_hT])
            else:
                P.op("act", R.copy(out=hT[:, kc:kc + 2, 0:nb * 128], in_=src),
                     reads=[B_psT[pb]], acc=[B_hT])
        return hT, B_hT

    def proj_fm(W, B_W, hT, B_hT, c0, m, n=512):
        pt, B_pt = next_ps()
        for kc in range(KC):
            P.op("pe", R.matmul(pt[0:m, 0:n], lhsT=W[:, kc, c0:c0 + m], rhs=hT[:, kc, 0:n],
                                                 start=(kc == 0), stop=(kc == KC - 1)),
                 reads=[B_W, B_hT], acc=[B_pt], inc=(kc == KC - 1))
        return pt, B_pt

    def proj_tm(W, B_W, hT, B_hT, j, c0, n):
        pt, B_pt = next_ps()
        for kc in range(KC):
            P.op("pe", R.matmul(pt[:, 0:n], lhsT=hT[:, kc, j * 128:(j + 1) * 128],
                                                 rhs=W[:, kc, c0:c0 + n], start=(kc == 0), stop=(kc == KC - 1)),
                 reads=[B_W, B_hT], acc=[B_pt], inc=(kc == KC - 1))
        return pt, B_pt

    if stop_after >= 1:
        ES.append(ExitStack())
        Wk = sb("Wk", [128, KC, 2112], BF16); B_Wk = Buf("Wk")
        stg = [sb("wstg%d" % i, [128, 2048], F32) for i in range(2)]
        B_stg = [Buf("wstg%d" % i) for i in range(2)]
        prep_weight(Wk, B_Wk, w_in, KC,
                    [(O_AK, 1024, 0, 1.0), (O_IK, 64, 1024, 1.0), (O_BK, 1024, 1088, 1.0)], 0, stg, B_stg)
        fe = make_front("a")
        fst = [sb("fst%d" % i, [128, 9, 512], BF16) for i in range(2)]
        B_fst = [Buf("fst%d" % i) for i in range(2)]
        avst = [sb("avst%d" % i, [128, 8, 4, 65], BF16) for i in range(2)]
        B_avst = [Buf("avst%d" % i) for i in range(2)]
        bvst = [sb("bvst%d" % i, [128, 8, 4, 64], BF16) for i in range(2)]
        B_bvst = [Buf("bvst%d" % i) for i in range(2)]
        for i in range(2):
            P.op("pool", R.memset(avst[i][:], 1.0), writes=[B_avst[i]])
        front_load(fe, 0, xf[0:512, :])
        for g in range(NGF):
            b = g % 2
            if g + 1 < NGF:
                front_load(fe, g + 1, xf[(g + 1) * 512:(g + 2) * 512, :])
            hT, B_hT = front(fe, g, None, loaded=True)
            for oc in range(9):
                if oc < 4:
                    c0, m = oc * 128, 128
                elif oc < 8:
                    c0, m = 1088 + (oc - 4) * 128, 128
                else:
                    c0, m = 1024, 64
                pt, B_pt = proj_fm(Wk, B_Wk, hT, B_hT, c0, m)
                evac(fst[b][0:m, oc, :], pt[0:m, :], B_pt, B_fst[b])
            store(akT[:, g * 512:(g + 1) * 512].rearrange("(c p) t -> p c t", p=128), fst[b][:, 0:4, :], B_fst[b])
            store(bkT[:, g * 512:(g + 1) * 512].rearrange("(c p) t -> p c t", p=128), fst[b][:, 4:8, :], B_fst[b])
            store(ikT[:, g * 512:(g + 1) * 512], fst[b][0:64, 8, :], B_fst[b])
            for j in range(4):
                pt, B_pt = proj_tm(Wk, B_Wk, hT, B_hT, j, 512, 512)
                evac(avst[b][:, :, j, 0:64], pt[:, :].rearrange("p (h d) -> p h d", h=8), B_pt, B_avst[b])
                pt, B_pt = proj_tm(Wk, B_Wk, hT, B_hT, j, 1600, 512)
                evac(bvst[b][:, :, j, :], pt[:, :].rearrange("p (h d) -> p h d", h=8), B_pt, B_bvst[b])
            for h in range(8):
                store(avh[h, :, g * 4:(g + 1) * 4, :], avst[b][:, h, :, :], B_avst[b])
                store(bvh[h, :, g * 4:(g + 1) * 4, :], bvst[b][:, h, :, :], B_bvst[b])
        P.barrier()
        ES.pop().close()

    if stop_after >= 2:
        ES.append(ExitStack())
        Wq = sb("Wq", [128, KC, 5128], BF16); B_Wq = Buf("Wq")
        Wm = sb("Wm", [128, KC, 1024], BF16); B_Wm = Buf("Wm")
        ES.append(ExitStack())
        stg = [sb("wstg%d" % i, [128, 2048], F32) for i in range(2)]
        B_stg = [Buf("wstgq%d" % i) for i in range(2)]
        prep_weight(Wm, B_Wm, w_mem_kv, KC, [(0, 1024, 0, 1.0)], 1, stg, B_stg)
        prep_weight(Wq, B_Wq, w_in, KC,
                    [(O_AQ, 512, 0, 0.125), (O_IQ, 512, 512, 1.0), (O_BQ, 512, 1024, 0.125),
                     (O_CQ, 512, 1536, 128 ** -0.5), (O_IW, 8, 2048, 1.0), (O_G, 3072, 2056, 1.0)], 0, stg, B_stg)
        P.barrier()
        ES.pop().close()
        fe = make_front("q")
        hT, B_hT = front(fe, 1, mem[:, :], nb=2)
        for h in range(4):
            pt, B_pt = proj_fm(Wm, B_Wm, hT, B_hT, h * 128, 128, n=NMEM)
            evac(mkT[:, h, :], pt[:, 0:NMEM], B_pt, B_mkT)
        for j in range(2):
            pt, B_pt = proj_tm(Wm, B_Wm, hT, B_hT, j, 512, 512)
            evac(mvS[:, j, :], pt[:, :], B_pt, B_mvS)
        qst = [sb("qst%d" % i, [128, 12, 512], BF16) for i in range(1)] * 2
        B_qst = [Buf("qst%d" % i) for i in range(1)] * 2
        cqs = [sb("cqs%d" % i, [128, 4, 512], BF16) for i in range(1)] * 2
        B_cqs = [Buf("cqs%d" % i) for i in range(1)] * 2
        gst = [sb("gst%d" % i, [128, 512], BF16) for i in range(4)]
        B_gst = [Buf("gst%d" % i) for i in range(4)]
        pm = [sb("pm%d" % i, [128, 512], BF16) for i in range(4)]
        B_pm = [Buf("pm%d" % i) for i in range(4)]
        rD = [sb("rD%d" % i, [128, 512], F32) for i in range(2)]
        B_rD = [Buf("rD%d" % i) for i in range(2)]
        ocst = [sb("ocst%d" % i, [128, 4, 512], BF16) for i in range(2)]
        B_ocst = [Buf("ocst%d" % i) for i in range(2)]
        gi = 0
        pmi = 0
        front_load(fe, 0, xq[0:512, :])
        for g in range(NSB):
            b = g % 2
            tsl = slice(g * 512, (g + 1) * 512)
            if g + 1 < NSB:
                front_load(fe, g + 1, xq[(g + 1) * 512:(g + 2) * 512, :])
            hT, B_hT = front(fe, g, None, loaded=True)
            for oc in range(12):
                pt, B_pt = proj_fm(Wq, B_Wq, hT, B_hT, oc * 128, 128)
                evac(qst[b][:, oc, :], pt[:, :], B_pt, B_qst[b])
            store(aqT[:, tsl].rearrange("(c p) t -> p c t", p=128), qst[b][:, 0:4, :], B_qst[b])
            store(iqT[:, tsl].rearrange("(c p) t -> p c t", p=128), qst[b][:, 4:8, :], B_qst[b])
            store(bqT[:, tsl].rearrange("(c p) t -> p c t", p=128), qst[b][:, 8:12, :], B_qst[b])
            for h in range(4):
                pt, B_pt = proj_fm(Wq, B_Wq, hT, B_hT, 1536 + h * 128, 128)
                evac(cqs[b][:, h, :], pt[:, :], B_pt, B_cqs[b])
            for j in range(4):
                pt, B_pt = proj_tm(Wq, B_Wq, hT, B_hT, j, 2048, 8)
                P.op("dve", R.tensor_scalar(
                    out=iwsgn[:, g * 4 + j, :], in0=pt[:, 0:8], scalar1=0.0, scalar2=0.5,
                    op0=ALU.is_ge, op1=ALU.subtract),
                    reads=[B_pt], acc=[B_iwsgn])
                P.op("dve", R.scalar_tensor_tensor(
                    out=iwabs[:, g * 4 + j, :], in0=pt[:, 0:8], scalar=2.0, in1=iwsgn[:, g * 4 + j, :],
                    op0=ALU.mult, op1=ALU.mult),
                    reads=[B_pt, B_iwsgn], acc=[B_iwabs])
            for h in range(4):
                pms = []
                for mt in range(2):
                    pz, B_pz = next_ps()
                    P.op("pe", R.matmul(
                        pz[:, :], lhsT=mkT[:, h, mt * 128:(mt + 1) * 128], rhs=cqs[b][:, h, :],
                        start=True, stop=True), reads=[B_mkT, B_cqs[b]], writes=[B_pz])
                    k = pmi % 4
                    pmi += 1
                    P.op("act", R.activation(out=pm[k][:], in_=pz[:, :], func=AF.Exp),
                         reads=[B_pz], writes=[B_pm[k]])
                    pms.append(k)
                po, B_po = next_ps()
                pd, B_pd = next_ps()
                for mt in range(2):
                    k = pms[mt]
                    P.op("pe", R.matmul(
                        po[:, :], lhsT=mvS[:, mt, h * 128:(h + 1) * 128], rhs=pm[k][:],
                        start=(mt == 0), stop=(mt == 1)), reads=[B_mvS, B_pm[k]], acc=[B_po], inc=(mt == 1))
                for mt in range(2):
                    k = pms[mt]
                    P.op("pe", R.matmul(
                        pd[:, :], lhsT=ones_b, rhs=pm[k][:], start=(mt == 0), stop=(mt == 1)),
                        reads=[B_cst, B_pm[k]], acc=[B_pd], inc=(mt == 1))
                r = h % 2
                P.op("dve", R.reciprocal(out=rD[r][:], in_=pd[:, :]),
                     reads=[B_pd], writes=[B_rD[r]])
                P.op("dve", R.tensor_tensor(
                    out=ocst[b][:, h, :], in0=po[:, :], in1=rD[r][:], op=ALU.mult),
                    reads=[B_po, B_rD[r]], acc=[B_ocst[b]])
            store(OcT[:, tsl].rearrange("(c p) t -> p c t", p=128), ocst[b][:, :, :], B_ocst[b])
            for oc in range(24):
                pt, B_pt = proj_fm(Wq, B_Wq, hT, B_hT, 2056 + oc * 128, 128)
                k = gi % 4
                gi += 1
                P.op("act", R.activation(
                    out=gst[k][:], in_=pt[:, :], func=AF.Sigmoid, bias=bgate[:, oc:oc + 1]),
                    reads=[B_pt, B_bgate], writes=[B_gst[k]])
                store(gT[oc, :, tsl], gst[k][:], B_gst[k])
        P.barrier()
        ES.pop().close()

    ONE_T = sb("one_t", [128, 1], F32); B_one = Buf("one")
    P.op("pool", R.memset(ONE_T[:], 1.0), writes=[B_one])
    if stop_after >= 3:
        ES.append(ExitStack())
        kTs = [sb("kTs%d" % i, [128, S], BF16) for i in range(2)]; B_kTs = [Buf("kTs%d" % i) for i in range(2)]
        Vs = [sb("Vs%d" % i, [128, NT, 64], BF16) for i in range(2)]; B_Vs = [Buf("Vs%d" % i) for i in range(2)]
        qs = [sb("qs%d" % i, [128, 512], BF16) for i in range(2)]; B_qs = [Buf("qs%d" % i) for i in range(2)]
        for i in range(2):
            P.op("pool", R.memset(kTs[i][64:128, :], 0.0), acc=[B_kTs[i]])
            P.op("pool", R.memset(qs[i][64:128, :], 0.0), acc=[B_qs[i]])
        maskc = sb("maskc", [128, 8, 512], BF16); B_maskc = Buf("maskc")
        P.dma(maskc[:], maskc_in[:, :, :], writes=[B_maskc], sem=B_maskc.sem)
        e_sb = [sb("e_sb%d" % i, [128, 512], F32) for i in range(2)]; B_e = [Buf("e%d" % i) for i in range(2)]
        sp_sb = [sb("sp_sb%d" % i, [128, 512], BF16) for i in range(4)]; B_sp = [Buf("sp%d" % i) for i in range(4)]
        spacc = [sb("spacc%d" % i, [128, 512], BF16) for i in range(2)]; B_spacc = [Buf("spacc%d" % i) for i in range(2)]
        a_sb = [sb("a_sb%d" % i, [128, 512], BF16) for i in range(3)]; B_a = [Buf("a%d" % i) for i in range(3)]
        obst = [sb("obst%d" % i, [64, 512], BF16) for i in range(2)]; B_obst = [Buf("obst%d" % i) for i in range(2)]
        items = [(G, h) for G in range(NSB) for h in range(8)]

        def sb_load(idx):
            G, h = items[idx]
            kb = idx % 2
            KL = (G + 1) * 1024
            P.dma(kTs[kb][0:64, 0:KL], bkT[h * 64:(h + 1) * 64, 0:KL], acc=[B_kTs[kb]], sem=B_kTs[kb].sem)
            P.dma(Vs[kb][:, 0:KL // 128, :], bvh[h, :, 0:KL // 128, :], writes=[B_Vs[kb]], sem=B_Vs[kb].sem)
            P.dma(qs[kb][0:64, :], bqT[h * 64:(h + 1) * 64, G * 512:(G + 1) * 512], acc=[B_qs[kb]], sem=B_qs[kb].sem)

        cz = [0]; csp = [0]; cc = [0]; ca = [0]
        sb_load(0)
        for idx, (G, h) in enumerate(items):
            kb = idx % 2
            if idx + 1 < len(items):
                sb_load(idx + 1)
            n = (G + 1) * 8
            psO, B_psO = psb[4 + kb], B_ps[4 + kb]
            st = {}
            st2 = {}

            def stage1(i):
                kt = n - 1 - i
                j = kt - 8 * G
                zi = cz[0] % 2; cz[0] += 1
                si = csp[0] % 4; csp[0] += 1
                st[i] = (zi, si)
                P.op("pe", R.matmul(psb[zi][:, :], lhsT=kTs[kb][:, kt * 128:(kt + 1) * 128], rhs=qs[kb][:],
                                    start=True, stop=True),
                     reads=[B_kTs[kb], B_qs[kb]], writes=[B_ps[zi]])
                P.op("act", R.activation(out=e_sb[zi % 2][:], in_=psb[zi][:, :], func=AF.Exp),
                     reads=[B_ps[zi]], writes=[B_e[zi % 2]])
                P.op("act", R.activation(out=sp_sb[si][:], in_=e_sb[zi % 2][:], func=AF.Ln, bias=ONE_T[:, 0:1]),
                     reads=[B_e[zi % 2], B_one], writes=[B_sp[si]])
                if j >= 0:
                    P.op("dve", R.tensor_tensor(out=sp_sb[si][:], in0=sp_sb[si][:], in1=maskc[:, j, :], op=ALU.mult),
                         reads=[B_sp[si], B_maskc], writes=[B_sp[si]])

            accprev = [None, None]

            def stage2(i):
                kt = n - 1 - i
                j = kt - 8 * G
                zi, si = st.pop(i)
                ci = 2 + (cc[0] % 2); cc[0] += 1
                ai = ca[0] % 3; ca[0] += 1
                st2[i] = ai
                P.op("pe", R.matmul(psb[ci][:, :], lhsT=kTs[kb][:, kt * 128:(kt + 1) * 128], rhs=qs[kb][:],
                                    start=True, stop=False),
                     reads=[B_kTs[kb], B_qs[kb]], writes=[B_ps[ci]], inc=False)
                P.op("pe", R.matmul(psb[ci][:, :], lhsT=negtri, rhs=sp_sb[si][:], start=False, stop=(i == 0)),
                     reads=[B_cst, B_sp[si]], acc=[B_ps[ci]], inc=(i == 0))
                if i > 0:
                    ap_prev, B_prev = accprev
                    P.op("pe", R.matmul(psb[ci][:, :], lhsT=negones, rhs=ap_prev, start=False, stop=True),
                         reads=[B_cst, B_prev], acc=[B_ps[ci]])
                if i < n - 1:
                    if i == 0:
                        accprev[0], accprev[1] = sp_sb[si][:], B_sp[si]
                    else:
                        ap_prev, B_prev = accprev
                        k = i % 2
                        P.op("pool", R.tensor_tensor(out=spacc[k][:], in0=ap_prev, in1=sp_sb[si][:], op=ALU.add),
                             reads=[B_prev, B_sp[si]], writes=[B_spacc[k]])
                        accprev[0], accprev[1] = spacc[k][:], B_spacc[k]
                P.op("act", R.activation(out=a_sb[ai][:], in_=psb[ci][:, :], func=AF.Exp),
                     reads=[B_ps[ci]], writes=[B_a[ai]])
                if j >= 0:
                    P.op("dve", R.tensor_tensor(out=a_sb[ai][:], in0=a_sb[ai][:], in1=maskc[:, j, :], op=ALU.mult),
                         reads=[B_a[ai], B_maskc], writes=[B_a[ai]])

            def stage3(i):
                kt = n - 1 - i
                ai = st2.pop(i)
                if i == 0:
                    P.op("pe", R.matmul(psO[0:64, :], lhsT=Vs[kb][:, kt, :], rhs=a_sb[ai][:],
                                        start=True, stop=(n == 1)),
                         reads=[B_Vs[kb], B_a[ai]], writes=[B_psO])
                else:
                    P.op("pe", R.matmul(psO[0:64, :], lhsT=Vs[kb][:, kt, :], rhs=a_sb[ai][:],
                                        start=False, stop=(i == n - 1)),
                         reads=[B_Vs[kb], B_a[ai]], acc=[B_psO])

            for step in range(n + 2):
                if step < n:
                    stage1(step)
                if 0 <= step - 1 < n:
                    stage2(step - 1)
                if 0 <= step - 2 < n:
                    stage3(step - 2)
            evac(obst[kb][:], psO[0:64, :], B_psO, B_obst[kb])
            store(ObT[h * 64:(h + 1) * 64, G * 512:(G + 1) * 512], obst[kb][:], B_obst[kb])
        P.barrier()
        ES.pop().close()

    if stop_after >= 4:
        ES.append(ExitStack())
        ikTs = sb("ikTs", [128, S], BF16); B_ikTs = Buf("ikTs")
        P.op("pool", R.memset(ikTs[64:128, :], 0.0), acc=[B_ikTs])
        P.dma(ikTs[0:64, :], ikT[:, :], acc=[B_ikTs], sem=B_ikTs.sem)
        negadm = sb("negadm", [128, 4, 1024], BF16); B_negadm = Buf("negadm")
        P.dma(negadm[:], negadm_in[:, :, :], writes=[B_negadm], sem=B_negadm.sem)
        iqs = [sb("iqs%d" % i, [128, 8, 512], BF16) for i in range(2)]; B_iqs = [Buf("iqs%d" % i) for i in range(2)]
        for i in range(2):
            P.op("pool", R.memset(iqs[i][64:128, :, :], 0.0), acc=[B_iqs[i]])
        sc = [sb("sc%d" % i, [128, S], F32) for i in range(2)]
        B_scc = [[Buf("sc%d_%d" % (i, c)) for c in range(S // 512)] for i in range(2)]
        B_sc = [Buf("scw%d" % i) for i in range(2)]
        r_sb = [sb("r_sb%d" % i, [128, 512], BF16) for i in range(4)]; B_r = [Buf("r%d" % i) for i in range(4)]
        dgs = [sb("dgs%d" % i, [128, 8, 128], BF16) for i in range(2)]; B_dgs = [Buf("dgs%d" % i) for i in range(2)]
        szi = [0]
        selt = [sb("selt%d" % i, [128, S], BF16) for i in range(2)]; B_sel = [Buf("sel%d" % i) for i in range(2)]
        junk4 = sb("junk4", [128, S], BF16); B_junk4 = Buf("junk4")
        junk5 = sb("junk5", [128, S], BF16); B_junk5 = Buf("junk5")
        sm = [sb("sm%d" % i, [128, 8], F32) for i in range(2)]
        B_sm = [[Buf("sm%d_%d" % (i, k)) for k in range(8)] for i in range(2)]
        mst = [sb("mst%d" % i, [128, 8, 128], BF16) for i in range(3)]; B_mst = [Buf("mst%d" % i) for i in range(3)]
        ri = 0
        msi = 0
        pti = 0
        P.dma(iqs[0][0:64, :, :], iqT.rearrange("(h d) t -> d h t", d=64)[:, :, 0:512], acc=[B_iqs[0]], sem=B_iqs[0].sem)
        for G in range(NSB):
            gb = G % 2
            if G + 1 < NSB:
                P.dma(iqs[1 - gb][0:64, :, :], iqT.rearrange("(h d) t -> d h t", d=64)[:, :, (G + 1) * 512:(G + 2) * 512],
                      acc=[B_iqs[1 - gb]], sem=B_iqs[1 - gb].sem)
            KL = (G + 1) * 1024
            nch = KL // 512
            for qp in range(2):
                chains = []
                for qi in range(2):
                    qb = 2 * qp + qi
                    blk = G * 4 + qb
                    cb = qi
                    scb = sc[cb]
                    smb, B_smb = sm[cb], B_sm[cb]
                    for ih in range(8):
                        P.op("pool", R.tensor_scalar(out=dgs[cb][:, ih, :], in0=ident, scalar1=iwabs[:, blk, ih:ih + 1],
                                                     scalar2=iwsgn[:, blk, ih:ih + 1], op0=ALU.mult, op1=ALU.mult),
                             reads=[B_cst, B_iwabs, B_iwsgn], acc=[B_dgs[cb]],
                             extra=(list(B_dgs[cb].r.items()) if ih == 0 else []))
                    its = [(c, ih) for c in range(nch) for ih in range(8)]
                    pend = []
                    for n_it in range(len(its) + 2):
                        if n_it < len(its):
                            c, ih = its[n_it]
                            z = szi[0] % 4; szi[0] += 1
                            P.op("pe", R.matmul(psb[z][:, :], lhsT=iqs[gb][:, ih, qb * 128:(qb + 1) * 128],
                                                rhs=ikTs[:, c * 512:(c + 1) * 512], start=True, stop=True),
                                 reads=[B_iqs[gb], B_ikTs], writes=[B_ps[z]])
                            k = ri % 4
                            ri += 1
                            P.op("act", R.activation(out=r_sb[k][:], in_=psb[z][:, :], func=AF.Relu),
                                 reads=[B_ps[z]], writes=[B_r[k]])
                            pend.append((c, ih, k))
                        if n_it >= 2:
                            c, ih, k = pend.pop(0)
                            ab = 4 + (c % 2)
                            if ih == 0:
                                P.op("pe", R.matmul(psb[ab][:, :], lhsT=dgs[cb][:, ih, :], rhs=r_sb[k][:],
                                                    start=True, stop=False),
                                     reads=[B_dgs[cb], B_r[k]], writes=[B_ps[ab]], inc=True)
                            else:
                                P.op("pe", R.matmul(psb[ab][:, :], lhsT=dgs[cb][:, ih, :], rhs=r_sb[k][:],
                                                    start=False, stop=(ih == 7)),
                                     reads=[B_dgs[cb], B_r[k]], acc=[B_ps[ab]], inc=True)
                            if ih == 7:
                                dst = scb[:, c * 512:(c + 1) * 512]
                                if c % 2 == 0:
                                    P.op("dve", R.tensor_copy(out=dst, in_=psb[ab][:, :]),
                                         reads=[B_ps[ab]], writes=[B_scc[cb][c]], extra=list(B_sc[cb].r.items()))
                                else:
                                    P.op("act", R.copy(out=dst, in_=psb[ab][:, :]),
                                         reads=[B_ps[ab]], writes=[B_scc[cb][c]], extra=list(B_sc[cb].r.items()))
                    chunks = [B_scc[cb][c] for c in range(nch)]
                    P.op("dve", R.tensor_reduce(out=smb[:, 0:1], in_=scb[:, 0:KL], axis=AX.X, op=ALU.max),
                         reads=chunks, writes=[B_smb[0]])
                    P.op("dve", R.tensor_reduce(out=smb[:, 1:2], in_=scb[:, 0:KL], axis=AX.X, op=ALU.min),
                         reads=chunks, writes=[B_smb[1]])
                    P.op("dve", R.tensor_tensor(out=scb[:, KL - 1024:KL], in0=scb[:, KL - 1024:KL],
                                                in1=negadm[:, qb, :], op=ALU.add),
                         reads=chunks + [B_negadm], writes=[B_sc[cb]] + chunks[-2:])
                    P.op("dve", R.tensor_tensor(out=smb[:, 2:3], in0=smb[:, 0:1], in1=smb[:, 1:2], op=ALU.subtract),
                         reads=[B_smb[0], B_smb[1]], writes=[B_smb[2]])
                    P.op("dve", R.tensor_scalar(out=smb[:, 3:4], in0=smb[:, 2:3], scalar1=1.0 + 2.0 ** -9,
                                                scalar2=2e-20, op0=ALU.mult, op1=ALU.add),
                         reads=[B_smb[2]], writes=[B_smb[3]])
                    P.op("dve", R.scalar_tensor_tensor(out=smb[:, 4:5], in0=smb[:, 2:3], scalar=-(2.0 ** -10),
                                                       in1=smb[:, 1:2], op0=ALU.mult, op1=ALU.add),
                         reads=[B_smb[2], B_smb[1]], writes=[B_smb[4]])
                    chains.append((qb, cb, scb, smb, B_smb))
                for k in range(NBIS):
                    hk = 2.0 ** -(k + 1)
                    for (qb, cb, scb, smb, B_smb) in chains:
                        if cb == 0:
                            P.op("dve", R.scalar_tensor_tensor(out=smb[:, 5:6], in0=smb[:, 3:4], scalar=hk,
                                                               in1=smb[:, 4:5], op0=ALU.mult, op1=ALU.add),
                                 reads=[B_smb[3], B_smb[4]], writes=[B_smb[5]])
                        else:
                            P.op("dve", R.scalar_tensor_tensor(out=smb[:, 5:6], in0=smb[:, 3:4], scalar=-hk,
                                                               in1=smb[:, 4:5], op0=ALU.mult, op1=ALU.subtract),
                                 reads=[B_smb[3], B_smb[4]], writes=[B_smb[5]])
                    for (qb, cb, scb, smb, B_smb) in reversed(chains):
                        if cb == 0:
                            P.op("dve", R.tensor_scalar(out=junk4[:, 0:KL], in0=scb[:, 0:KL], scalar1=smb[:, 5:6],
                                                        scalar2=None, op0=ALU.is_ge, op1=ALU.add,
                                                        accum_out=smb[:, 6:7]),
                                 reads=[B_sc[cb], B_smb[5]], writes=[B_junk4, B_smb[6]])
                        else:
                            P.op("act", R.activation(out=junk5[:, 0:KL], in_=scb[:, 0:KL], func=AF.Sign,
                                                     bias=smb[:, 5:6], accum_out=smb[:, 6:7]),
                                 reads=[B_sc[cb], B_smb[5]], writes=[B_junk5, B_smb[6]])
                    for (qb, cb, scb, smb, B_smb) in chains:
                        thr = (KSEL - 0.5) if cb == 0 else (2.0 * KSEL - 1.0 - KL)
                        P.op("dve", R.tensor_scalar(out=smb[:, 7:8], in0=smb[:, 6:7], scalar1=thr,
                                                    scalar2=hk, op0=ALU.is_ge, op1=ALU.mult),
                             reads=[B_smb[6]], writes=[B_smb[7]])
                        P.op("dve", R.scalar_tensor_tensor(out=smb[:, 4:5], in0=smb[:, 7:8], scalar=smb[:, 3:4],
                                                           in1=smb[:, 4:5], op0=ALU.mult, op1=ALU.add),
                             reads=[B_smb[7], B_smb[3], B_smb[4]], writes=[B_smb[4]])
                for (qb, cb, scb, smb, B_smb) in chains:
                    P.op("dve", R.tensor_scalar(out=selt[cb][:, 0:KL], in0=scb[:, 0:KL], scalar1=smb[:, 4:5],
                                                scalar2=None, op0=ALU.is_ge),
                         reads=[B_sc[cb], B_smb[4]], writes=[B_sel[cb]])
                    for k0 in range(0, KL // 128, 8):
                        pb = pti % 2
                        pti += 1
                        for kk in range(8):
                            kt = k0 + kk
                            P.op("pe", R.transpose(
                                out=psT[pb][:, kk * 128:(kk + 1) * 128], in_=selt[cb][:, kt * 128:(kt + 1) * 128],
                                identity=ident), reads=[B_sel[cb], B_cst], acc=[B_psT[pb]], inc=(kk == 7))
                        m = msi % 3
                        msi += 1
                        P.op("act", R.copy(out=mst[m][:], in_=psT[pb][:, :].rearrange(
                            "p (k t) -> p k t", k=8)), reads=[B_psT[pb]], writes=[B_mst[m]])
                        store(mskT[G, :, k0:k0 + 8, qb * 128:(qb + 1) * 128], mst[m][:], B_mst[m])
        P.barrier()
        ES.pop().close()

    if stop_after >= 5:
        ES.append(ExitStack())
        kTs = [sb("akTs%d" % i, [128, S], BF16) for i in range(2)]; B_kTs = [Buf("akTs%d" % i) for i in range(2)]
        Vs = [sb("aVs%d" % i, [128, NT, 65], BF16) for i in range(2)]; B_Vs = [Buf("aVs%d" % i) for i in range(2)]
        qs = [sb("aqs%d" % i, [128, 512], BF16) for i in range(2)]; B_qs = [Buf("aqs%d" % i) for i in range(2)]
        for i in range(2):
            P.op("pool", R.memset(kTs[i][64:128, :], 0.0), acc=[B_kTs[i]])
            P.op("pool", R.memset(qs[i][64:128, :], 0.0), acc=[B_qs[i]])
        bsf = [sb("bsf%d" % i, [128, 9, 512], F32) for i in range(2)]; B_bsf = [Buf("bsf%d" % i) for i in range(2)]
        bsb = [sb("bsb%d" % i, [128, 9, 512], BF16) for i in range(2)]; B_bsb = [Buf("bsb%d" % i) for i in range(2)]
        biasc = sb("biasc", [128, 8], F32); B_biasc = Buf("biasc")
        P.dma(biasc[:], biasc_in[:, :], writes=[B_biasc], sem=B_biasc.sem)
        mk_sb = sb("mk_sb", [128, NT, 512], BF16); B_mk = [Buf("mk%d" % i) for i in range(NT // 8)]
        p_sb = [sb("p_sb%d" % i, [128, 512], BF16) for i in range(3)]; B_p = [Buf("p%d" % i) for i in range(3)]
        pm_sb = [sb("pm_sb%d" % i, [128, 512], BF16) for i in range(4)]; B_pmm = [Buf("pmm%d" % i) for i in range(4)]
        Osb = [sb("Osb%d" % i, [65, 512], F32) for i in range(2)]; B_Osb = [Buf("Osb%d" % i) for i in range(2)]
        oast = [sb("oast%d" % i, [64, 512], BF16) for i in range(2)]; B_oast = [Buf("oast%d" % i) for i in range(2)]
        sel65 = sb("sel65", [65, 64], F32); B_sel65 = Buf("sel65")
        P.op("pool", R.memset(sel65[:], 0.0), writes=[B_sel65])
        P.op("pool", R.memset(sel65[64:65, :], 1.0), reads=[B_sel65], writes=[B_sel65])
        items = [(G, h) for G in range(NSB) for h in range(8)]

        def dsa_load(idx):
            G, h = items[idx]
            kb = idx % 2
            KL = (G + 1) * 1024
            P.dma(kTs[kb][0:64, 0:KL], akT[h * 64:(h + 1) * 64, 0:KL], acc=[B_kTs[kb]], sem=B_kTs[kb].sem)
            P.dma(Vs[kb][:, 0:KL // 128, :], avh[h, :, 0:KL // 128, :], writes=[B_Vs[kb]], sem=B_Vs[kb].sem)
            P.dma(qs[kb][0:64, :], aqT[h * 64:(h + 1) * 64, G * 512:(G + 1) * 512], acc=[B_qs[kb]], sem=B_qs[kb].sem)
            P.dma(bsf[kb][:], biasT_in[h, :, :, :], writes=[B_bsf[kb]], sem=B_bsf[kb].sem)
            P.op("pool", R.tensor_copy(out=bsb[kb][:], in_=bsf[kb][:]), reads=[B_bsf[kb]], writes=[B_bsb[kb]])

        pi = 0
        zi = 0
        dsa_load(0)
        for idx, (G, h) in enumerate(items):
            kb = idx % 2
            if h == 0:
                for sg in range(G + 1):
                    P.dma(mk_sb[:, sg * 8:(sg + 1) * 8, :], mskT[G, :, sg * 8:(sg + 1) * 8, :],
                          writes=[B_mk[sg]], sem=B_mk[sg].sem)
            if idx + 1 < len(items):
                dsa_load(idx + 1)
            n = (G + 1) * 8
            psO, B_psO = psb[4 + kb], B_ps[4 + kb]
            LA = 2
            pmk = {}
            for step in range(n + LA):
                if step < n:
                    kt = step
                    j = kt - 8 * G
                    near = j >= -1
                    z = zi % 4
                    zi += 1
                    k = pi % 3
                    km = pi % 4
                    pi += 1
                    pmk[kt] = km
                    P.op("pe", R.matmul(
                        psb[z][:, :], lhsT=kTs[kb][:, kt * 128:(kt + 1) * 128], rhs=qs[kb][:], start=True, stop=not near),
                        reads=[B_kTs[kb], B_qs[kb]], writes=[B_ps[z]], inc=not near)
                    if near:
                        P.op("pe", R.matmul(psb[z][:, :], lhsT=ident, rhs=bsb[kb][:, j + 1, :], start=False, stop=True),
                             reads=[B_cst, B_bsb[kb]], acc=[B_ps[z]])
                        P.op("act", R.activation(out=p_sb[k][:], in_=psb[z][:, :], func=AF.Exp),
                             reads=[B_ps[z]], writes=[B_p[k]])
                    else:
                        P.op("act", R.activation(out=p_sb[k][:], in_=psb[z][:, :], func=AF.Exp, bias=biasc[:, h:h + 1]),
                             reads=[B_ps[z], B_biasc], writes=[B_p[k]])
                    P.op("dve", R.tensor_tensor(out=pm_sb[km][:], in0=p_sb[k][:], in1=mk_sb[:, kt, :], op=ALU.mult),
                         reads=[B_p[k], B_mk[kt // 8]], writes=[B_pmm[km]])
                kt = step - LA
                if kt >= 0:
                    km = pmk.pop(kt)
                    if kt == 0:
                        P.op("pe", R.matmul(psO[0:65, :], lhsT=Vs[kb][:, kt, :], rhs=pm_sb[km][:],
                                            start=True, stop=(n == 1)),
                             reads=[B_Vs[kb], B_pmm[km]], writes=[B_psO])
                    else:
                        P.op("pe", R.matmul(psO[0:65, :], lhsT=Vs[kb][:, kt, :], rhs=pm_sb[km][:],
                                            start=False, stop=(kt == n - 1)),
                             reads=[B_Vs[kb], B_pmm[km]], acc=[B_psO])
            P.op("act", R.copy(out=Osb[kb][:], in_=psO[0:65, :]), reads=[B_psO], writes=[B_Osb[kb]])
            P.op("dve", R.reciprocal(out=Osb[kb][64:65, :], in_=Osb[kb][64:65, :]),
                 reads=[B_Osb[kb]], writes=[B_Osb[kb]])
            pt, B_pt = psb[zi % 4], B_ps[zi % 4]
            zi += 1
            P.op("pe", R.matmul(pt[0:64, :], lhsT=sel65[:, :], rhs=Osb[kb][:, :], start=True, stop=True),
                 reads=[B_sel65, B_Osb[kb]], writes=[B_pt])
            P.op("dve", R.tensor_tensor(out=oast[kb][:], in0=Osb[kb][0:64, :], in1=pt[0:64, :],
                                                         op=ALU.mult),
                 reads=[B_Osb[kb], B_pt], writes=[B_oast[kb]])
            store(OaT[h * 64:(h + 1) * 64, G * 512:(G + 1) * 512], oast[kb][:], B_oast[kb])
        P.barrier()
        ES.pop().close()

    gpost = sb("gpost", [128, 2, D], F32); B_gpost = Buf("gpost")
    P.dma(gpost[:], grows[:, :, :], writes=[B_gpost], sem=B_gpost.sem)

    def post_norm(psA, B_psA, psB_, B_psB, gi, res_ap, B_res, out_ap, B_out, tmp, B_tmp, ss2, B_ss2, junk, B_junk):
        P.op("act", R.activation(out=junk[:, 0:512], in_=psA[:, :], func=AF.Square, accum_out=ss2[:, 0:1]),
             reads=[B_psA], writes=[B_junk], acc=[B_ss2])
        P.op("act", R.activation(out=junk[:, 0:512], in_=psB_[:, :], func=AF.Square, accum_out=ss2[:, 1:2]),
             reads=[B_psB], writes=[B_junk], acc=[B_ss2])
        P.op("dve", R.tensor_tensor(out=ss2[:, 2:3], in0=ss2[:, 0:1], in1=ss2[:, 1:2], op=ALU.add),
             reads=[B_ss2], writes=[B_ss2])
        P.op("act", R.activation(out=ss2[:, 3:4], in_=ss2[:, 2:3], func=AF.Sqrt, bias=EPS_T[:, 0:1],
                                           scale=1.0 / D), reads=[B_ss2, B_eps], writes=[B_ss2])
        P.op("dve", R.reciprocal(out=ss2[:, 3:4], in_=ss2[:, 3:4]), reads=[B_ss2], writes=[B_ss2])
        for half, (pp, B_pp) in enumerate(((psA, B_psA), (psB_, B_psB))):
            hs = slice(half * 512, (half + 1) * 512)
            P.op("dve", R.scalar_tensor_tensor(
                out=tmp[:, hs], in0=pp[:, :], scalar=ss2[:, 3:4], in1=gpost[:, gi, hs], op0=ALU.mult, op1=ALU.mult),
                reads=[B_pp, B_ss2, B_gpost], acc=[B_tmp])
        P.op("pool", R.tensor_tensor(out=out_ap, in0=tmp[:, :], in1=res_ap, op=ALU.add),
             reads=[B_tmp, B_res], acc=[B_out])

    if stop_after >= 6:
        ES.append(ExitStack())
        Wup = sb("Wup", [128, 12, D], BF16); B_Wup = Buf("Wup")
        Wo = sb("Wo", [128, KC, D], BF16); B_Wo = Buf("Wo")
        stg = [sb("wstg6%d" % i, [128, 2048], F32) for i in range(2)]
        B_stg = [Buf("wstg6%d" % i) for i in range(2)]
        prep_weight(Wup, B_Wup, w_up.rearrange("r k n -> (r k) n"), 12, [(0, 1024, 0, 1.0)], None, stg, B_stg)
        prep_weight(Wo, B_Wo, w_out, KC, [(0, 1024, 0, 1.0)], None, stg, B_stg)
        OT = [sb("OT%d" % r, [128, 4, 512], BF16) for r in range(3)]; B_OT = [Buf("OT%d" % r) for r in range(3)]
        gts = sb("gts", [128, 24, 512], BF16); B_gts = Buf("gts")
        xo = sb("xo", [128, 4, D], F32); B_xo = Buf("xo")
        mT = sb("mT", [128, KC, 512], BF16); B_mT = Buf("mT")
        mt_ = [sb("mtmp%d" % i, [128, 512], F32) for i in range(4)]; B_mt = [Buf("mtmp%d" % i) for i in range(4)]
        tmp6 = sb("tmp6", [128, D], F32); B_tmp6 = Buf("tmp6")
        ss6 = sb("ss6", [128, 4], F32); B_ss6 = Buf("ss6")
        junk6 = sb("junk6", [128, 512], BF16); B_junk6 = Buf("junk6")
        x1st = sb("x1st", [128, 4, D], F32); B_x1st = Buf("x1st")
        srcs = [OaT, ObT, OcT]
        for G in range(NSB):
            tsl = slice(G * 512, (G + 1) * 512)
            for r in range(3):
                P.dma(OT[r][:], srcs[r][:, tsl].rearrange("(c p) t -> p c t", p=128), writes=[B_OT[r]], sem=B_OT[r].sem)
            P.dma(gts[:], gT[:, :, tsl].rearrange("c p t -> p c t"), writes=[B_gts], sem=B_gts.sem)
            P.dma(xo[:], xq[tsl, :].rearrange("(n p) d -> p n d", p=128), writes=[B_xo], sem=B_xo.sem)
            for oc in range(KC):
                pys = []
                for r in range(3):
                    pt, B_pt = next_ps()
                    for kc in range(4):
                        P.op("pe", R.matmul(
                            pt[:, :], lhsT=Wup[:, r * 4 + kc, oc * 128:(oc + 1) * 128], rhs=OT[r][:, kc, :],
                            start=(kc == 0), stop=(kc == 3)), reads=[B_Wup, B_OT[r]], acc=[B_pt], inc=(kc == 3))
                    pys.append((pt, B_pt))
                for r in range(3):
                    pt, B_pt = pys[r]
                    P.op("dve", R.tensor_tensor(
                        out=mt_[r][:], in0=pt[:, :], in1=gts[:, r * 8 + oc, :], op=ALU.mult),
                        reads=[B_pt, B_gts], writes=[B_mt[r]])
                P.op("pool", R.tensor_tensor(out=mt_[3][:], in0=mt_[0][:], in1=mt_[1][:], op=ALU.add),
                     reads=[B_mt[0], B_mt[1]], writes=[B_mt[3]])
                P.op("pool", R.tensor_tensor(out=mT[:, oc, :], in0=mt_[3][:], in1=mt_[2][:], op=ALU.add),
                     reads=[B_mt[3], B_mt[2]], acc=[B_mT])
            for j in range(4):
                pp = []
                for half in range(2):
                    pt, B_pt = next_ps()
                    for kc in range(KC):
                        P.op("pe", R.matmul(
                            pt[:, :], lhsT=mT[:, kc, j * 128:(j + 1) * 128], rhs=Wo[:, kc, half * 512:(half + 1) * 512],
                            start=(kc == 0), stop=(kc == KC - 1)), reads=[B_mT, B_Wo], acc=[B_pt], inc=(kc == KC - 1))
                    pp.append((pt, B_pt))
                post_norm(pp[0][0], pp[0][1], pp[1][0], pp[1][1], 0, xo[:, j, :], B_xo, x1st[:, j, :], B_x1st,
                          tmp6, B_tmp6, ss6, B_ss6, junk6, B_junk6)
            store(x1[tsl, :].rearrange("(n p) d -> p n d", p=128), x1st[:], B_x1st)
        P.barrier()
        ES.pop().close()

    if stop_after >= 7:
        ES.append(ExitStack())
        Wfi = sb("Wfi", [128, KC, 2 * DFF], BF16); B_Wfi = Buf("Wfi")
        Wfo = sb("Wfo", [128, FC, D], BF16); B_Wfo = Buf("Wfo")
        ES.append(ExitStack())
        stg = [sb("wstg7%d" % i, [128, 2048], F32) for i in range(2)]
        B_stg = [Buf("wstg7%d" % i) for i in range(2)]
        prep_weight(Wfi, B_Wfi, w_ffn_in, KC, [(0, 2 * DFF, 0, 1.0)], 2, stg, B_stg)
        prep_weight(Wfo, B_Wfo, w_ffn_out, FC, [(0, D, 0, 1.0)], None, stg, B_stg)
        P.barrier()
        ES.pop().close()
        fe = make_front("f", nbm=2)
        aT = sb("aT", [128, FC, 256], BF16); B_aT = Buf("aT")
        sg_ = [sb("sg%d" % i, [128, 256], F32) for i in range(2)]; B_sg = [Buf("sg%d" % i) for i in range(2)]
        tmp7 = sb("tmp7", [128, D], F32); B_tmp7 = Buf("tmp7")
        ss7 = sb("ss7", [128, 4], F32); B_ss7 = Buf("ss7")
        junk7 = sb("junk7", [128, 512], BF16); B_junk7 = Buf("junk7")
        ost = [sb("ost%d" % i, [128, 2, D], F32) for i in range(1)] * 2; B_ost = [Buf("ost%d" % i) for i in range(1)] * 2
        NG7 = SO // 256
        front_load(fe, 0, x1[0:256, :], nb=2)
        for g in range(NG7):
            b = g % 2
            if g + 1 < NG7:
                front_load(fe, g + 1, x1[(g + 1) * 256:(g + 2) * 256, :], nb=2)
            hT, B_hT = front(fe, g, None, nb=2, loaded=True)
            for fc in range(FC):
                pg, B_pg = proj_fm(Wfi, B_Wfi, hT, B_hT, fc * 128, 128, n=256)
                pu, B_pu = proj_fm(Wfi, B_Wfi, hT, B_hT, DFF + fc * 128, 128, n=256)
                k = fc % 2
                P.op("act", R.activation(out=sg_[k][:], in_=pg[:, 0:256], func=AF.Silu),
                     reads=[B_pg], writes=[B_sg[k]])
                P.op("dve", R.tensor_tensor(out=aT[:, fc, :], in0=sg_[k][:], in1=pu[:, 0:256],
                                                                         op=ALU.mult),
                     reads=[B_sg[k], B_pu], acc=[B_aT])
            for j in range(2):
                pp = []
                for half in range(2):
                    pt, B_pt = next_ps()
                    for fc in range(FC):
                        P.op("pe", R.matmul(
                            pt[:, :], lhsT=aT[:, fc, j * 128:(j + 1) * 128], rhs=Wfo[:, fc, half * 512:(half + 1) * 512],
                            start=(fc == 0), stop=(fc == FC - 1)), reads=[B_aT, B_Wfo], acc=[B_pt], inc=(fc == FC - 1))
                    pp.append((pt, B_pt))
                post_norm(pp[0][0], pp[0][1], pp[1][0], pp[1][1], 1, fe["xt"][b][:, j, :], fe["B_xt"][b],
                          ost[b][:, j, :], B_ost[b], tmp7, B_tmp7, ss7, B_ss7, junk7, B_junk7)
            store(out[g * 256:(g + 1) * 256, :].rearrange("(n p) d -> p n d", p=128), ost[b][:], B_ost[b])
        P.barrier()
        ES.pop().close()


    final = [(k, v) for k, v in P.cnt.items() if k not in P.ENG]
    P.run(nc, final_waits=final)
    while ES:
        ES.pop().close()
    return nc


def _rel_bucket(rel):
    nb = 16
    max_exact = 8
    n = np.abs(rel)
    large = max_exact + (np.log(np.maximum(n, 1).astype(np.float32) / max_exact)
                         / np.float32(np.log(128 / max_exact)) * (nb - max_exact)).astype(np.int32)
    large = np.minimum(large, nb - 1)
    return np.where(rel > 0, nb, 0) + np.where(n < max_exact, n, large)


def make_consts():
    c = np.zeros((128, 512), np.float32)
    c[:, 0:128] = np.eye(128)
    j = np.arange(128)[:, None]
    s = np.arange(128)[None, :]
    c[:, 128:256] = -(j >= s).astype(np.float32)
    c[:, 256:384] = 1.0
    c[:, 384:512] = -1.0
    return c.astype(ml_dtypes.bfloat16)


def core_consts(hf, rel_bias):
    p = np.arange(128)[:, None, None]
    j = np.arange(8)[None, :, None]
    t = np.arange(512)[None, None, :]
    krel = j * 128 + p
    qrel = hf * 512 + t
    maskc = (krel < qrel).astype(np.float32).astype(ml_dtypes.bfloat16)
    qp = np.arange(128)[:, None, None]
    qb = np.arange(4)[None, :, None]
    kr = np.arange(1024)[None, None, :]
    qpos = hf * 512 + qb * 128 + qp
    adm = (kr // 64) <= (qpos // 64)
    negadm = np.where(adm, 0.0, -1e30).astype(np.float32).astype(ml_dtypes.bfloat16)
    jj = np.arange(9)[None, :, None]
    rel = ((jj - 1) * 128 + p) - qrel
    bidx = _rel_bucket(rel)
    biasT = np.ascontiguousarray(np.transpose(rel_bias[bidx], (3, 0, 1, 2))).astype(np.float32)
    biasc = np.ascontiguousarray(np.broadcast_to(rel_bias[15][None, :], (128, 8))).astype(np.float32)
    return maskc, negadm, biasT, biasc


def make_in_maps(S, x, mem, rel_bias, g_mix_pre, w_in, b_gate, g_mem, w_mem_kv, w_up_a, w_up_b,
                 w_up_c, w_out, g_mix_post, g_ffn_pre, w_ffn_in, w_ffn_out, g_ffn_post):
    f = lambda a: np.ascontiguousarray(np.asarray(a, dtype=np.float32))
    x = f(x); mem = f(mem); rel_bias = f(rel_bias)
    B = x.shape[0]
    SO = S // 2
    NSB = SO // 512
    col = lambda g: f(g)[0].reshape(KC, 128).T
    gcols = np.ascontiguousarray(np.concatenate([col(g_mix_pre), col(g_mem), col(g_ffn_pre), col(g_ffn_pre)], axis=1))
    grows = np.ascontiguousarray(np.broadcast_to(np.stack([f(g_mix_post)[0], f(g_ffn_post)[0]])[None], (128, 2, D)))
    bg = np.ascontiguousarray(f(b_gate)[0].reshape(24, 128).T)
    w_up = np.ascontiguousarray(np.stack([f(w_up_a)[0], f(w_up_b)[0], f(w_up_c)[0]]))
    consts = make_consts()
    cc = [core_consts(hf, rel_bias) for hf in range(2)]
    shared = dict(w_in=f(w_in)[0], b_gate=bg, gcols=gcols, grows=grows, w_mem_kv=f(w_mem_kv)[0], w_up=w_up,
                  w_out=f(w_out)[0], w_ffn_in=f(w_ffn_in)[0], w_ffn_out=f(w_ffn_out)[0], consts=consts)
    in_maps = []
    for c in range(2 * B):
        b, hf = c // 2, c % 2
        xb = x[b]
        xq = np.ascontiguousarray(xb.reshape(NSB, 2, 512, D)[:, hf].reshape(SO, D))
        maskc, negadm, biasT, biasc = cc[hf]
        m = dict(shared)
        m.update(xf=xb, xq=xq, mem=mem[b], maskc=maskc, negadm=negadm, biasT=biasT, biasc=biasc)
        in_maps.append(m)
    return in_maps


_NC_CACHE = {}


def kernel(**inputs):
    x = np.asarray(inputs["x"])
    B, S, _ = x.shape
    SO = S // 2
    NSB = SO // 512
    if S not in _NC_CACHE:
        _NC_CACHE[S] = build(S)
    nc = _NC_CACHE[S]
    in_maps = make_in_maps(S, **inputs)
    res = run_bass_kernel_spmd(nc, in_maps, core_ids=list(range(2 * B)))
    outp = np.empty((B, S, D), np.float32)
    for c in range(2 * B):
        b, hf = c // 2, c % 2
        outp[b].reshape(NSB, 2, 512, D)[:, hf] = np.asarray(res.results[c]["out"]).reshape(NSB, 512, D)
    return outp
```

```python
import numpy as np
import ml_dtypes
from contextlib import ExitStack
import concourse.bass as bass
import concourse.mybir as mybir
from concourse.bass_utils import run_bass_kernel_spmd

F32 = mybir.dt.float32
BF16 = mybir.dt.bfloat16
AF = mybir.ActivationFunctionType
ALU = mybir.AluOpType
AX = mybir.AxisListType

D = 1024
KC = 8
NMEM = 256
DFF = 2816
FC = DFF // 128
EPS = 1e-6
KSEL = 256.0
NBIS = 16
O_AQ, O_AK, O_AV, O_IQ, O_IK, O_IW, O_BQ, O_BK, O_BV, O_CQ, O_G = (
    0, 512, 1024, 1536, 2048, 2112, 2120, 2632, 3144, 3656, 4168)


class Buf:
    __slots__ = ("name", "w", "r", "sem")

    def __init__(self, name):
        self.name = name
        self.w = {}
        self.r = {}
        self.sem = "d_" + name


class _Rec:
    def __getattr__(self, name):
        def mk(*a, **kw):
            return (name, a, kw)
        return mk


R = _Rec()


def _put(d, tok):
    if tok is not None and d.get(tok[0], 0) < tok[1]:
        d[tok[0]] = tok[1]


class Prog:
    ENG = ("pe", "act", "dve", "pool", "sp")

    def __init__(self):
        self.q = {e: [] for e in self.ENG}
        self.cnt = {}
        self.seen = {e: {} for e in self.ENG}
        self.dma_sems = []
        self.pending = {e: ([], [], []) for e in self.ENG}

    def _emit(self, eng, fn, deps, inc, dma_sem):
        waits = []
        for d in deps:
            if d is None:
                continue
            k, v = d
            if self.seen[eng].get(k, 0) >= v:
                continue
            self.seen[eng][k] = v
            waits.append((k, v))
        tok = None
        if dma_sem is not None:
            if dma_sem not in self.cnt:
                self.dma_sems.append(dma_sem)
            self.cnt[dma_sem] = self.cnt.get(dma_sem, 0) + 16
            tok = (dma_sem, self.cnt[dma_sem])
            self.q[eng].append((fn, waits, (dma_sem, 16)))
        elif inc:
            self.cnt[eng] = self.cnt.get(eng, 0) + 1
            tok = (eng, self.cnt[eng])
            self.q[eng].append((fn, waits, (eng, 1)))
        else:
            self.q[eng].append((fn, waits, None))
        return tok

    @staticmethod
    def _deps(reads, writes, acc, extra, eng=None):
        deps = list(extra)
        for b in reads:
            deps.extend(b.w.items())
        for b in writes:
            deps.extend(b.r.items())
            deps.extend(b.w.items())
        for b in acc:
            deps.extend(b.r.items())
            deps.extend((k, v) for k, v in b.w.items() if k != eng)
        return deps

    @staticmethod
    def _commit(tok, reads, writes, acc):
        for b in reads:
            _put(b.r, tok)
        for b in writes:
            b.w = {tok[0]: tok[1]}
            b.r = {}
        for b in acc:
            _put(b.w, tok)

    def op(self, eng, fn, reads=(), writes=(), inc=True, acc=(), extra=()):
        tok = self._emit(eng, fn, self._deps(reads, writes, acc, extra, eng), inc, None)
        pr, pw, pa = self.pending[eng]
        if tok is None:
            pr.extend(reads); pw.extend(writes); pa.extend(acc)
        else:
            self._commit(tok, list(reads) + pr, list(writes) + pw, list(acc) + pa)
            self.pending[eng] = ([], [], [])
        return tok

    def dma(self, out_ap, in_ap, reads=(), writes=(), acc=(), sem=None, eng="sp", extra=()):
        tok = self._emit(eng, ("dma_start", (), dict(out=out_ap, in_=in_ap)),
                         self._deps(reads, writes, acc, extra, eng), False, sem)
        self._commit(tok, reads, writes, acc)
        return tok

    def barrier(self):
        deps = list(self.cnt.items())
        for e in self.ENG:
            self._emit(e, None, deps, False, None)

    def run(self, nc, final_waits=()):
        with ExitStack() as es:
            sems = {}
            for k in list(self.ENG) + self.dma_sems:
                sems[k] = es.enter_context(nc.semaphore("s_" + k))
            block = es.enter_context(nc.Block())

            def replay(engname):
                def f(e):
                    for fn, waits, inc in self.q[engname]:
                        for k, v in waits:
                            e.wait_ge(sems[k], v)
                        if fn is None:
                            continue
                        ins = getattr(e, fn[0])(*fn[1], **fn[2])
                        if inc is not None:
                            ins.then_inc(sems[inc[0]], inc[1])
                    if engname == "sp":
                        for k, v in final_waits:
                            e.wait_ge(sems[k], v)
                return f

            block.sync(replay("sp"))
            block.tensor(replay("pe"))
            block.scalar(replay("act"))
            block.vector(replay("dve"))
            block.gpsimd(replay("pool"))


def build(S, stop_after=99, debug=False):
    SO = S // 2
    NSB = SO // 512
    NT = S // 128
    NGF = S // 512
    nc = bass.Bass("TRN2", target_bir_lowering=False)
    P = Prog()
    ES = [ExitStack()]

    def din(name, shape, dt=F32):
        return nc.dram_tensor(name, list(shape), dt, kind="ExternalInput").ap()

    dbg_kind = "ExternalOutput" if debug else "Internal"

    def dscr(name, shape, dt=BF16):
        return nc.dram_tensor(name, list(shape), dt, kind=dbg_kind).ap()

    xf = din("xf", [S, D])
    xq = din("xq", [SO, D])
    mem = din("mem", [NMEM, D])
    w_in = din("w_in", [D, 7240])
    b_gate = din("b_gate", [128, 24])
    gcols = din("gcols", [128, 4 * KC])
    grows = din("grows", [128, 2, D])
    w_mem_kv = din("w_mem_kv", [D, 1024])
    w_up = din("w_up", [3, 512, D])
    w_out = din("w_out", [D, D])
    w_ffn_in = din("w_ffn_in", [D, 2 * DFF])
    w_ffn_out = din("w_ffn_out", [DFF, D])
    biasT_in = din("biasT", [8, 128, 9, 512])
    biasc_in = din("biasc", [128, 8])
    maskc_in = din("maskc", [128, 8, 512], BF16)
    negadm_in = din("negadm", [128, 4, 1024], BF16)
    consts_in = din("consts", [128, 512], BF16)
    out = nc.dram_tensor("out", [SO, D], F32, kind="ExternalOutput").ap()

    akT = dscr("akT", [512, S]); bkT = dscr("bkT", [512, S]); ikT = dscr("ikT", [64, S])
    avh = dscr("avh", [8, 128, NT, 65]); bvh = dscr("bvh", [8, 128, NT, 64])
    aqT = dscr("aqT", [512, SO]); iqT = dscr("iqT", [512, SO]); bqT = dscr("bqT", [512, SO])
    gT = dscr("gT", [24, 128, SO])
    OaT = dscr("OaT", [512, SO]); ObT = dscr("ObT", [512, SO]); OcT = dscr("OcT", [512, SO])
    mskT = dscr("mskT", [NSB, 128, NT, 512], mybir.dt.uint8)
    biasTb = dscr("biasTb", [8, 128, 9, 512])
    x1 = dscr("x1", [SO, D], F32)

    sbc = [0]

    def sb(name, shape, dt):
        sbc[0] += 1
        return ES[-1].enter_context(nc.sbuf_tensor("%s_%d" % (name, sbc[0]), list(shape), dt))

    def ps(name, shape, dt=F32):
        return ES[-1].enter_context(nc.psum_tensor(name, list(shape), dt))

    def store(dst_ap, src_ap, srcbuf):
        return P.dma(dst_ap, src_ap, reads=[srcbuf], sem=srcbuf.sem)

    cst = sb("cst", [128, 512], BF16)
    B_cst = Buf("cst")
    ident = cst[:, 0:128]
    negtri = cst[:, 128:256]
    ones_b = cst[:, 256:384]
    negones = cst[:, 384:512]
    P.dma(cst[:], consts_in[:, :], writes=[B_cst], sem=B_cst.sem)
    gcol = sb("gcol", [128, 4 * KC], F32); B_gcol = Buf("gcol")
    P.dma(gcol[:], gcols[:, :], writes=[B_gcol], sem=B_gcol.sem)
    bgate = sb("bgate", [128, 24], F32); B_bgate = Buf("bgate")
    P.dma(bgate[:], b_gate[:, :], writes=[B_bgate], sem=B_bgate.sem)
    iwabs = sb("iwabs", [128, SO // 128, 8], F32); B_iwabs = Buf("iwabs")
    iwsgn = sb("iwsgn", [128, SO // 128, 8], F32); B_iwsgn = Buf("iwsgn")
    mkT = sb("mkT", [128, 4, NMEM], BF16); B_mkT = Buf("mkT")
    mvS = sb("mvS", [128, 2, 512], BF16); B_mvS = Buf("mvS")
    EPS_T = sb("eps_t", [128, 1], F32); B_eps = Buf("eps")
    P.op("pool", R.memset(EPS_T[:], EPS), writes=[B_eps])

    psb = [ps("psb%d" % i, [128, 512], F32) for i in range(6)]
    B_ps = [Buf("ps%d" % i) for i in range(6)]
    psT = [ps("psT%d" % i, [128, 1024], BF16) for i in range(2)]
    B_psT = [Buf("psT%d" % i) for i in range(2)]
    NPS = 6
    ps_rr = [0]

    def next_ps():
        i = ps_rr[0] % NPS
        ps_rr[0] += 1
        return psb[i], B_ps[i]

    evac_rr = [0]

    def evac(out_ap, in_ap, B_in, B_out, eng=None):
        if eng is None:
            eng = "act" if evac_rr[0] % 2 == 0 else "dve"
            evac_rr[0] += 1
        if eng == "act":
            return P.op("act", R.copy(out=out_ap, in_=in_ap), reads=[B_in], acc=[B_out])
        return P.op("dve", R.tensor_copy(out=out_ap, in_=in_ap), reads=[B_in], acc=[B_out])

    def prep_weight(dst, B_dst, src, nk, segs, gidx, stg, B_stg):
        i = 0
        for kc in range(nk):
            for (s0, n, d0, scale) in segs:
                for c in range(0, n, 2048):
                    m = min(2048, n - c)
                    k = i % 2
                    i += 1
                    P.dma(stg[k][:, 0:m], src[kc * 128:(kc + 1) * 128, s0 + c:s0 + c + m],
                          writes=[B_stg[k]], sem=B_stg[k].sem)
                    eng = "pool" if (i % 2) else "dve"
                    if gidx is None:
                        P.op(eng, R.tensor_scalar(
                            out=dst[:, kc, d0 + c:d0 + c + m], in0=stg[k][:, 0:m], scalar1=float(scale),
                            scalar2=1.0, op0=ALU.mult, op1=ALU.mult),
                            reads=[B_stg[k]], acc=[B_dst])
                    else:
                        P.op(eng, R.tensor_scalar(
                            out=dst[:, kc, d0 + c:d0 + c + m], in0=stg[k][:, 0:m],
                            scalar1=gcol[:, gidx * KC + kc:gidx * KC + kc + 1],
                            scalar2=float(scale), op0=ALU.mult, op1=ALU.mult),
                            reads=[B_stg[k], B_gcol], acc=[B_dst])

    def make_front(sfx, nbm=4):
        fe = {}
        fe["xt"] = [sb("xt%s%d" % (sfx, i), [128, nbm, D], F32) for i in range(2)]
        fe["B_xt"] = [Buf("xt%s%d" % (sfx, i)) for i in range(2)]
        fe["xn"] = sb("xn" + sfx, [128, nbm, D], BF16); fe["B_xn"] = Buf("xn" + sfx)
        fe["hT"] = [sb("hT%s%d" % (sfx, i), [128, KC, nbm * 128], BF16) for i in range(2)]
        fe["B_hT"] = [Buf("hT%s%d" % (sfx, i)) for i in range(2)]
        fe["ss"] = sb("ss" + sfx, [128, 8], F32); fe["B_ss"] = [Buf("ss%s%d" % (sfx, i)) for i in range(2)]
        fe["rs"] = sb("rs" + sfx, [128, 8], F32); fe["B_rs"] = [Buf("rs%s%d" % (sfx, i)) for i in range(2)]
        fe["junk"] = sb("junk" + sfx, [128, D], BF16); fe["B_junk"] = Buf("junk" + sfx)
        return fe

    def front_load(fe, g, src_rows, nb=4):
        b = g % 2
        xt, B_xt = fe["xt"][b], fe["B_xt"][b]
        P.dma(xt[:, 0:nb, :], src_rows.rearrange("(n p) d -> p n d", p=128), writes=[B_xt], sem=B_xt.sem)

    def front(fe, g, src_rows, nb=4, loaded=False):
        b = g % 2
        xt, B_xt = fe["xt"][b], fe["B_xt"][b]
        if not loaded:
            front_load(fe, g, src_rows, nb)
        ss, rs = fe["ss"], fe["rs"]
        for j in range(nb):
            P.op("act", R.activation(out=fe["junk"][:], in_=xt[:, j, :], func=AF.Square,
                                                    accum_out=ss[:, b * 4 + j:b * 4 + j + 1]),
                 reads=[B_xt], writes=[fe["B_junk"]], acc=[fe["B_ss"][b]])
        P.op("act", R.activation(out=rs[:, b * 4:b * 4 + nb], in_=ss[:, b * 4:b * 4 + nb], func=AF.Sqrt,
                                           bias=EPS_T[:, 0:1], scale=1.0 / D),
             reads=[fe["B_ss"][b], B_eps], writes=[fe["B_rs"][b]])
        P.op("dve", R.reciprocal(out=rs[:, b * 4:b * 4 + nb], in_=rs[:, b * 4:b * 4 + nb]),
             reads=[fe["B_rs"][b]], writes=[fe["B_rs"][b]])
        xn = fe["xn"]
        for j in range(nb):
            if j % 2 == 0:
                P.op("act", R.activation(out=xn[:, j, :], in_=xt[:, j, :], func=AF.Copy,
                                                        scale=rs[:, b * 4 + j:b * 4 + j + 1]),
                     reads=[B_xt, fe["B_rs"][b]], acc=[fe["B_xn"]])
            else:
                P.op("dve", R.tensor_scalar(out=xn[:, j, :], in0=xt[:, j, :],
                                                           scalar1=rs[:, b * 4 + j:b * 4 + j + 1], scalar2=None,
                                                           op0=ALU.mult),
                     reads=[B_xt, fe["B_rs"][b]], acc=[fe["B_xn"]])
        hT, B_hT = fe["hT"][b], fe["B_hT"][b]
        for kc in range(0, KC, 2):
            pb = (kc // 2) % 2
            for k2 in range(2):
                for j in range(nb):
                    last = (k2 == 1 and j == nb - 1)
                    P.op("pe", R.transpose(
                        out=psT[pb][:, k2 * 512 + j * 128:k2 * 512 + (j + 1) * 128],
                        in_=xn[:, j, (kc + k2) * 128:(kc + k2 + 1) * 128], identity=ident),
                        reads=[fe["B_xn"], B_cst], acc=[B_psT[pb]], inc=last)
            src = psT[pb][:, :].rearrange("p (k t) -> p k t", k=2)[:, :, 0:nb * 128]
            if (kc // 2) % 2 == 0:
                P.op("dve", R.tensor_copy(out=hT[:, kc:kc + 2, 0:nb * 128], in_=src),
                     reads=[B_psT[pb]], acc=[B_hT])
            else:
                P.op("act", R.copy(out=hT[:, kc:kc + 2, 0:nb * 128], in_=src),
                     reads=[B_psT[pb]], acc=[B_hT])
        return hT, B_hT

    def proj_fm(W, B_W, hT, B_hT, c0, m, n=512):
        pt, B_pt = next_ps()
        for kc in range(KC):
            P.op("pe", R.matmul(pt[0:m, 0:n], lhsT=W[:, kc, c0:c0 + m], rhs=hT[:, kc, 0:n],
                                                 start=(kc == 0), stop=(kc == KC - 1)),
                 reads=[B_W, B_hT], acc=[B_pt], inc=(kc == KC - 1))
        return pt, B_pt

    def proj_tm(W, B_W, hT, B_hT, j, c0, n):
        pt, B_pt = next_ps()
        for kc in range(KC):
            P.op("pe", R.matmul(pt[:, 0:n], lhsT=hT[:, kc, j * 128:(j + 1) * 128],
                                                 rhs=W[:, kc, c0:c0 + n], start=(kc == 0), stop=(kc == KC - 1)),
                 reads=[B_W, B_hT], acc=[B_pt], inc=(kc == KC - 1))
        return pt, B_pt

    if stop_after >= 1:
        ES.append(ExitStack())
        Wk = sb("Wk", [128, KC, 2112], BF16); B_Wk = Buf("Wk")
        stg = [sb("wstg%d" % i, [128, 2048], F32) for i in range(2)]
        B_stg = [Buf("wstg%d" % i) for i in range(2)]
        prep_weight(Wk, B_Wk, w_in, KC,
                    [(O_AK, 1024, 0, 1.0), (O_IK, 64, 1024, 1.0), (O_BK, 1024, 1088, 1.0)], 0, stg, B_stg)
        fe = make_front("a")
        fst = [sb("fst%d" % i, [128, 9, 512], BF16) for i in range(2)]
        B_fst = [Buf("fst%d" % i) for i in range(2)]
        avst = [sb("avst%d" % i, [128, 8, 4, 65], BF16) for i in range(2)]
        B_avst = [Buf("avst%d" % i) for i in range(2)]
        bvst = [sb("bvst%d" % i, [128, 8, 4, 64], BF16) for i in range(2)]
        B_bvst = [Buf("bvst%d" % i) for i in range(2)]
        for i in range(2):
            P.op("pool", R.memset(avst[i][:], 1.0), writes=[B_avst[i]])
        front_load(fe, 0, xf[0:512, :])
        for g in range(NGF):
            b = g % 2
            if g + 1 < NGF:
                front_load(fe, g + 1, xf[(g + 1) * 512:(g + 2) * 512, :])
            hT, B_hT = front(fe, g, None, loaded=True)
            for oc in range(9):
                if oc < 4:
                    c0, m = oc * 128, 128
                elif oc < 8:
                    c0, m = 1088 + (oc - 4) * 128, 128
                else:
                    c0, m = 1024, 64
                pt, B_pt = proj_fm(Wk, B_Wk, hT, B_hT, c0, m)
                evac(fst[b][0:m, oc, :], pt[0:m, :], B_pt, B_fst[b])
            store(akT[:, g * 512:(g + 1) * 512].rearrange("(c p) t -> p c t", p=128), fst[b][:, 0:4, :], B_fst[b])
            store(bkT[:, g * 512:(g + 1) * 512].rearrange("(c p) t -> p c t", p=128), fst[b][:, 4:8, :], B_fst[b])
            store(ikT[:, g * 512:(g + 1) * 512], fst[b][0:64, 8, :], B_fst[b])
            for j in range(4):
                pt, B_pt = proj_tm(Wk, B_Wk, hT, B_hT, j, 512, 512)
                evac(avst[b][:, :, j, 0:64], pt[:, :].rearrange("p (h d) -> p h d", h=8), B_pt, B_avst[b])
                pt, B_pt = proj_tm(Wk, B_Wk, hT, B_hT, j, 1600, 512)
                evac(bvst[b][:, :, j, :], pt[:, :].rearrange("p (h d) -> p h d", h=8), B_pt, B_bvst[b])
            for h in range(8):
                store(avh[h, :, g * 4:(g + 1) * 4, :], avst[b][:, h, :, :], B_avst[b])
                store(bvh[h, :, g * 4:(g + 1) * 4, :], bvst[b][:, h, :, :], B_bvst[b])
        P.barrier()
        ES.pop().close()

    if stop_after >= 2:
        ES.append(ExitStack())
        Wq = sb("Wq", [128, KC, 5128], BF16); B_Wq = Buf("Wq")
        Wm = sb("Wm", [128, KC, 1024], BF16); B_Wm = Buf("Wm")
        ES.append(ExitStack())
        stg = [sb("wstg%d" % i, [128, 2048], F32) for i in range(2)]
        B_stg = [Buf("wstgq%d" % i) for i in range(2)]
        prep_weight(Wm, B_Wm, w_mem_kv, KC, [(0, 1024, 0, 1.0)], 1, stg, B_stg)
        prep_weight(Wq, B_Wq, w_in, KC,
                    [(O_AQ, 512, 0, 0.125), (O_IQ, 512, 512, 1.0), (O_BQ, 512, 1024, 0.125),
                     (O_CQ, 512, 1536, 128 ** -0.5), (O_IW, 8, 2048, 1.0), (O_G, 3072, 2056, 1.0)], 0, stg, B_stg)
        P.barrier()
        ES.pop().close()
        fe = make_front("q")
        hT, B_hT = front(fe, 1, mem[:, :], nb=2)
        for h in range(4):
            pt, B_pt = proj_fm(Wm, B_Wm, hT, B_hT, h * 128, 128, n=NMEM)
            evac(mkT[:, h, :], pt[:, 0:NMEM], B_pt, B_mkT)
        for j in range(2):
            pt, B_pt = proj_tm(Wm, B_Wm, hT, B_hT, j, 512, 512)
            evac(mvS[:, j, :], pt[:, :], B_pt, B_mvS)
        qst = [sb("qst%d" % i, [128, 12, 512], BF16) for i in range(1)] * 2
        B_qst = [Buf("qst%d" % i) for i in range(1)] * 2
        cqs = [sb("cqs%d" % i, [128, 4, 512], BF16) for i in range(1)] * 2
        B_cqs = [Buf("cqs%d" % i) for i in range(1)] * 2
        gst = [sb("gst%d" % i, [128, 512], BF16) for i in range(4)]
        B_gst = [Buf("gst%d" % i) for i in range(4)]
        pm = [sb("pm%d" % i, [128, 512], BF16) for i in range(4)]
        B_pm = [Buf("pm%d" % i) for i in range(4)]
        rD = [sb("rD%d" % i, [128, 512], F32) for i in range(2)]
        B_rD = [Buf("rD%d" % i) for i in range(2)]
        ocst = [sb("ocst%d" % i, [128, 4, 512], BF16) for i in range(2)]
        B_ocst = [Buf("ocst%d" % i) for i in range(2)]
        gi = 0
        pmi = 0
        front_load(fe, 0, xq[0:512, :])
        for g in range(NSB):
            b = g % 2
            tsl = slice(g * 512, (g + 1) * 512)
            if g + 1 < NSB:
                front_load(fe, g + 1, xq[(g + 1) * 512:(g + 2) * 512, :])
            hT, B_hT = front(fe, g, None, loaded=True)
            for oc in range(12):
                pt, B_pt = proj_fm(Wq, B_Wq, hT, B_hT, oc * 128, 128)
                evac(qst[b][:, oc, :], pt[:, :], B_pt, B_qst[b])
            store(aqT[:, tsl].rearrange("(c p) t -> p c t", p=128), qst[b][:, 0:4, :], B_qst[b])
            store(iqT[:, tsl].rearrange("(c p) t -> p c t", p=128), qst[b][:, 4:8, :], B_qst[b])
            store(bqT[:, tsl].rearrange("(c p) t -> p c t", p=128), qst[b][:, 8:12, :], B_qst[b])
            for h in range(4):
                pt, B_pt = proj_fm(Wq, B_Wq, hT, B_hT, 1536 + h * 128, 128)
                evac(cqs[b][:, h, :], pt[:, :], B_pt, B_cqs[b])
            for j in range(4):
                pt, B_pt = proj_tm(Wq, B_Wq, hT, B_hT, j, 2048, 8)
                P.op("dve", R.tensor_scalar(
                    out=iwsgn[:, g * 4 + j, :], in0=pt[:, 0:8], scalar1=0.0, scalar2=0.5,
                    op0=ALU.is_ge, op1=ALU.subtract),
                    reads=[B_pt], acc=[B_iwsgn])
                P.op("dve", R.scalar_tensor_tensor(
                    out=iwabs[:, g * 4 + j, :], in0=pt[:, 0:8], scalar=2.0, in1=iwsgn[:, g * 4 + j, :],
                    op0=ALU.mult, op1=ALU.mult),
                    reads=[B_pt, B_iwsgn], acc=[B_iwabs])
            for h in range(4):
                pms = []
                for mt in range(2):
                    pz, B_pz = next_ps()
                    P.op("pe", R.matmul(
                        pz[:, :], lhsT=mkT[:, h, mt * 128:(mt + 1) * 128], rhs=cqs[b][:, h, :],
                        start=True, stop=True), reads=[B_mkT, B_cqs[b]], writes=[B_pz])
                    k = pmi % 4
                    pmi += 1
                    P.op("act", R.activation(out=pm[k][:], in_=pz[:, :], func=AF.Exp),
                         reads=[B_pz], writes=[B_pm[k]])
                    pms.append(k)
                po, B_po = next_ps()
                pd, B_pd = next_ps()
                for mt in range(2):
                    k = pms[mt]
                    P.op("pe", R.matmul(
                        po[:, :], lhsT=mvS[:, mt, h * 128:(h + 1) * 128], rhs=pm[k][:],
                        start=(mt == 0), stop=(mt == 1)), reads=[B_mvS, B_pm[k]], acc=[B_po], inc=(mt == 1))
                for mt in range(2):
                    k = pms[mt]
                    P.op("pe", R.matmul(
                        pd[:, :], lhsT=ones_b, rhs=pm[k][:], start=(mt == 0), stop=(mt == 1)),
                        reads=[B_cst, B_pm[k]], acc=[B_pd], inc=(mt == 1))
                r = h % 2
                P.op("dve", R.reciprocal(out=rD[r][:], in_=pd[:, :]),
                     reads=[B_pd], writes=[B_rD[r]])
                P.op("dve", R.tensor_tensor(
                    out=ocst[b][:, h, :], in0=po[:, :], in1=rD[r][:], op=ALU.mult),
                    reads=[B_po, B_rD[r]], acc=[B_ocst[b]])
            store(OcT[:, tsl].rearrange("(c p) t -> p c t", p=128), ocst[b][:, :, :], B_ocst[b])
            for oc in range(24):
                pt, B_pt = proj_fm(Wq, B_Wq, hT, B_hT, 2056 + oc * 128, 128)
                k = gi % 4
                gi += 1
                P.op("act", R.activation(
                    out=gst[k][:], in_=pt[:, :], func=AF.Sigmoid, bias=bgate[:, oc:oc + 1]),
                    reads=[B_pt, B_bgate], writes=[B_gst[k]])
                store(gT[oc, :, tsl], gst[k][:], B_gst[k])
        P.barrier()
        ES.pop().close()

    ONE_T = sb("one_t", [128, 1], F32); B_one = Buf("one")
    P.op("pool", R.memset(ONE_T[:], 1.0), writes=[B_one])
    if stop_after >= 3:
        ES.append(ExitStack())
        kTs = [sb("kTs%d" % i, [128, S], BF16) for i in range(2)]; B_kTs = [Buf("kTs%d" % i) for i in range(2)]
        Vs = [sb("Vs%d" % i, [128, NT, 64], BF16) for i in range(2)]; B_Vs = [Buf("Vs%d" % i) for i in range(2)]
        qs = [sb("qs%d" % i, [128, 512], BF16) for i in range(2)]; B_qs = [Buf("qs%d" % i) for i in range(2)]
        for i in range(2):
            P.op("pool", R.memset(kTs[i][64:128, :], 0.0), acc=[B_kTs[i]])
            P.op("pool", R.memset(qs[i][64:128, :], 0.0), acc=[B_qs[i]])
        maskc = sb("maskc", [128, 8, 512], BF16); B_maskc = Buf("maskc")
        P.dma(maskc[:], maskc_in[:, :, :], writes=[B_maskc], sem=B_maskc.sem)
        e_sb = [sb("e_sb%d" % i, [128, 512], F32) for i in range(2)]; B_e = [Buf("e%d" % i) for i in range(2)]
        sp_sb = [sb("sp_sb%d" % i, [128, 512], BF16) for i in range(4)]; B_sp = [Buf("sp%d" % i) for i in range(4)]
        spacc = [sb("spacc%d" % i, [128, 512], BF16) for i in range(2)]; B_spacc = [Buf("spacc%d" % i) for i in range(2)]
        a_sb = [sb("a_sb%d" % i, [128, 512], BF16) for i in range(3)]; B_a = [Buf("a%d" % i) for i in range(3)]
        obst = [sb("obst%d" % i, [64, 512], BF16) for i in range(2)]; B_obst = [Buf("obst%d" % i) for i in range(2)]
        items = [(G, h) for G in range(NSB) for h in range(8)]

        def sb_load(idx):
            G, h = items[idx]
            kb = idx % 2
            KL = (G + 1) * 1024
            P.dma(kTs[kb][0:64, 0:KL], bkT[h * 64:(h + 1) * 64, 0:KL], acc=[B_kTs[kb]], sem=B_kTs[kb].sem)
            P.dma(Vs[kb][:, 0:KL // 128, :], bvh[h, :, 0:KL // 128, :], writes=[B_Vs[kb]], sem=B_Vs[kb].sem)
            P.dma(qs[kb][0:64, :], bqT[h * 64:(h + 1) * 64, G * 512:(G + 1) * 512], acc=[B_qs[kb]], sem=B_qs[kb].sem)

        cz = [0]; csp = [0]; cc = [0]; ca = [0]
        sb_load(0)
        for idx, (G, h) in enumerate(items):
            kb = idx % 2
            if idx + 1 < len(items):
                sb_load(idx + 1)
            n = (G + 1) * 8
            psO, B_psO = psb[4 + kb], B_ps[4 + kb]
            st = {}
            st2 = {}

            def stage1(i):
                kt = n - 1 - i
                j = kt - 8 * G
                zi = cz[0] % 2; cz[0] += 1
                si = csp[0] % 4; csp[0] += 1
                st[i] = (zi, si)
                P.op("pe", R.matmul(psb[zi][:, :], lhsT=kTs[kb][:, kt * 128:(kt + 1) * 128], rhs=qs[kb][:],
                                    start=True, stop=True),
                     reads=[B_kTs[kb], B_qs[kb]], writes=[B_ps[zi]])
                P.op("act", R.activation(out=e_sb[zi % 2][:], in_=psb[zi][:, :], func=AF.Exp),
                     reads=[B_ps[zi]], writes=[B_e[zi % 2]])
                P.op("act", R.activation(out=sp_sb[si][:], in_=e_sb[zi % 2][:], func=AF.Ln, bias=ONE_T[:, 0:1]),
                     reads=[B_e[zi % 2], B_one], writes=[B_sp[si]])
                if j >= 0:
                    P.op("dve", R.tensor_tensor(out=sp_sb[si][:], in0=sp_sb[si][:], in1=maskc[:, j, :], op=ALU.mult),
                         reads=[B_sp[si], B_maskc], writes=[B_sp[si]])

            accprev = [None, None]

            def stage2(i):
                kt = n - 1 - i
                j = kt - 8 * G
                zi, si = st.pop(i)
                ci = 2 + (cc[0] % 2); cc[0] += 1
                ai = ca[0] % 3; ca[0] += 1
                st2[i] = ai
                P.op("pe", R.matmul(psb[ci][:, :], lhsT=kTs[kb][:, kt * 128:(kt + 1) * 128], rhs=qs[kb][:],
                                    start=True, stop=False),
                     reads=[B_kTs[kb], B_qs[kb]], writes=[B_ps[ci]], inc=False)
                P.op("pe", R.matmul(psb[ci][:, :], lhsT=negtri, rhs=sp_sb[si][:], start=False, stop=(i == 0)),
                     reads=[B_cst, B_sp[si]], acc=[B_ps[ci]], inc=(i == 0))
                if i > 0:
                    ap_prev, B_prev = accprev
                    P.op("pe", R.matmul(psb[ci][:, :], lhsT=negones, rhs=ap_prev, start=False, stop=True),
                         reads=[B_cst, B_prev], acc=[B_ps[ci]])
                if i < n - 1:
                    if i == 0:
                        accprev[0], accprev[1] = sp_sb[si][:], B_sp[si]
                    else:
                        ap_prev, B_prev = accprev
                        k = i % 2
                        P.op("pool", R.tensor_tensor(out=spacc[k][:], in0=ap_prev, in1=sp_sb[si][:], op=ALU.add),
                             reads=[B_prev, B_sp[si]], writes=[B_spacc[k]])
                        accprev[0], accprev[1] = spacc[k][:], B_spacc[k]
                P.op("act", R.activation(out=a_sb[ai][:], in_=psb[ci][:, :], func=AF.Exp),
                     reads=[B_ps[ci]], writes=[B_a[ai]])
                if j >= 0:
                    P.op("dve", R.tensor_tensor(out=a_sb[ai][:], in0=a_sb[ai][:], in1=maskc[:, j, :], op=ALU.mult),
                         reads=[B_a[ai], B_maskc], writes=[B_a[ai]])

            def stage3(i):
                kt = n - 1 - i
                ai = st2.pop(i)
                if i == 0:
                    P.op("pe", R.matmul(psO[0:64, :], lhsT=Vs[kb][:, kt, :], rhs=a_sb[ai][:],
                                        start=True, stop=(n == 1)),
                         reads=[B_Vs[kb], B_a[ai]], writes=[B_psO])
                else:
                    P.op("pe", R.matmul(psO[0:64, :], lhsT=Vs[kb][:, kt, :], rhs=a_sb[ai][:],
                                        start=False, stop=(i == n - 1)),
                         reads=[B_Vs[kb], B_a[ai]], acc=[B_psO])

            for step in range(n + 2):
                if step < n:
                    stage1(step)
                if 0 <= step - 1 < n:
                    stage2(step - 1)
                if 0 <= step - 2 < n:
                    stage3(step - 2)
            evac(obst[kb][:], psO[0:64, :], B_psO, B_obst[kb])
            store(ObT[h * 64:(h + 1) * 64, G * 512:(G + 1) * 512], obst[kb][:], B_obst[kb])
        P.barrier()
        ES.pop().close()

    if stop_after >= 4:
        ES.append(ExitStack())
        ikTs = sb("ikTs", [128, S], BF16); B_ikTs = Buf("ikTs")
        P.op("pool", R.memset(ikTs[64:128, :], 0.0), acc=[B_ikTs])
        P.dma(ikTs[0:64, :], ikT[:, :], acc=[B_ikTs], sem=B_ikTs.sem)
        negadm = sb("negadm", [128, 4, 1024], BF16); B_negadm = Buf("negadm")
        P.dma(negadm[:], negadm_in[:, :, :], writes=[B_negadm], sem=B_negadm.sem)
        iqs = [sb("iqs%d" % i, [128, 8, 512], BF16) for i in range(2)]; B_iqs = [Buf("iqs%d" % i) for i in range(2)]
        for i in range(2):
            P.op("pool", R.memset(iqs[i][64:128, :, :], 0.0), acc=[B_iqs[i]])
        sc = [sb("sc%d" % i, [128, S], F32) for i in range(2)]
        B_scc = [[Buf("sc%d_%d" % (i, c)) for c in range(S // 512)] for i in range(2)]
        B_sc = [Buf("scw%d" % i) for i in range(2)]
        r_sb = [sb("r_sb%d" % i, [128, 512], BF16) for i in range(4)]; B_r = [Buf("r%d" % i) for i in range(4)]
        dgs = [sb("dgs%d" % i, [128, 8, 128], BF16) for i in range(2)]; B_dgs = [Buf("dgs%d" % i) for i in range(2)]
        szi = [0]
        selt = [sb("selt%d" % i, [128, S], BF16) for i in range(2)]; B_sel = [Buf("sel%d" % i) for i in range(2)]
        junk4 = sb("junk4", [128, S], BF16); B_junk4 = Buf("junk4")
        junk5 = sb("junk5", [128, S], BF16); B_junk5 = Buf("junk5")
        sm = [sb("sm%d" % i, [128, 8], F32) for i in range(2)]
        B_sm = [[Buf("sm%d_%d" % (i, k)) for k in range(8)] for i in range(2)]
        mst = [sb("mst%d" % i, [128, 8, 128], mybir.dt.uint8) for i in range(3)]; B_mst = [Buf("mst%d" % i) for i in range(3)]
        ri = 0
        msi = 0
        pti = 0
        P.dma(iqs[0][0:64, :, :], iqT.rearrange("(h d) t -> d h t", d=64)[:, :, 0:512], acc=[B_iqs[0]], sem=B_iqs[0].sem)
        for G in range(NSB):
            gb = G % 2
            if G + 1 < NSB:
                P.dma(iqs[1 - gb][0:64, :, :], iqT.rearrange("(h d) t -> d h t", d=64)[:, :, (G + 1) * 512:(G + 2) * 512],
                      acc=[B_iqs[1 - gb]], sem=B_iqs[1 - gb].sem)
            KL = (G + 1) * 1024
            nch = KL // 512
            for qp in range(2):
                chains = []
                for qi in range(2):
                    qb = 2 * qp + qi
                    blk = G * 4 + qb
                    cb = qi
                    scb = sc[cb]
                    smb, B_smb = sm[cb], B_sm[cb]
                    for ih in range(8):
                        P.op("pool", R.tensor_scalar(out=dgs[cb][:, ih, :], in0=ident, scalar1=iwabs[:, blk, ih:ih + 1],
                                                     scalar2=iwsgn[:, blk, ih:ih + 1], op0=ALU.mult, op1=ALU.mult),
                             reads=[B_cst, B_iwabs, B_iwsgn], acc=[B_dgs[cb]],
                             extra=(list(B_dgs[cb].r.items()) if ih == 0 else []))
                    its = [(c, ih) for c in range(nch) for ih in range(8)]
                    pend = []
                    for n_it in range(len(its) + 2):
                        if n_it < len(its):
                            c, ih = its[n_it]
                            z = szi[0] % 4; szi[0] += 1
                            P.op("pe", R.matmul(psb[z][:, :], lhsT=iqs[gb][:, ih, qb * 128:(qb + 1) * 128],
                                                rhs=ikTs[:, c * 512:(c + 1) * 512], start=True, stop=True),
                                 reads=[B_iqs[gb], B_ikTs], writes=[B_ps[z]])
                            k = ri % 4
                            ri += 1
                            P.op("act", R.activation(out=r_sb[k][:], in_=psb[z][:, :], func=AF.Relu),
                                 reads=[B_ps[z]], writes=[B_r[k]])
                            pend.append((c, ih, k))
                        if n_it >= 2:
                            c, ih, k = pend.pop(0)
                            ab = 4 + (c % 2)
                            if ih == 0:
                                P.op("pe", R.matmul(psb[ab][:, :], lhsT=dgs[cb][:, ih, :], rhs=r_sb[k][:],
                                                    start=True, stop=False),
                                     reads=[B_dgs[cb], B_r[k]], writes=[B_ps[ab]], inc=True)
                            else:
                                P.op("pe", R.matmul(psb[ab][:, :], lhsT=dgs[cb][:, ih, :], rhs=r_sb[k][:],
                                                    start=False, stop=(ih == 7)),
                                     reads=[B_dgs[cb], B_r[k]], acc=[B_ps[ab]], inc=True)
                            if ih == 7:
                                dst = scb[:, c * 512:(c + 1) * 512]
                                if c % 2 == 0:
                                    P.op("dve", R.tensor_copy(out=dst, in_=psb[ab][:, :]),
                                         reads=[B_ps[ab]], writes=[B_scc[cb][c]], extra=list(B_sc[cb].r.items()))
                                else:
                                    P.op("act", R.copy(out=dst, in_=psb[ab][:, :]),
                                         reads=[B_ps[ab]], writes=[B_scc[cb][c]], extra=list(B_sc[cb].r.items()))
                    chunks = [B_scc[cb][c] for c in range(nch)]
                    P.op("dve", R.tensor_reduce(out=smb[:, 0:1], in_=scb[:, 0:KL], axis=AX.X, op=ALU.max),
                         reads=chunks, writes=[B_smb[0]])
                    P.op("dve", R.tensor_reduce(out=smb[:, 1:2], in_=scb[:, 0:KL], axis=AX.X, op=ALU.min),
                         reads=chunks, writes=[B_smb[1]])
                    P.op("dve", R.tensor_tensor(out=scb[:, KL - 1024:KL], in0=scb[:, KL - 1024:KL],
                                                in1=negadm[:, qb, :], op=ALU.add),
                         reads=chunks + [B_negadm], writes=[B_sc[cb]] + chunks[-2:])
                    P.op("dve", R.tensor_tensor(out=smb[:, 2:3], in0=smb[:, 0:1], in1=smb[:, 1:2], op=ALU.subtract),
                         reads=[B_smb[0], B_smb[1]], writes=[B_smb[2]])
                    P.op("dve", R.tensor_scalar(out=smb[:, 3:4], in0=smb[:, 2:3], scalar1=1.0 + 2.0 ** -9,
                                                scalar2=2e-20, op0=ALU.mult, op1=ALU.add),
                         reads=[B_smb[2]], writes=[B_smb[3]])
                    P.op("dve", R.scalar_tensor_tensor(out=smb[:, 4:5], in0=smb[:, 2:3], scalar=-(2.0 ** -10),
                                                       in1=smb[:, 1:2], op0=ALU.mult, op1=ALU.add),
                         reads=[B_smb[2], B_smb[1]], writes=[B_smb[4]])
                    chains.append((qb, cb, scb, smb, B_smb))
                for k in range(NBIS):
                    hk = 2.0 ** -(k + 1)
                    for (qb, cb, scb, smb, B_smb) in chains:
                        if cb == 0:
                            P.op("dve", R.scalar_tensor_tensor(out=smb[:, 5:6], in0=smb[:, 3:4], scalar=hk,
                                                               in1=smb[:, 4:5], op0=ALU.mult, op1=ALU.add),
                                 reads=[B_smb[3], B_smb[4]], writes=[B_smb[5]])
                        else:
                            P.op("dve", R.scalar_tensor_tensor(out=smb[:, 5:6], in0=smb[:, 3:4], scalar=-hk,
                                                               in1=smb[:, 4:5], op0=ALU.mult, op1=ALU.subtract),
                                 reads=[B_smb[3], B_smb[4]], writes=[B_smb[5]])
                    for (qb, cb, scb, smb, B_smb) in reversed(chains):
                        if cb == 0:
                            P.op("dve", R.tensor_scalar(out=junk4[:, 0:KL], in0=scb[:, 0:KL], scalar1=smb[:, 5:6],
                                                        scalar2=None, op0=ALU.is_ge, op1=ALU.add,
                                                        accum_out=smb[:, 6:7]),
                                 reads=[B_sc[cb], B_smb[5]], writes=[B_junk4, B_smb[6]])
                        else:
                            P.op("act", R.activation(out=junk5[:, 0:KL], in_=scb[:, 0:KL], func=AF.Sign,
                                                     bias=smb[:, 5:6], accum_out=smb[:, 6:7]),
                                 reads=[B_sc[cb], B_smb[5]], writes=[B_junk5, B_smb[6]])
                    for (qb, cb, scb, smb, B_smb) in chains:
                        thr = (KSEL - 0.5) if cb == 0 else (2.0 * KSEL - 1.0 - KL)
                        P.op("dve", R.tensor_scalar(out=smb[:, 7:8], in0=smb[:, 6:7], scalar1=thr,
                                                    scalar2=hk, op0=ALU.is_ge, op1=ALU.mult),
                             reads=[B_smb[6]], writes=[B_smb[7]])
                        P.op("dve", R.scalar_tensor_tensor(out=smb[:, 4:5], in0=smb[:, 7:8], scalar=smb[:, 3:4],
                                                           in1=smb[:, 4:5], op0=ALU.mult, op1=ALU.add),
                             reads=[B_smb[7], B_smb[3], B_smb[4]], writes=[B_smb[4]])
                for (qb, cb, scb, smb, B_smb) in chains:
                    P.op("dve", R.tensor_scalar(out=selt[cb][:, 0:KL], in0=scb[:, 0:KL], scalar1=smb[:, 4:5],
                                                scalar2=None, op0=ALU.is_ge),
                         reads=[B_sc[cb], B_smb[4]], writes=[B_sel[cb]])
                    for k0 in range(0, KL // 128, 8):
                        pb = pti % 2
                        pti += 1
                        for kk in range(8):
                            kt = k0 + kk
                            P.op("pe", R.transpose(
                                out=psT[pb][:, kk * 128:(kk + 1) * 128], in_=selt[cb][:, kt * 128:(kt + 1) * 128],
                                identity=ident), reads=[B_sel[cb], B_cst], acc=[B_psT[pb]], inc=(kk == 7))
                        m = msi % 3
                        msi += 1
                        P.op("act", R.copy(out=mst[m][:], in_=psT[pb][:, :].rearrange(
                            "p (k t) -> p k t", k=8)), reads=[B_psT[pb]], writes=[B_mst[m]])
                        store(mskT[G, :, k0:k0 + 8, qb * 128:(qb + 1) * 128], mst[m][:], B_mst[m])
        P.barrier()
        ES.pop().close()

    if stop_after >= 5:
        ES.append(ExitStack())
        kTs = [sb("akTs%d" % i, [128, S], BF16) for i in range(2)]; B_kTs = [Buf("akTs%d" % i) for i in range(2)]
        Vs = [sb("aVs%d" % i, [128, NT, 65], BF16) for i in range(2)]; B_Vs = [Buf("aVs%d" % i) for i in range(2)]
        qs = [sb("aqs%d" % i, [128, 512], BF16) for i in range(2)]; B_qs = [Buf("aqs%d" % i) for i in range(2)]
        for i in range(2):
            P.op("pool", R.memset(kTs[i][64:128, :], 0.0), acc=[B_kTs[i]])
            P.op("pool", R.memset(qs[i][64:128, :], 0.0), acc=[B_qs[i]])
        bsb_all = sb("bsb_all", [128, 8, 9, 512], BF16); B_bsball = Buf("bsb_all")
        biasc = sb("biasc", [128, 8], F32); B_biasc = Buf("biasc")
        P.dma(biasc[:], biasc_in[:, :], writes=[B_biasc], sem=B_biasc.sem)
        mk_sb = sb("mk_sb", [128, NT, 512], mybir.dt.uint8); B_mk = [Buf("mk%d" % i) for i in range(NT // 8)]
        p_sb = [sb("p_sb%d" % i, [128, 512], BF16) for i in range(4)]; B_p = [Buf("p%d" % i) for i in range(4)]
        pm_sb = [sb("pm_sb%d" % i, [128, 512], BF16) for i in range(6)]; B_pmm = [Buf("pmm%d" % i) for i in range(6)]
        Osb = [sb("Osb%d" % i, [65, 512], F32) for i in range(2)]; B_Osb = [Buf("Osb%d" % i) for i in range(2)]
        oast = [sb("oast%d" % i, [64, 512], BF16) for i in range(2)]; B_oast = [Buf("oast%d" % i) for i in range(2)]
        sel65 = sb("sel65", [65, 64], F32); B_sel65 = Buf("sel65")
        P.op("pool", R.memset(sel65[:], 0.0), writes=[B_sel65])
        P.op("pool", R.memset(sel65[64:65, :], 1.0), reads=[B_sel65], writes=[B_sel65])
        items = [(G, h) for G in range(NSB) for h in range(8)]

        def dsa_load(idx):
            G, h = items[idx]
            kb = idx % 2
            KL = (G + 1) * 1024
            P.dma(kTs[kb][0:64, 0:KL], akT[h * 64:(h + 1) * 64, 0:KL], acc=[B_kTs[kb]], sem=B_kTs[kb].sem)
            P.dma(Vs[kb][:, 0:KL // 128, :], avh[h, :, 0:KL // 128, :], writes=[B_Vs[kb]], sem=B_Vs[kb].sem)
            P.dma(qs[kb][0:64, :], aqT[h * 64:(h + 1) * 64, G * 512:(G + 1) * 512], acc=[B_qs[kb]], sem=B_qs[kb].sem)

        ES.append(ExitStack())
        bsf = [sb("bsf%d" % i, [128, 9, 512], F32) for i in range(1)]; B_bsf = [Buf("bsf%d" % i) for i in range(1)]
        for hh in range(8):
            P.dma(bsf[0][:], biasT_in[hh, :, :, :], writes=[B_bsf[0]], sem=B_bsf[0].sem)
            if hh % 2 == 0:
                P.op("act", R.copy(out=bsb_all[:, hh, :, :], in_=bsf[0][:]), reads=[B_bsf[0]], acc=[B_bsball])
            else:
                P.op("dve", R.tensor_copy(out=bsb_all[:, hh, :, :], in_=bsf[0][:]), reads=[B_bsf[0]], acc=[B_bsball])
        P.barrier()
        ES.pop().close()
        pi = 0
        zi = 0
        dsa_load(0)
        for idx, (G, h) in enumerate(items):
            kb = idx % 2
            if h == 0:
                for sg in range(G + 1):
                    P.dma(mk_sb[:, sg * 8:(sg + 1) * 8, :], mskT[G, :, sg * 8:(sg + 1) * 8, :],
                          writes=[B_mk[sg]], sem=B_mk[sg].sem)
            if idx + 1 < len(items):
                dsa_load(idx + 1)
            n = (G + 1) * 8
            psO, B_psO = psb[4 + kb], B_ps[4 + kb]
            LA = 4
            pmk = {}
            for step in range(n + LA):
                if step < n:
                    kt = step
                    j = kt - 8 * G
                    near = j >= -1
                    z = zi % 4
                    zi += 1
                    k = pi % 4
                    km = pi % 6
                    pi += 1
                    pmk[kt] = km
                    P.op("pe", R.matmul(
                        psb[z][:, :], lhsT=kTs[kb][:, kt * 128:(kt + 1) * 128], rhs=qs[kb][:], start=True, stop=not near),
                        reads=[B_kTs[kb], B_qs[kb]], writes=[B_ps[z]], inc=not near)
                    if near:
                        P.op("pe", R.matmul(psb[z][:, :], lhsT=ident, rhs=bsb_all[:, h, j + 1, :], start=False, stop=True),
                             reads=[B_cst, B_bsball], acc=[B_ps[z]])
                        P.op("act", R.activation(out=p_sb[k][:], in_=psb[z][:, :], func=AF.Exp),
                             reads=[B_ps[z]], writes=[B_p[k]])
                    else:
                        P.op("act", R.activation(out=p_sb[k][:], in_=psb[z][:, :], func=AF.Exp, bias=biasc[:, h:h + 1]),
                             reads=[B_ps[z], B_biasc], writes=[B_p[k]])
                    P.op("dve" if (kt % 3) != 2 else "pool",
                         R.tensor_tensor(out=pm_sb[km][:], in0=p_sb[k][:], in1=mk_sb[:, kt, :], op=ALU.mult),
                         reads=[B_p[k], B_mk[kt // 8]], writes=[B_pmm[km]])
                kt = step - LA
                if kt >= 0:
                    km = pmk.pop(kt)
                    if kt == 0:
                        P.op("pe", R.matmul(psO[0:65, :], lhsT=Vs[kb][:, kt, :], rhs=pm_sb[km][:],
                                            start=True, stop=(n == 1)),
                             reads=[B_Vs[kb], B_pmm[km]], writes=[B_psO])
                    else:
                        P.op("pe", R.matmul(psO[0:65, :], lhsT=Vs[kb][:, kt, :], rhs=pm_sb[km][:],
                                            start=False, stop=(kt == n - 1)),
                             reads=[B_Vs[kb], B_pmm[km]], acc=[B_psO])
            P.op("act", R.copy(out=Osb[kb][:], in_=psO[0:65, :]), reads=[B_psO], writes=[B_Osb[kb]])
            P.op("dve", R.reciprocal(out=Osb[kb][64:65, :], in_=Osb[kb][64:65, :]),
                 reads=[B_Osb[kb]], writes=[B_Osb[kb]])
            pt, B_pt = psb[zi % 4], B_ps[zi % 4]
            zi += 1
            P.op("pe", R.matmul(pt[0:64, :], lhsT=sel65[:, :], rhs=Osb[kb][:, :], start=True, stop=True),
                 reads=[B_sel65, B_Osb[kb]], writes=[B_pt])
            P.op("dve", R.tensor_tensor(out=oast[kb][:], in0=Osb[kb][0:64, :], in1=pt[0:64, :],
                                                         op=ALU.mult),
                 reads=[B_Osb[kb], B_pt], writes=[B_oast[kb]])
            store(OaT[h * 64:(h + 1) * 64, G * 512:(G + 1) * 512], oast[kb][:], B_oast[kb])
        P.barrier()
        ES.pop().close()

    gpost = sb("gpost", [128, 2, D], F32); B_gpost = Buf("gpost")
    P.dma(gpost[:], grows[:, :, :], writes=[B_gpost], sem=B_gpost.sem)

    def post_norm(psA, B_psA, psB_, B_psB, gi, res_ap, B_res, out_ap, B_out, tmp, B_tmp, ss2, B_ss2, junk, B_junk):
        P.op("act", R.activation(out=junk[:, 0:512], in_=psA[:, :], func=AF.Square, accum_out=ss2[:, 0:1]),
             reads=[B_psA], writes=[B_junk], acc=[B_ss2])
        P.op("act", R.activation(out=junk[:, 0:512], in_=psB_[:, :], func=AF.Square, accum_out=ss2[:, 1:2]),
             reads=[B_psB], writes=[B_junk], acc=[B_ss2])
        P.op("dve", R.tensor_tensor(out=ss2[:, 2:3], in0=ss2[:, 0:1], in1=ss2[:, 1:2], op=ALU.add),
             reads=[B_ss2], writes=[B_ss2])
        P.op("act", R.activation(out=ss2[:, 3:4], in_=ss2[:, 2:3], func=AF.Sqrt, bias=EPS_T[:, 0:1],
                                           scale=1.0 / D), reads=[B_ss2, B_eps], writes=[B_ss2])
        P.op("dve", R.reciprocal(out=ss2[:, 3:4], in_=ss2[:, 3:4]), reads=[B_ss2], writes=[B_ss2])
        for half, (pp, B_pp) in enumerate(((psA, B_psA), (psB_, B_psB))):
            hs = slice(half * 512, (half + 1) * 512)
            P.op("dve", R.scalar_tensor_tensor(
                out=tmp[:, hs], in0=pp[:, :], scalar=ss2[:, 3:4], in1=gpost[:, gi, hs], op0=ALU.mult, op1=ALU.mult),
                reads=[B_pp, B_ss2, B_gpost], acc=[B_tmp])
        P.op("pool", R.tensor_tensor(out=out_ap, in0=tmp[:, :], in1=res_ap, op=ALU.add),
             reads=[B_tmp, B_res], acc=[B_out])

    if stop_after >= 6:
        ES.append(ExitStack())
        Wup = sb("Wup", [128, 12, D], BF16); B_Wup = Buf("Wup")
        Wo = sb("Wo", [128, KC, D], BF16); B_Wo = Buf("Wo")
        stg = [sb("wstg6%d" % i, [128, 2048], F32) for i in range(2)]
        B_stg = [Buf("wstg6%d" % i) for i in range(2)]
        prep_weight(Wup, B_Wup, w_up.rearrange("r k n -> (r k) n"), 12, [(0, 1024, 0, 1.0)], None, stg, B_stg)
        prep_weight(Wo, B_Wo, w_out, KC, [(0, 1024, 0, 1.0)], None, stg, B_stg)
        OT = [sb("OT%d" % r, [128, 4, 512], BF16) for r in range(3)]; B_OT = [Buf("OT%d" % r) for r in range(3)]
        gts = sb("gts", [128, 24, 512], BF16); B_gts = Buf("gts")
        xo = sb("xo", [128, 4, D], F32); B_xo = Buf("xo")
        mT = sb("mT", [128, KC, 512], BF16); B_mT = Buf("mT")
        mt_ = [sb("mtmp%d" % i, [128, 512], F32) for i in range(4)]; B_mt = [Buf("mtmp%d" % i) for i in range(4)]
        tmp6 = sb("tmp6", [128, D], F32); B_tmp6 = Buf("tmp6")
        ss6 = sb("ss6", [128, 4], F32); B_ss6 = Buf("ss6")
        junk6 = sb("junk6", [128, 512], BF16); B_junk6 = Buf("junk6")
        x1st = sb("x1st", [128, 4, D], F32); B_x1st = Buf("x1st")
        srcs = [OaT, ObT, OcT]
        for G in range(NSB):
            tsl = slice(G * 512, (G + 1) * 512)
            for r in range(3):
                P.dma(OT[r][:], srcs[r][:, tsl].rearrange("(c p) t -> p c t", p=128), writes=[B_OT[r]], sem=B_OT[r].sem)
            P.dma(gts[:], gT[:, :, tsl].rearrange("c p t -> p c t"), writes=[B_gts], sem=B_gts.sem)
            P.dma(xo[:], xq[tsl, :].rearrange("(n p) d -> p n d", p=128), writes=[B_xo], sem=B_xo.sem)
            for oc in range(KC):
                pys = []
                for r in range(3):
                    pt, B_pt = next_ps()
                    for kc in range(4):
                        P.op("pe", R.matmul(
                            pt[:, :], lhsT=Wup[:, r * 4 + kc, oc * 128:(oc + 1) * 128], rhs=OT[r][:, kc, :],
                            start=(kc == 0), stop=(kc == 3)), reads=[B_Wup, B_OT[r]], acc=[B_pt], inc=(kc == 3))
                    pys.append((pt, B_pt))
                for r in range(3):
                    pt, B_pt = pys[r]
                    P.op("dve", R.tensor_tensor(
                        out=mt_[r][:], in0=pt[:, :], in1=gts[:, r * 8 + oc, :], op=ALU.mult),
                        reads=[B_pt, B_gts], writes=[B_mt[r]])
                P.op("pool", R.tensor_tensor(out=mt_[3][:], in0=mt_[0][:], in1=mt_[1][:], op=ALU.add),
                     reads=[B_mt[0], B_mt[1]], writes=[B_mt[3]])
                P.op("pool", R.tensor_tensor(out=mT[:, oc, :], in0=mt_[3][:], in1=mt_[2][:], op=ALU.add),
                     reads=[B_mt[3], B_mt[2]], acc=[B_mT])
            for j in range(4):
                pp = []
                for half in range(2):
                    pt, B_pt = next_ps()
                    for kc in range(KC):
                        P.op("pe", R.matmul(
                            pt[:, :], lhsT=mT[:, kc, j * 128:(j + 1) * 128], rhs=Wo[:, kc, half * 512:(half + 1) * 512],
                            start=(kc == 0), stop=(kc == KC - 1)), reads=[B_mT, B_Wo], acc=[B_pt], inc=(kc == KC - 1))
                    pp.append((pt, B_pt))
                post_norm(pp[0][0], pp[0][1], pp[1][0], pp[1][1], 0, xo[:, j, :], B_xo, x1st[:, j, :], B_x1st,
                          tmp6, B_tmp6, ss6, B_ss6, junk6, B_junk6)
            store(x1[tsl, :].rearrange("(n p) d -> p n d", p=128), x1st[:], B_x1st)
        P.barrier()
        ES.pop().close()

    if stop_after >= 7:
        ES.append(ExitStack())
        Wfi = sb("Wfi", [128, KC, 2 * DFF], BF16); B_Wfi = Buf("Wfi")
        Wfo = sb("Wfo", [128, FC, D], BF16); B_Wfo = Buf("Wfo")
        ES.append(ExitStack())
        stg = [sb("wstg7%d" % i, [128, 2048], F32) for i in range(2)]
        B_stg = [Buf("wstg7%d" % i) for i in range(2)]
        prep_weight(Wfi, B_Wfi, w_ffn_in, KC, [(0, 2 * DFF, 0, 1.0)], 2, stg, B_stg)
        prep_weight(Wfo, B_Wfo, w_ffn_out, FC, [(0, D, 0, 1.0)], None, stg, B_stg)
        P.barrier()
        ES.pop().close()
        fe = make_front("f", nbm=2)
        aT = sb("aT", [128, FC, 256], BF16); B_aT = Buf("aT")
        sg_ = [sb("sg%d" % i, [128, 256], F32) for i in range(2)]; B_sg = [Buf("sg%d" % i) for i in range(2)]
        tmp7 = sb("tmp7", [128, D], F32); B_tmp7 = Buf("tmp7")
        ss7 = sb("ss7", [128, 4], F32); B_ss7 = Buf("ss7")
        junk7 = sb("junk7", [128, 512], BF16); B_junk7 = Buf("junk7")
        ost = [sb("ost%d" % i, [128, 2, D], F32) for i in range(1)] * 2; B_ost = [Buf("ost%d" % i) for i in range(1)] * 2
        NG7 = SO // 256
        front_load(fe, 0, x1[0:256, :], nb=2)
        for g in range(NG7):
            b = g % 2
            if g + 1 < NG7:
                front_load(fe, g + 1, x1[(g + 1) * 256:(g + 2) * 256, :], nb=2)
            hT, B_hT = front(fe, g, None, nb=2, loaded=True)
            for fc in range(FC):
                pg, B_pg = proj_fm(Wfi, B_Wfi, hT, B_hT, fc * 128, 128, n=256)
                pu, B_pu = proj_fm(Wfi, B_Wfi, hT, B_hT, DFF + fc * 128, 128, n=256)
                k = fc % 2
                P.op("act", R.activation(out=sg_[k][:], in_=pg[:, 0:256], func=AF.Silu),
                     reads=[B_pg], writes=[B_sg[k]])
                P.op("dve", R.tensor_tensor(out=aT[:, fc, :], in0=sg_[k][:], in1=pu[:, 0:256],
                                                                         op=ALU.mult),
                     reads=[B_sg[k], B_pu], acc=[B_aT])
            for j in range(2):
                pp = []
                for half in range(2):
                    pt, B_pt = next_ps()
                    for fc in range(FC):
                        P.op("pe", R.matmul(
                            pt[:, :], lhsT=aT[:, fc, j * 128:(j + 1) * 128], rhs=Wfo[:, fc, half * 512:(half + 1) * 512],
                            start=(fc == 0), stop=(fc == FC - 1)), reads=[B_aT, B_Wfo], acc=[B_pt], inc=(fc == FC - 1))
                    pp.append((pt, B_pt))
                post_norm(pp[0][0], pp[0][1], pp[1][0], pp[1][1], 1, fe["xt"][b][:, j, :], fe["B_xt"][b],
                          ost[b][:, j, :], B_ost[b], tmp7, B_tmp7, ss7, B_ss7, junk7, B_junk7)
            store(out[g * 256:(g + 1) * 256, :].rearrange("(n p) d -> p n d", p=128), ost[b][:], B_ost[b])
        P.barrier()
        ES.pop().close()


    final = [(k, v) for k, v in P.cnt.items() if k not in P.ENG]
    P.run(nc, final_waits=final)
    while ES:
        ES.pop().close()
    return nc


def _rel_bucket(rel):
    nb = 16
    max_exact = 8
    n = np.abs(rel)
    large = max_exact + (np.log(np.maximum(n, 1).astype(np.float32) / max_exact)
                         / np.float32(np.log(128 / max_exact)) * (nb - max_exact)).astype(np.int32)
    large = np.minimum(large, nb - 1)
    return np.where(rel > 0, nb, 0) + np.where(n < max_exact, n, large)


def make_consts():
    c = np.zeros((128, 512), np.float32)
    c[:, 0:128] = np.eye(128)
    j = np.arange(128)[:, None]
    s = np.arange(128)[None, :]
    c[:, 128:256] = -(j >= s).astype(np.float32)
    c[:, 256:384] = 1.0
    c[:, 384:512] = -1.0
    return c.astype(ml_dtypes.bfloat16)


def core_consts(hf, rel_bias):
    p = np.arange(128)[:, None, None]
    j = np.arange(8)[None, :, None]
    t = np.arange(512)[None, None, :]
    krel = j * 128 + p
    qrel = hf * 512 + t
    maskc = (krel < qrel).astype(np.float32).astype(ml_dtypes.bfloat16)
    qp = np.arange(128)[:, None, None]
    qb = np.arange(4)[None, :, None]
    kr = np.arange(1024)[None, None, :]
    qpos = hf * 512 + qb * 128 + qp
    adm = (kr // 64) <= (qpos // 64)
    negadm = np.where(adm, 0.0, -1e30).astype(np.float32).astype(ml_dtypes.bfloat16)
    jj = np.arange(9)[None, :, None]
    rel = ((jj - 1) * 128 + p) - qrel
    bidx = _rel_bucket(rel)
    biasT = np.ascontiguousarray(np.transpose(rel_bias[bidx], (3, 0, 1, 2))).astype(np.float32)
    biasc = np.ascontiguousarray(np.broadcast_to(rel_bias[15][None, :], (128, 8))).astype(np.float32)
    return maskc, negadm, biasT, biasc


def make_in_maps(S, x, mem, rel_bias, g_mix_pre, w_in, b_gate, g_mem, w_mem_kv, w_up_a, w_up_b,
                 w_up_c, w_out, g_mix_post, g_ffn_pre, w_ffn_in, w_ffn_out, g_ffn_post):
    f = lambda a: np.ascontiguousarray(np.asarray(a, dtype=np.float32))
    x = f(x); mem = f(mem); rel_bias = f(rel_bias)
    B = x.shape[0]
    SO = S // 2
    NSB = SO // 512
    col = lambda g: f(g)[0].reshape(KC, 128).T
    gcols = np.ascontiguousarray(np.concatenate([col(g_mix_pre), col(g_mem), col(g_ffn_pre), col(g_ffn_pre)], axis=1))
    grows = np.ascontiguousarray(np.broadcast_to(np.stack([f(g_mix_post)[0], f(g_ffn_post)[0]])[None], (128, 2, D)))
    bg = np.ascontiguousarray(f(b_gate)[0].reshape(24, 128).T)
    w_up = np.ascontiguousarray(np.stack([f(w_up_a)[0], f(w_up_b)[0], f(w_up_c)[0]]))
    consts = make_consts()
    cc = [core_consts(hf, rel_bias) for hf in range(2)]
    shared = dict(w_in=f(w_in)[0], b_gate=bg, gcols=gcols, grows=grows, w_mem_kv=f(w_mem_kv)[0], w_up=w_up,
                  w_out=f(w_out)[0], w_ffn_in=f(w_ffn_in)[0], w_ffn_out=f(w_ffn_out)[0], consts=consts)
    in_maps = []
    for c in range(2 * B):
        b, hf = c // 2, c % 2
        xb = x[b]
        xq = np.ascontiguousarray(xb.reshape(NSB, 2, 512, D)[:, hf].reshape(SO, D))
        maskc, negadm, biasT, biasc = cc[hf]
        m = dict(shared)
        m.update(xf=xb, xq=xq, mem=mem[b], maskc=maskc, negadm=negadm, biasT=biasT, biasc=biasc)
        in_maps.append(m)
    return in_maps


_NC_CACHE = {}


def kernel(**inputs):
    x = np.asarray(inputs["x"])
    B, S, _ = x.shape
    SO = S // 2
    NSB = SO // 512
    if S not in _NC_CACHE:
        _NC_CACHE[S] = build(S)
    nc = _NC_CACHE[S]
    in_maps = make_in_maps(S, **inputs)
    res = run_bass_kernel_spmd(nc, in_maps, core_ids=list(range(2 * B)))
    outp = np.empty((B, S, D), np.float32)
    for c in range(2 * B):
        b, hf = c // 2, c % 2
        outp[b].reshape(NSB, 2, 512, D)[:, hf] = np.asarray(res.results[c]["out"]).reshape(NSB, 512, D)
    return outp
```

```python
import numpy as np
import ml_dtypes
from contextlib import ExitStack
import concourse.bass as bass
import concourse.mybir as mybir
from concourse.bass_utils import run_bass_kernel_spmd

F32 = mybir.dt.float32
BF16 = mybir.dt.bfloat16
AF = mybir.ActivationFunctionType
ALU = mybir.AluOpType
AX = mybir.AxisListType

D = 1024
KC = 8
NMEM = 256
DFF = 2816
FC = DFF // 128
EPS = 1e-6
KSEL = 256.0
NBIS = 16
O_AQ, O_AK, O_AV, O_IQ, O_IK, O_IW, O_BQ, O_BK, O_BV, O_CQ, O_G = (
    0, 512, 1024, 1536, 2048, 2112, 2120, 2632, 3144, 3656, 4168)


class Buf:
    __slots__ = ("name", "w", "r", "sem")

    def __init__(self, name):
        self.name = name
        self.w = {}
        self.r = {}
        self.sem = "d_" + name


class _Rec:
    def __getattr__(self, name):
        def mk(*a, **kw):
            return (name, a, kw)
        return mk


R = _Rec()


def _put(d, tok):
    if tok is not None and d.get(tok[0], 0) < tok[1]:
        d[tok[0]] = tok[1]


class Prog:
    ENG = ("pe", "act", "dve", "pool", "sp")

    def __init__(self):
        self.q = {e: [] for e in self.ENG}
        self.cnt = {}
        self.seen = {e: {} for e in self.ENG}
        self.dma_sems = []
        self.pending = {e: ([], [], []) for e in self.ENG}

    def _emit(self, eng, fn, deps, inc, dma_sem):
        waits = []
        for d in deps:
            if d is None:
                continue
            k, v = d
            if self.seen[eng].get(k, 0) >= v:
                continue
            self.seen[eng][k] = v
            waits.append((k, v))
        tok = None
        if dma_sem is not None:
            if dma_sem not in self.cnt:
                self.dma_sems.append(dma_sem)
            self.cnt[dma_sem] = self.cnt.get(dma_sem, 0) + 16
            tok = (dma_sem, self.cnt[dma_sem])
            self.q[eng].append((fn, waits, (dma_sem, 16)))
        elif inc:
            self.cnt[eng] = self.cnt.get(eng, 0) + 1
            tok = (eng, self.cnt[eng])
            self.q[eng].append((fn, waits, (eng, 1)))
        else:
            self.q[eng].append((fn, waits, None))
        return tok

    @staticmethod
    def _deps(reads, writes, acc, extra, eng=None):
        deps = list(extra)
        for b in reads:
            deps.extend(b.w.items())
        for b in writes:
            deps.extend(b.r.items())
            deps.extend(b.w.items())
        for b in acc:
            deps.extend(b.r.items())
            deps.extend((k, v) for k, v in b.w.items() if k != eng)
        return deps

    @staticmethod
    def _commit(tok, reads, writes, acc):
        for b in reads:
            _put(b.r, tok)
        for b in writes:
            b.w = {tok[0]: tok[1]}
            b.r = {}
        for b in acc:
            _put(b.w, tok)

    def op(self, eng, fn, reads=(), writes=(), inc=True, acc=(), extra=()):
        tok = self._emit(eng, fn, self._deps(reads, writes, acc, extra, eng), inc, None)
        pr, pw, pa = self.pending[eng]
        if tok is None:
            pr.extend(reads); pw.extend(writes); pa.extend(acc)
        else:
            self._commit(tok, list(reads) + pr, list(writes) + pw, list(acc) + pa)
            self.pending[eng] = ([], [], [])
        return tok

    def dma(self, out_ap, in_ap, reads=(), writes=(), acc=(), sem=None, eng="sp", extra=()):
        tok = self._emit(eng, ("dma_start", (), dict(out=out_ap, in_=in_ap)),
                         self._deps(reads, writes, acc, extra, eng), False, sem)
        self._commit(tok, reads, writes, acc)
        return tok

    def barrier(self):
        deps = list(self.cnt.items())
        for e in self.ENG:
            self._emit(e, None, deps, False, None)

    def run(self, nc, final_waits=()):
        with ExitStack() as es:
            sems = {}
            for k in list(self.ENG) + self.dma_sems:
                sems[k] = es.enter_context(nc.semaphore("s_" + k))
            block = es.enter_context(nc.Block())

            def replay(engname):
                def f(e):
                    for fn, waits, inc in self.q[engname]:
                        for k, v in waits:
                            e.wait_ge(sems[k], v)
                        if fn is None:
                            continue
                        ins = getattr(e, fn[0])(*fn[1], **fn[2])
                        if inc is not None:
                            ins.then_inc(sems[inc[0]], inc[1])
                    if engname == "sp":
                        for k, v in final_waits:
                            e.wait_ge(sems[k], v)
                return f

            block.sync(replay("sp"))
            block.tensor(replay("pe"))
            block.scalar(replay("act"))
            block.vector(replay("dve"))
            block.gpsimd(replay("pool"))


def build(S, stop_after=99, debug=False):
    SO = S // 2
    NSB = SO // 512
    NT = S // 128
    NGF = S // 512
    nc = bass.Bass("TRN2", target_bir_lowering=False)
    P = Prog()
    ES = [ExitStack()]

    def din(name, shape, dt=F32):
        return nc.dram_tensor(name, list(shape), dt, kind="ExternalInput").ap()

    dbg_kind = "ExternalOutput" if debug else "Internal"

    def dscr(name, shape, dt=BF16):
        return nc.dram_tensor(name, list(shape), dt, kind=dbg_kind).ap()

    xf = din("xf", [S, D])
    xq = din("xq", [SO, D])
    mem = din("mem", [NMEM, D])
    w_in = din("w_in", [D, 7240])
    b_gate = din("b_gate", [128, 24])
    gcols = din("gcols", [128, 4 * KC])
    grows = din("grows", [128, 2, D])
    w_mem_kv = din("w_mem_kv", [D, 1024])
    w_up = din("w_up", [3, 512, D])
    w_out = din("w_out", [D, D])
    w_ffn_in = din("w_ffn_in", [D, 2 * DFF])
    w_ffn_out = din("w_ffn_out", [DFF, D])
    biasT_in = din("biasT", [8, 128, 9, 512])
    biasc_in = din("biasc", [128, 8])
    maskc_in = din("maskc", [128, 8, 512], BF16)
    negadm_in = din("negadm", [128, 4, 1024], BF16)
    consts_in = din("consts", [128, 512], BF16)
    out = nc.dram_tensor("out", [SO, D], F32, kind="ExternalOutput").ap()

    akT = dscr("akT", [512, S]); bkT = dscr("bkT", [512, S]); ikT = dscr("ikT", [64, S])
    avh = dscr("avh", [8, 128, NT, 65]); bvh = dscr("bvh", [8, 128, NT, 64])
    aqT = dscr("aqT", [512, SO]); iqT = dscr("iqT", [512, SO]); bqT = dscr("bqT", [512, SO])
    gT = dscr("gT", [24, 128, SO])
    OaT = dscr("OaT", [512, SO]); ObT = dscr("ObT", [512, SO]); OcT = dscr("OcT", [512, SO])
    mskT = dscr("mskT", [NSB, 128, NT, 512], mybir.dt.uint8)
    biasTb = dscr("biasTb", [8, 128, 9, 512])
    x1 = dscr("x1", [SO, D], F32)

    sbc = [0]

    def sb(name, shape, dt):
        sbc[0] += 1
        return ES[-1].enter_context(nc.sbuf_tensor("%s_%d" % (name, sbc[0]), list(shape), dt))

    def ps(name, shape, dt=F32):
        return ES[-1].enter_context(nc.psum_tensor(name, list(shape), dt))

    def store(dst_ap, src_ap, srcbuf):
        return P.dma(dst_ap, src_ap, reads=[srcbuf], sem=srcbuf.sem)

    cst = sb("cst", [128, 512], BF16)
    B_cst = Buf("cst")
    ident = cst[:, 0:128]
    negtri = cst[:, 128:256]
    ones_b = cst[:, 256:384]
    negones = cst[:, 384:512]
    P.dma(cst[:], consts_in[:, :], writes=[B_cst], sem=B_cst.sem)
    gcol = sb("gcol", [128, 4 * KC], F32); B_gcol = Buf("gcol")
    P.dma(gcol[:], gcols[:, :], writes=[B_gcol], sem=B_gcol.sem)
    bgate = sb("bgate", [128, 24], F32); B_bgate = Buf("bgate")
    P.dma(bgate[:], b_gate[:, :], writes=[B_bgate], sem=B_bgate.sem)
    iwabs = sb("iwabs", [128, SO // 128, 8], F32); B_iwabs = Buf("iwabs")
    iwsgn = sb("iwsgn", [128, SO // 128, 8], F32); B_iwsgn = Buf("iwsgn")
    mkT = sb("mkT", [128, 4, NMEM], BF16); B_mkT = Buf("mkT")
    mvS = sb("mvS", [128, 2, 512], BF16); B_mvS = Buf("mvS")
    EPS_T = sb("eps_t", [128, 1], F32); B_eps = Buf("eps")
    P.op("pool", R.memset(EPS_T[:], EPS), writes=[B_eps])

    psb = [ps("psb%d" % i, [128, 512], F32) for i in range(6)]
    B_ps = [Buf("ps%d" % i) for i in range(6)]
    psT = [ps("psT%d" % i, [128, 1024], BF16) for i in range(2)]
    B_psT = [Buf("psT%d" % i) for i in range(2)]
    NPS = 6
    ps_rr = [0]

    def next_ps():
        i = ps_rr[0] % NPS
        ps_rr[0] += 1
        return psb[i], B_ps[i]

    evac_rr = [0]

    def evac(out_ap, in_ap, B_in, B_out, eng=None):
        if eng is None:
            eng = "act" if evac_rr[0] % 2 == 0 else "dve"
            evac_rr[0] += 1
        if eng == "act":
            return P.op("act", R.copy(out=out_ap, in_=in_ap), reads=[B_in], acc=[B_out])
        return P.op("dve", R.tensor_copy(out=out_ap, in_=in_ap), reads=[B_in], acc=[B_out])

    def prep_weight(dst, B_dst, src, nk, segs, gidx, stg, B_stg):
        i = 0
        for kc in range(nk):
            for (s0, n, d0, scale) in segs:
                for c in range(0, n, 2048):
                    m = min(2048, n - c)
                    k = i % 2
                    i += 1
                    P.dma(stg[k][:, 0:m], src[kc * 128:(kc + 1) * 128, s0 + c:s0 + c + m],
                          writes=[B_stg[k]], sem=B_stg[k].sem)
                    eng = "dve"
                    if gidx is None:
                        P.op(eng, R.tensor_scalar(
                            out=dst[:, kc, d0 + c:d0 + c + m], in0=stg[k][:, 0:m], scalar1=float(scale),
                            scalar2=1.0, op0=ALU.mult, op1=ALU.mult),
                            reads=[B_stg[k]], acc=[B_dst])
                    else:
                        P.op(eng, R.tensor_scalar(
                            out=dst[:, kc, d0 + c:d0 + c + m], in0=stg[k][:, 0:m],
                            scalar1=gcol[:, gidx * KC + kc:gidx * KC + kc + 1],
                            scalar2=float(scale), op0=ALU.mult, op1=ALU.mult),
                            reads=[B_stg[k], B_gcol], acc=[B_dst])

    def make_front(sfx, nbm=4):
        fe = {}
        fe["xt"] = [sb("xt%s%d" % (sfx, i), [128, nbm, D], F32) for i in range(2)]
        fe["B_xt"] = [Buf("xt%s%d" % (sfx, i)) for i in range(2)]
        fe["xn"] = sb("xn" + sfx, [128, nbm, D], BF16); fe["B_xn"] = Buf("xn" + sfx)
        fe["hT"] = [sb("hT%s%d" % (sfx, i), [128, KC, nbm * 128], BF16) for i in range(2)]
        fe["B_hT"] = [Buf("hT%s%d" % (sfx, i)) for i in range(2)]
        fe["ss"] = sb("ss" + sfx, [128, 8], F32); fe["B_ss"] = [Buf("ss%s%d" % (sfx, i)) for i in range(2)]
        fe["rs"] = sb("rs" + sfx, [128, 8], F32); fe["B_rs"] = [Buf("rs%s%d" % (sfx, i)) for i in range(2)]
        fe["junk"] = sb("junk" + sfx, [128, D], BF16); fe["B_junk"] = Buf("junk" + sfx)
        return fe

    def front_load(fe, g, src_rows, nb=4):
        b = g % 2
        xt, B_xt = fe["xt"][b], fe["B_xt"][b]
        P.dma(xt[:, 0:nb, :], src_rows.rearrange("(n p) d -> p n d", p=128), writes=[B_xt], sem=B_xt.sem)

    def front(fe, g, src_rows, nb=4, loaded=False):
        b = g % 2
        xt, B_xt = fe["xt"][b], fe["B_xt"][b]
        if not loaded:
            front_load(fe, g, src_rows, nb)
        ss, rs = fe["ss"], fe["rs"]
        for j in range(nb):
            P.op("act", R.activation(out=fe["junk"][:], in_=xt[:, j, :], func=AF.Square,
                                                    accum_out=ss[:, b * 4 + j:b * 4 + j + 1]),
                 reads=[B_xt], writes=[fe["B_junk"]], acc=[fe["B_ss"][b]])
        P.op("act", R.activation(out=rs[:, b * 4:b * 4 + nb], in_=ss[:, b * 4:b * 4 + nb], func=AF.Sqrt,
                                           bias=EPS_T[:, 0:1], scale=1.0 / D),
             reads=[fe["B_ss"][b], B_eps], writes=[fe["B_rs"][b]])
        P.op("dve", R.reciprocal(out=rs[:, b * 4:b * 4 + nb], in_=rs[:, b * 4:b * 4 + nb]),
             reads=[fe["B_rs"][b]], writes=[fe["B_rs"][b]])
        xn = fe["xn"]
        for j in range(nb):
            if j % 2 == 0:
                P.op("act", R.activation(out=xn[:, j, :], in_=xt[:, j, :], func=AF.Copy,
                                                        scale=rs[:, b * 4 + j:b * 4 + j + 1]),
                     reads=[B_xt, fe["B_rs"][b]], acc=[fe["B_xn"]])
            else:
                P.op("dve", R.tensor_scalar(out=xn[:, j, :], in0=xt[:, j, :],
                                                           scalar1=rs[:, b * 4 + j:b * 4 + j + 1], scalar2=None,
                                                           op0=ALU.mult),
                     reads=[B_xt, fe["B_rs"][b]], acc=[fe["B_xn"]])
        hT, B_hT = fe["hT"][b], fe["B_hT"][b]
        for kc in range(0, KC, 2):
            pb = (kc // 2) % 2
            for k2 in range(2):
                for j in range(nb):
                    last = (k2 == 1 and j == nb - 1)
                    P.op("pe", R.transpose(
                        out=psT[pb][:, k2 * 512 + j * 128:k2 * 512 + (j + 1) * 128],
                        in_=xn[:, j, (kc + k2) * 128:(kc + k2 + 1) * 128], identity=ident),
                        reads=[fe["B_xn"], B_cst], acc=[B_psT[pb]], inc=last)
            src = psT[pb][:, :].rearrange("p (k t) -> p k t", k=2)[:, :, 0:nb * 128]
            if (kc // 2) % 2 == 0:
                P.op("dve", R.tensor_copy(out=hT[:, kc:kc + 2, 0:nb * 128], in_=src),
                     reads=[B_psT[pb]], acc=[B_hT])
            else:
                P.op("act", R.copy(out=hT[:, kc:kc + 2, 0:nb * 128], in_=src),
                     reads=[B_psT[pb]], acc=[B_hT])
        return hT, B_hT

    def proj_fm(W, B_W, hT, B_hT, c0, m, n=512):
        pt, B_pt = next_ps()
        for kc in range(KC):
            P.op("pe", R.matmul(pt[0:m, 0:n], lhsT=W[:, kc, c0:c0 + m], rhs=hT[:, kc, 0:n],
                                                 start=(kc == 0), stop=(kc == KC - 1)),
                 reads=[B_W, B_hT], acc=[B_pt], inc=(kc == KC - 1))
        return pt, B_pt

    def proj_tm(W, B_W, hT, B_hT, j, c0, n):
        pt, B_pt = next_ps()
        for kc in range(KC):
            P.op("pe", R.matmul(pt[:, 0:n], lhsT=hT[:, kc, j * 128:(j + 1) * 128],
                                                 rhs=W[:, kc, c0:c0 + n], start=(kc == 0), stop=(kc == KC - 1)),
                 reads=[B_W, B_hT], acc=[B_pt], inc=(kc == KC - 1))
        return pt, B_pt

    if stop_after >= 1:
        ES.append(ExitStack())
        Wk = sb("Wk", [128, KC, 2112], BF16); B_Wk = Buf("Wk")
        stg = [sb("wstg%d" % i, [128, 2048], F32) for i in range(2)]
        B_stg = [Buf("wstg%d" % i) for i in range(2)]
        prep_weight(Wk, B_Wk, w_in, KC,
                    [(O_AK, 1024, 0, 1.0), (O_IK, 64, 1024, 1.0), (O_BK, 1024, 1088, 1.0)], 0, stg, B_stg)
        fe = make_front("a")
        fst = [sb("fst%d" % i, [128, 9, 512], BF16) for i in range(2)]
        B_fst = [Buf("fst%d" % i) for i in range(2)]
        avst = [sb("avst%d" % i, [128, 8, 4, 65], BF16) for i in range(2)]
        B_avst = [Buf("avst%d" % i) for i in range(2)]
        bvst = [sb("bvst%d" % i, [128, 8, 4, 64], BF16) for i in range(2)]
        B_bvst = [Buf("bvst%d" % i) for i in range(2)]
        for i in range(2):
            P.op("pool", R.memset(avst[i][:], 1.0), writes=[B_avst[i]])
        front_load(fe, 0, xf[0:512, :])
        for g in range(NGF):
            b = g % 2
            if g + 1 < NGF:
                front_load(fe, g + 1, xf[(g + 1) * 512:(g + 2) * 512, :])
            hT, B_hT = front(fe, g, None, loaded=True)
            for oc in range(9):
                if oc < 4:
                    c0, m = oc * 128, 128
                elif oc < 8:
                    c0, m = 1088 + (oc - 4) * 128, 128
                else:
                    c0, m = 1024, 64
                pt, B_pt = proj_fm(Wk, B_Wk, hT, B_hT, c0, m)
                evac(fst[b][0:m, oc, :], pt[0:m, :], B_pt, B_fst[b])
            store(akT[:, g * 512:(g + 1) * 512].rearrange("(c p) t -> p c t", p=128), fst[b][:, 0:4, :], B_fst[b])
            store(bkT[:, g * 512:(g + 1) * 512].rearrange("(c p) t -> p c t", p=128), fst[b][:, 4:8, :], B_fst[b])
            store(ikT[:, g * 512:(g + 1) * 512], fst[b][0:64, 8, :], B_fst[b])
            for j in range(4):
                pt, B_pt = proj_tm(Wk, B_Wk, hT, B_hT, j, 512, 512)
                evac(avst[b][:, :, j, 0:64], pt[:, :].rearrange("p (h d) -> p h d", h=8), B_pt, B_avst[b])
                pt, B_pt = proj_tm(Wk, B_Wk, hT, B_hT, j, 1600, 512)
                evac(bvst[b][:, :, j, :], pt[:, :].rearrange("p (h d) -> p h d", h=8), B_pt, B_bvst[b])
            for h in range(8):
                store(avh[h, :, g * 4:(g + 1) * 4, :], avst[b][:, h, :, :], B_avst[b])
                store(bvh[h, :, g * 4:(g + 1) * 4, :], bvst[b][:, h, :, :], B_bvst[b])
        P.barrier()
        ES.pop().close()

    if stop_after >= 2:
        ES.append(ExitStack())
        Wq = sb("Wq", [128, KC, 5128], BF16); B_Wq = Buf("Wq")
        Wm = sb("Wm", [128, KC, 1024], BF16); B_Wm = Buf("Wm")
        ES.append(ExitStack())
        stg = [sb("wstg%d" % i, [128, 2048], F32) for i in range(2)]
        B_stg = [Buf("wstgq%d" % i) for i in range(2)]
        prep_weight(Wm, B_Wm, w_mem_kv, KC, [(0, 1024, 0, 1.0)], 1, stg, B_stg)
        prep_weight(Wq, B_Wq, w_in, KC,
                    [(O_AQ, 512, 0, 0.125), (O_IQ, 512, 512, 1.0), (O_BQ, 512, 1024, 0.125),
                     (O_CQ, 512, 1536, 128 ** -0.5), (O_IW, 8, 2048, 1.0), (O_G, 3072, 2056, 1.0)], 0, stg, B_stg)
        P.barrier()
        ES.pop().close()
        fe = make_front("q")
        hT, B_hT = front(fe, 1, mem[:, :], nb=2)
        for h in range(4):
            pt, B_pt = proj_fm(Wm, B_Wm, hT, B_hT, h * 128, 128, n=NMEM)
            evac(mkT[:, h, :], pt[:, 0:NMEM], B_pt, B_mkT)
        for j in range(2):
            pt, B_pt = proj_tm(Wm, B_Wm, hT, B_hT, j, 512, 512)
            evac(mvS[:, j, :], pt[:, :], B_pt, B_mvS)
        qst = [sb("qst%d" % i, [128, 12, 512], BF16) for i in range(1)] * 2
        B_qst = [Buf("qst%d" % i) for i in range(1)] * 2
        cqs = [sb("cqs%d" % i, [128, 4, 512], BF16) for i in range(1)] * 2
        B_cqs = [Buf("cqs%d" % i) for i in range(1)] * 2
        gst = [sb("gst%d" % i, [128, 512], BF16) for i in range(4)]
        B_gst = [Buf("gst%d" % i) for i in range(4)]
        pm = [sb("pm%d" % i, [128, 512], BF16) for i in range(4)]
        B_pm = [Buf("pm%d" % i) for i in range(4)]
        rD = [sb("rD%d" % i, [128, 512], F32) for i in range(2)]
        B_rD = [Buf("rD%d" % i) for i in range(2)]
        ocst = [sb("ocst%d" % i, [128, 4, 512], BF16) for i in range(2)]
        B_ocst = [Buf("ocst%d" % i) for i in range(2)]
        gi = 0
        pmi = 0
        front_load(fe, 0, xq[0:512, :])
        for g in range(NSB):
            b = g % 2
            tsl = slice(g * 512, (g + 1) * 512)
            if g + 1 < NSB:
                front_load(fe, g + 1, xq[(g + 1) * 512:(g + 2) * 512, :])
            hT, B_hT = front(fe, g, None, loaded=True)
            for oc in range(12):
                pt, B_pt = proj_fm(Wq, B_Wq, hT, B_hT, oc * 128, 128)
                evac(qst[b][:, oc, :], pt[:, :], B_pt, B_qst[b])
            store(aqT[:, tsl].rearrange("(c p) t -> p c t", p=128), qst[b][:, 0:4, :], B_qst[b])
            store(iqT[:, tsl].rearrange("(c p) t -> p c t", p=128), qst[b][:, 4:8, :], B_qst[b])
            store(bqT[:, tsl].rearrange("(c p) t -> p c t", p=128), qst[b][:, 8:12, :], B_qst[b])
            for h in range(4):
                pt, B_pt = proj_fm(Wq, B_Wq, hT, B_hT, 1536 + h * 128, 128)
                evac(cqs[b][:, h, :], pt[:, :], B_pt, B_cqs[b])
            for j in range(4):
                pt, B_pt = proj_tm(Wq, B_Wq, hT, B_hT, j, 2048, 8)
                P.op("dve", R.tensor_scalar(
                    out=iwsgn[:, g * 4 + j, :], in0=pt[:, 0:8], scalar1=0.0, scalar2=0.5,
                    op0=ALU.is_ge, op1=ALU.subtract),
                    reads=[B_pt], acc=[B_iwsgn])
                P.op("dve", R.scalar_tensor_tensor(
                    out=iwabs[:, g * 4 + j, :], in0=pt[:, 0:8], scalar=2.0, in1=iwsgn[:, g * 4 + j, :],
                    op0=ALU.mult, op1=ALU.mult),
                    reads=[B_pt, B_iwsgn], acc=[B_iwabs])
            for h in range(4):
                pms = []
                for mt in range(2):
                    pz, B_pz = next_ps()
                    P.op("pe", R.matmul(
                        pz[:, :], lhsT=mkT[:, h, mt * 128:(mt + 1) * 128], rhs=cqs[b][:, h, :],
                        start=True, stop=True), reads=[B_mkT, B_cqs[b]], writes=[B_pz])
                    k = pmi % 4
                    pmi += 1
                    P.op("act", R.activation(out=pm[k][:], in_=pz[:, :], func=AF.Exp),
                         reads=[B_pz], writes=[B_pm[k]])
                    pms.append(k)
                po, B_po = next_ps()
                pd, B_pd = next_ps()
                for mt in range(2):
                    k = pms[mt]
                    P.op("pe", R.matmul(
                        po[:, :], lhsT=mvS[:, mt, h * 128:(h + 1) * 128], rhs=pm[k][:],
                        start=(mt == 0), stop=(mt == 1)), reads=[B_mvS, B_pm[k]], acc=[B_po], inc=(mt == 1))
                for mt in range(2):
                    k = pms[mt]
                    P.op("pe", R.matmul(
                        pd[:, :], lhsT=ones_b, rhs=pm[k][:], start=(mt == 0), stop=(mt == 1)),
                        reads=[B_cst, B_pm[k]], acc=[B_pd], inc=(mt == 1))
                r = h % 2
                P.op("dve", R.reciprocal(out=rD[r][:], in_=pd[:, :]),
                     reads=[B_pd], writes=[B_rD[r]])
                P.op("dve", R.tensor_tensor(
                    out=ocst[b][:, h, :], in0=po[:, :], in1=rD[r][:], op=ALU.mult),
                    reads=[B_po, B_rD[r]], acc=[B_ocst[b]])
            store(OcT[:, tsl].rearrange("(c p) t -> p c t", p=128), ocst[b][:, :, :], B_ocst[b])
            for oc in range(24):
                pt, B_pt = proj_fm(Wq, B_Wq, hT, B_hT, 2056 + oc * 128, 128)
                k = gi % 4
                gi += 1
                P.op("act", R.activation(
                    out=gst[k][:], in_=pt[:, :], func=AF.Sigmoid, bias=bgate[:, oc:oc + 1]),
                    reads=[B_pt, B_bgate], writes=[B_gst[k]])
                store(gT[oc, :, tsl], gst[k][:], B_gst[k])
        P.barrier()
        ES.pop().close()

    ONE_T = sb("one_t", [128, 1], F32); B_one = Buf("one")
    P.op("pool", R.memset(ONE_T[:], 1.0), writes=[B_one])
    if stop_after >= 3:
        ES.append(ExitStack())
        kTs = [sb("kTs%d" % i, [128, S], BF16) for i in range(2)]; B_kTs = [Buf("kTs%d" % i) for i in range(2)]
        Vs = [sb("Vs%d" % i, [128, NT, 64], BF16) for i in range(2)]; B_Vs = [Buf("Vs%d" % i) for i in range(2)]
        qs = [sb("qs%d" % i, [128, 512], BF16) for i in range(2)]; B_qs = [Buf("qs%d" % i) for i in range(2)]
        for i in range(2):
            P.op("pool", R.memset(kTs[i][64:128, :], 0.0), acc=[B_kTs[i]])
            P.op("pool", R.memset(qs[i][64:128, :], 0.0), acc=[B_qs[i]])
        maskc = sb("maskc", [128, 8, 512], BF16); B_maskc = Buf("maskc")
        P.dma(maskc[:], maskc_in[:, :, :], writes=[B_maskc], sem=B_maskc.sem)
        e_sb = [sb("e_sb%d" % i, [128, 512], F32) for i in range(2)]; B_e = [Buf("e%d" % i) for i in range(2)]
        sp_sb = [sb("sp_sb%d" % i, [128, 512], BF16) for i in range(4)]; B_sp = [Buf("sp%d" % i) for i in range(4)]
        spacc = [sb("spacc%d" % i, [128, 512], BF16) for i in range(2)]; B_spacc = [Buf("spacc%d" % i) for i in range(2)]
        a_sb = [sb("a_sb%d" % i, [128, 512], BF16) for i in range(3)]; B_a = [Buf("a%d" % i) for i in range(3)]
        obst = [sb("obst%d" % i, [64, 512], BF16) for i in range(2)]; B_obst = [Buf("obst%d" % i) for i in range(2)]
        items = [(G, h) for G in range(NSB) for h in range(8)]

        def sb_load(idx):
            G, h = items[idx]
            kb = idx % 2
            KL = (G + 1) * 1024
            P.dma(kTs[kb][0:64, 0:KL], bkT[h * 64:(h + 1) * 64, 0:KL], acc=[B_kTs[kb]], sem=B_kTs[kb].sem)
            P.dma(Vs[kb][:, 0:KL // 128, :], bvh[h, :, 0:KL // 128, :], writes=[B_Vs[kb]], sem=B_Vs[kb].sem)
            P.dma(qs[kb][0:64, :], bqT[h * 64:(h + 1) * 64, G * 512:(G + 1) * 512], acc=[B_qs[kb]], sem=B_qs[kb].sem)

        cz = [0]; csp = [0]; cc = [0]; ca = [0]
        sb_load(0)
        for idx, (G, h) in enumerate(items):
            kb = idx % 2
            if idx + 1 < len(items):
                sb_load(idx + 1)
            n = (G + 1) * 8
            psO, B_psO = psb[4 + kb], B_ps[4 + kb]
            st = {}
            st2 = {}

            def stage1(i):
                kt = n - 1 - i
                j = kt - 8 * G
                zi = cz[0] % 2; cz[0] += 1
                si = csp[0] % 4; csp[0] += 1
                st[i] = (zi, si)
                P.op("pe", R.matmul(psb[zi][:, :], lhsT=kTs[kb][:, kt * 128:(kt + 1) * 128], rhs=qs[kb][:],
                                    start=True, stop=True),
                     reads=[B_kTs[kb], B_qs[kb]], writes=[B_ps[zi]])
                P.op("act", R.activation(out=e_sb[zi % 2][:], in_=psb[zi][:, :], func=AF.Exp),
                     reads=[B_ps[zi]], writes=[B_e[zi % 2]])
                P.op("act", R.activation(out=sp_sb[si][:], in_=e_sb[zi % 2][:], func=AF.Ln, bias=ONE_T[:, 0:1]),
                     reads=[B_e[zi % 2], B_one], writes=[B_sp[si]])
                if j >= 0:
                    P.op("dve", R.tensor_tensor(out=sp_sb[si][:], in0=sp_sb[si][:], in1=maskc[:, j, :], op=ALU.mult),
                         reads=[B_sp[si], B_maskc], writes=[B_sp[si]])

            accprev = [None, None]

            def stage2(i):
                kt = n - 1 - i
                j = kt - 8 * G
                zi, si = st.pop(i)
                ci = 2 + (cc[0] % 2); cc[0] += 1
                ai = ca[0] % 3; ca[0] += 1
                st2[i] = ai
                P.op("pe", R.matmul(psb[ci][:, :], lhsT=kTs[kb][:, kt * 128:(kt + 1) * 128], rhs=qs[kb][:],
                                    start=True, stop=False),
                     reads=[B_kTs[kb], B_qs[kb]], writes=[B_ps[ci]], inc=False)
                P.op("pe", R.matmul(psb[ci][:, :], lhsT=negtri, rhs=sp_sb[si][:], start=False, stop=(i == 0)),
                     reads=[B_cst, B_sp[si]], acc=[B_ps[ci]], inc=(i == 0))
                if i > 0:
                    ap_prev, B_prev = accprev
                    P.op("pe", R.matmul(psb[ci][:, :], lhsT=negones, rhs=ap_prev, start=False, stop=True),
                         reads=[B_cst, B_prev], acc=[B_ps[ci]])
                if i < n - 1:
                    if i == 0:
                        accprev[0], accprev[1] = sp_sb[si][:], B_sp[si]
                    else:
                        ap_prev, B_prev = accprev
                        k = i % 2
                        P.op("pool", R.tensor_tensor(out=spacc[k][:], in0=ap_prev, in1=sp_sb[si][:], op=ALU.add),
                             reads=[B_prev, B_sp[si]], writes=[B_spacc[k]])
                        accprev[0], accprev[1] = spacc[k][:], B_spacc[k]
                P.op("act", R.activation(out=a_sb[ai][:], in_=psb[ci][:, :], func=AF.Exp),
                     reads=[B_ps[ci]], writes=[B_a[ai]])
                if j >= 0:
                    P.op("dve", R.tensor_tensor(out=a_sb[ai][:], in0=a_sb[ai][:], in1=maskc[:, j, :], op=ALU.mult),
                         reads=[B_a[ai], B_maskc], writes=[B_a[ai]])

            def stage3(i):
                kt = n - 1 - i
                ai = st2.pop(i)
                if i == 0:
                    P.op("pe", R.matmul(psO[0:64, :], lhsT=Vs[kb][:, kt, :], rhs=a_sb[ai][:],
                                        start=True, stop=(n == 1)),
                         reads=[B_Vs[kb], B_a[ai]], writes=[B_psO])
                else:
                    P.op("pe", R.matmul(psO[0:64, :], lhsT=Vs[kb][:, kt, :], rhs=a_sb[ai][:],
                                        start=False, stop=(i == n - 1)),
                         reads=[B_Vs[kb], B_a[ai]], acc=[B_psO])

            for step in range(n + 2):
                if step < n:
                    stage1(step)
                if 0 <= step - 1 < n:
                    stage2(step - 1)
                if 0 <= step - 2 < n:
                    stage3(step - 2)
            evac(obst[kb][:], psO[0:64, :], B_psO, B_obst[kb])
            store(ObT[h * 64:(h + 1) * 64, G * 512:(G + 1) * 512], obst[kb][:], B_obst[kb])
        P.barrier()
        ES.pop().close()

    if stop_after >= 4:
        ES.append(ExitStack())
        ikTs = sb("ikTs", [128, S], BF16); B_ikTs = Buf("ikTs")
        P.op("pool", R.memset(ikTs[64:128, :], 0.0), acc=[B_ikTs])
        P.dma(ikTs[0:64, :], ikT[:, :], acc=[B_ikTs], sem=B_ikTs.sem)
        negadm = sb("negadm", [128, 4, 1024], BF16); B_negadm = Buf("negadm")
        P.dma(negadm[:], negadm_in[:, :, :], writes=[B_negadm], sem=B_negadm.sem)
        iqs = [sb("iqs%d" % i, [128, 8, 512], BF16) for i in range(2)]; B_iqs = [Buf("iqs%d" % i) for i in range(2)]
        for i in range(2):
            P.op("pool", R.memset(iqs[i][64:128, :, :], 0.0), acc=[B_iqs[i]])
        sc = [sb("sc%d" % i, [128, S], F32) for i in range(2)]
        B_scc = [[Buf("sc%d_%d" % (i, c)) for c in range(S // 512)] for i in range(2)]
        B_sc = [Buf("scw%d" % i) for i in range(2)]
        r_sb = [sb("r_sb%d" % i, [128, 512], BF16) for i in range(4)]; B_r = [Buf("r%d" % i) for i in range(4)]
        dgs = [sb("dgs%d" % i, [128, 8, 128], BF16) for i in range(2)]; B_dgs = [Buf("dgs%d" % i) for i in range(2)]
        szi = [0]
        selt = [sb("selt%d" % i, [128, S], BF16) for i in range(2)]; B_sel = [Buf("sel%d" % i) for i in range(2)]
        junk4 = sb("junk4", [128, S], BF16); B_junk4 = Buf("junk4")
        junk5 = sb("junk5", [128, S], BF16); B_junk5 = Buf("junk5")
        sm = [sb("sm%d" % i, [128, 8], F32) for i in range(2)]
        B_sm = [[Buf("sm%d_%d" % (i, k)) for k in range(8)] for i in range(2)]
        mst = [sb("mst%d" % i, [128, 8, 128], mybir.dt.uint8) for i in range(3)]; B_mst = [Buf("mst%d" % i) for i in range(3)]
        ri = 0
        msi = 0
        pti = 0
        P.dma(iqs[0][0:64, :, :], iqT.rearrange("(h d) t -> d h t", d=64)[:, :, 0:512], acc=[B_iqs[0]], sem=B_iqs[0].sem)
        for G in range(NSB):
            gb = G % 2
            if G + 1 < NSB:
                P.dma(iqs[1 - gb][0:64, :, :], iqT.rearrange("(h d) t -> d h t", d=64)[:, :, (G + 1) * 512:(G + 2) * 512],
                      acc=[B_iqs[1 - gb]], sem=B_iqs[1 - gb].sem)
            KL = (G + 1) * 1024
            nch = KL // 512
            for qp in range(2):
                chains = []
                for qi in range(2):
                    qb = 2 * qp + qi
                    blk = G * 4 + qb
                    cb = qi
                    scb = sc[cb]
                    smb, B_smb = sm[cb], B_sm[cb]
                    for ih in range(8):
                        P.op("pool", R.tensor_scalar(out=dgs[cb][:, ih, :], in0=ident, scalar1=iwabs[:, blk, ih:ih + 1],
                                                     scalar2=iwsgn[:, blk, ih:ih + 1], op0=ALU.mult, op1=ALU.mult),
                             reads=[B_cst, B_iwabs, B_iwsgn], acc=[B_dgs[cb]],
                             extra=(list(B_dgs[cb].r.items()) if ih == 0 else []))
                    its = [(c, ih) for c in range(nch) for ih in range(8)]
                    pend = []
                    for n_it in range(len(its) + 2):
                        if n_it < len(its):
                            c, ih = its[n_it]
                            z = szi[0] % 4; szi[0] += 1
                            P.op("pe", R.matmul(psb[z][:, :], lhsT=iqs[gb][:, ih, qb * 128:(qb + 1) * 128],
                                                rhs=ikTs[:, c * 512:(c + 1) * 512], start=True, stop=True),
                                 reads=[B_iqs[gb], B_ikTs], writes=[B_ps[z]])
                            k = ri % 4
                            ri += 1
                            P.op("act", R.activation(out=r_sb[k][:], in_=psb[z][:, :], func=AF.Relu),
                                 reads=[B_ps[z]], writes=[B_r[k]])
                            pend.append((c, ih, k))
                        if n_it >= 2:
                            c, ih, k = pend.pop(0)
                            ab = 4 + (c % 2)
                            if ih == 0:
                                P.op("pe", R.matmul(psb[ab][:, :], lhsT=dgs[cb][:, ih, :], rhs=r_sb[k][:],
                                                    start=True, stop=False),
                                     reads=[B_dgs[cb], B_r[k]], writes=[B_ps[ab]], inc=True)
                            else:
                                P.op("pe", R.matmul(psb[ab][:, :], lhsT=dgs[cb][:, ih, :], rhs=r_sb[k][:],
                                                    start=False, stop=(ih == 7)),
                                     reads=[B_dgs[cb], B_r[k]], acc=[B_ps[ab]], inc=True)
                            if ih == 7:
                                dst = scb[:, c * 512:(c + 1) * 512]
                                if c % 2 == 0:
                                    P.op("dve", R.tensor_copy(out=dst, in_=psb[ab][:, :]),
                                         reads=[B_ps[ab]], writes=[B_scc[cb][c]], extra=list(B_sc[cb].r.items()))
                                else:
                                    P.op("act", R.copy(out=dst, in_=psb[ab][:, :]),
                                         reads=[B_ps[ab]], writes=[B_scc[cb][c]], extra=list(B_sc[cb].r.items()))
                    chunks = [B_scc[cb][c] for c in range(nch)]
                    P.op("dve", R.tensor_reduce(out=smb[:, 0:1], in_=scb[:, 0:KL], axis=AX.X, op=ALU.max),
                         reads=chunks, writes=[B_smb[0]])
                    P.op("dve", R.tensor_reduce(out=smb[:, 1:2], in_=scb[:, 0:KL], axis=AX.X, op=ALU.min),
                         reads=chunks, writes=[B_smb[1]])
                    P.op("dve", R.tensor_tensor(out=scb[:, KL - 1024:KL], in0=scb[:, KL - 1024:KL],
                                                in1=negadm[:, qb, :], op=ALU.add),
                         reads=chunks + [B_negadm], writes=[B_sc[cb]] + chunks[-2:])
                    P.op("dve", R.tensor_tensor(out=smb[:, 2:3], in0=smb[:, 0:1], in1=smb[:, 1:2], op=ALU.subtract),
                         reads=[B_smb[0], B_smb[1]], writes=[B_smb[2]])
                    P.op("dve", R.tensor_scalar(out=smb[:, 3:4], in0=smb[:, 2:3], scalar1=1.0 + 2.0 ** -9,
                                                scalar2=2e-20, op0=ALU.mult, op1=ALU.add),
                         reads=[B_smb[2]], writes=[B_smb[3]])
                    P.op("dve", R.scalar_tensor_tensor(out=smb[:, 4:5], in0=smb[:, 2:3], scalar=-(2.0 ** -10),
                                                       in1=smb[:, 1:2], op0=ALU.mult, op1=ALU.add),
                         reads=[B_smb[2], B_smb[1]], writes=[B_smb[4]])
                    chains.append((qb, cb, scb, smb, B_smb))
                for k in range(NBIS):
                    hk = 2.0 ** -(k + 1)
                    for (qb, cb, scb, smb, B_smb) in chains:
                        if cb == 0:
                            P.op("dve", R.scalar_tensor_tensor(out=smb[:, 5:6], in0=smb[:, 3:4], scalar=hk,
                                                               in1=smb[:, 4:5], op0=ALU.mult, op1=ALU.add),
                                 reads=[B_smb[3], B_smb[4]], writes=[B_smb[5]])
                        else:
                            P.op("dve", R.scalar_tensor_tensor(out=smb[:, 5:6], in0=smb[:, 3:4], scalar=-hk,
                                                               in1=smb[:, 4:5], op0=ALU.mult, op1=ALU.subtract),
                                 reads=[B_smb[3], B_smb[4]], writes=[B_smb[5]])
                    for (qb, cb, scb, smb, B_smb) in reversed(chains):
                        if cb == 0:
                            P.op("dve", R.tensor_scalar(out=junk4[:, 0:KL], in0=scb[:, 0:KL], scalar1=smb[:, 5:6],
                                                        scalar2=None, op0=ALU.is_ge, op1=ALU.add,
                                                        accum_out=smb[:, 6:7]),
                                 reads=[B_sc[cb], B_smb[5]], writes=[B_junk4, B_smb[6]])
                        else:
                            P.op("act", R.activation(out=junk5[:, 0:KL], in_=scb[:, 0:KL], func=AF.Sign,
                                                     bias=smb[:, 5:6], accum_out=smb[:, 6:7]),
                                 reads=[B_sc[cb], B_smb[5]], writes=[B_junk5, B_smb[6]])
                    for (qb, cb, scb, smb, B_smb) in chains:
                        thr = (KSEL - 0.5) if cb == 0 else (2.0 * KSEL - 1.0 - KL)
                        P.op("dve", R.tensor_scalar(out=smb[:, 7:8], in0=smb[:, 6:7], scalar1=thr,
                                                    scalar2=hk, op0=ALU.is_ge, op1=ALU.mult),
                             reads=[B_smb[6]], writes=[B_smb[7]])
                        P.op("dve", R.scalar_tensor_tensor(out=smb[:, 4:5], in0=smb[:, 7:8], scalar=smb[:, 3:4],
                                                           in1=smb[:, 4:5], op0=ALU.mult, op1=ALU.add),
                             reads=[B_smb[7], B_smb[3], B_smb[4]], writes=[B_smb[4]])
                for (qb, cb, scb, smb, B_smb) in chains:
                    P.op("dve", R.tensor_scalar(out=selt[cb][:, 0:KL], in0=scb[:, 0:KL], scalar1=smb[:, 4:5],
                                                scalar2=None, op0=ALU.is_ge),
                         reads=[B_sc[cb], B_smb[4]], writes=[B_sel[cb]])
                    for k0 in range(0, KL // 128, 8):
                        pb = pti % 2
                        pti += 1
                        for kk in range(8):
                            kt = k0 + kk
                            P.op("pe", R.transpose(
                                out=psT[pb][:, kk * 128:(kk + 1) * 128], in_=selt[cb][:, kt * 128:(kt + 1) * 128],
                                identity=ident), reads=[B_sel[cb], B_cst], acc=[B_psT[pb]], inc=(kk == 7))
                        m = msi % 3
                        msi += 1
                        P.op("act", R.copy(out=mst[m][:], in_=psT[pb][:, :].rearrange(
                            "p (k t) -> p k t", k=8)), reads=[B_psT[pb]], writes=[B_mst[m]])
                        store(mskT[G, :, k0:k0 + 8, qb * 128:(qb + 1) * 128], mst[m][:], B_mst[m])
        P.barrier()
        ES.pop().close()

    if stop_after >= 5:
        ES.append(ExitStack())
        kTs = [sb("akTs%d" % i, [128, S], BF16) for i in range(2)]; B_kTs = [Buf("akTs%d" % i) for i in range(2)]
        Vs = [sb("aVs%d" % i, [128, NT, 65], BF16) for i in range(2)]; B_Vs = [Buf("aVs%d" % i) for i in range(2)]
        qs = [sb("aqs%d" % i, [128, 512], BF16) for i in range(2)]; B_qs = [Buf("aqs%d" % i) for i in range(2)]
        for i in range(2):
            P.op("pool", R.memset(kTs[i][64:128, :], 0.0), acc=[B_kTs[i]])
            P.op("pool", R.memset(qs[i][64:128, :], 0.0), acc=[B_qs[i]])
        bsb_all = sb("bsb_all", [128, 8, 9, 512], BF16); B_bsball = Buf("bsb_all")
        biasc = sb("biasc", [128, 8], F32); B_biasc = Buf("biasc")
        P.dma(biasc[:], biasc_in[:, :], writes=[B_biasc], sem=B_biasc.sem)
        mk_sb = sb("mk_sb", [128, NT, 512], mybir.dt.uint8); B_mk = [Buf("mk%d" % i) for i in range(NT // 8)]
        p_sb = [sb("p_sb%d" % i, [128, 512], BF16) for i in range(4)]; B_p = [Buf("p%d" % i) for i in range(4)]
        pm_sb = [sb("pm_sb%d" % i, [128, 512], BF16) for i in range(6)]; B_pmm = [Buf("pmm%d" % i) for i in range(6)]
        Osb = [sb("Osb%d" % i, [65, 512], F32) for i in range(2)]; B_Osb = [Buf("Osb%d" % i) for i in range(2)]
        oast = [sb("oast%d" % i, [64, 512], BF16) for i in range(2)]; B_oast = [Buf("oast%d" % i) for i in range(2)]
        sel65 = sb("sel65", [65, 64], F32); B_sel65 = Buf("sel65")
        P.op("pool", R.memset(sel65[:], 0.0), writes=[B_sel65])
        P.op("pool", R.memset(sel65[64:65, :], 1.0), reads=[B_sel65], writes=[B_sel65])
        items = [(G, h) for G in range(NSB) for h in range(8)]

        def dsa_load(idx):
            G, h = items[idx]
            kb = idx % 2
            KL = (G + 1) * 1024
            P.dma(kTs[kb][0:64, 0:KL], akT[h * 64:(h + 1) * 64, 0:KL], acc=[B_kTs[kb]], sem=B_kTs[kb].sem)
            P.dma(Vs[kb][:, 0:KL // 128, :], avh[h, :, 0:KL // 128, :], writes=[B_Vs[kb]], sem=B_Vs[kb].sem)
            P.dma(qs[kb][0:64, :], aqT[h * 64:(h + 1) * 64, G * 512:(G + 1) * 512], acc=[B_qs[kb]], sem=B_qs[kb].sem)

        ES.append(ExitStack())
        bsf = [sb("bsf%d" % i, [128, 9, 512], F32) for i in range(1)]; B_bsf = [Buf("bsf%d" % i) for i in range(1)]
        for hh in range(8):
            P.dma(bsf[0][:], biasT_in[hh, :, :, :], writes=[B_bsf[0]], sem=B_bsf[0].sem)
            if hh % 2 == 0:
                P.op("act", R.copy(out=bsb_all[:, hh, :, :], in_=bsf[0][:]), reads=[B_bsf[0]], acc=[B_bsball])
            else:
                P.op("dve", R.tensor_copy(out=bsb_all[:, hh, :, :], in_=bsf[0][:]), reads=[B_bsf[0]], acc=[B_bsball])
        P.barrier()
        ES.pop().close()
        pi = 0
        zi = 0
        dsa_load(0)
        for idx, (G, h) in enumerate(items):
            kb = idx % 2
            if h == 0:
                for sg in range(G + 1):
                    P.dma(mk_sb[:, sg * 8:(sg + 1) * 8, :], mskT[G, :, sg * 8:(sg + 1) * 8, :],
                          writes=[B_mk[sg]], sem=B_mk[sg].sem)
            if idx + 1 < len(items):
                dsa_load(idx + 1)
            n = (G + 1) * 8
            psO, B_psO = psb[4 + kb], B_ps[4 + kb]
            LA = 4
            pmk = {}
            for step in range(n + LA):
                if step < n:
                    kt = step
                    j = kt - 8 * G
                    near = j >= -1
                    z = zi % 4
                    zi += 1
                    k = pi % 4
                    km = pi % 6
                    pi += 1
                    pmk[kt] = km
                    P.op("pe", R.matmul(
                        psb[z][:, :], lhsT=kTs[kb][:, kt * 128:(kt + 1) * 128], rhs=qs[kb][:], start=True, stop=not near),
                        reads=[B_kTs[kb], B_qs[kb]], writes=[B_ps[z]], inc=not near)
                    if near:
                        P.op("pe", R.matmul(psb[z][:, :], lhsT=ident, rhs=bsb_all[:, h, j + 1, :], start=False, stop=True),
                             reads=[B_cst, B_bsball], acc=[B_ps[z]])
                        P.op("act", R.activation(out=p_sb[k][:], in_=psb[z][:, :], func=AF.Exp),
                             reads=[B_ps[z]], writes=[B_p[k]])
                    else:
                        P.op("act", R.activation(out=p_sb[k][:], in_=psb[z][:, :], func=AF.Exp, bias=biasc[:, h:h + 1]),
                             reads=[B_ps[z], B_biasc], writes=[B_p[k]])
                    P.op("dve" if (kt % 3) != 2 else "pool",
                         R.tensor_tensor(out=pm_sb[km][:], in0=p_sb[k][:], in1=mk_sb[:, kt, :], op=ALU.mult),
                         reads=[B_p[k], B_mk[kt // 8]], writes=[B_pmm[km]])
                kt = step - LA
                if kt >= 0:
                    km = pmk.pop(kt)
                    if kt == 0:
                        P.op("pe", R.matmul(psO[0:65, :], lhsT=Vs[kb][:, kt, :], rhs=pm_sb[km][:],
                                            start=True, stop=(n == 1)),
                             reads=[B_Vs[kb], B_pmm[km]], writes=[B_psO])
                    else:
                        P.op("pe", R.matmul(psO[0:65, :], lhsT=Vs[kb][:, kt, :], rhs=pm_sb[km][:],
                                            start=False, stop=(kt == n - 1)),
                             reads=[B_Vs[kb], B_pmm[km]], acc=[B_psO])
            P.op("act", R.copy(out=Osb[kb][:], in_=psO[0:65, :]), reads=[B_psO], writes=[B_Osb[kb]])
            P.op("dve", R.reciprocal(out=Osb[kb][64:65, :], in_=Osb[kb][64:65, :]),
                 reads=[B_Osb[kb]], writes=[B_Osb[kb]])
            pt, B_pt = psb[zi % 4], B_ps[zi % 4]
            zi += 1
            P.op("pe", R.matmul(pt[0:64, :], lhsT=sel65[:, :], rhs=Osb[kb][:, :], start=True, stop=True),
                 reads=[B_sel65, B_Osb[kb]], writes=[B_pt])
            P.op("dve", R.tensor_tensor(out=oast[kb][:], in0=Osb[kb][0:64, :], in1=pt[0:64, :],
                                                         op=ALU.mult),
                 reads=[B_Osb[kb], B_pt], writes=[B_oast[kb]])
            store(OaT[h * 64:(h + 1) * 64, G * 512:(G + 1) * 512], oast[kb][:], B_oast[kb])
        P.barrier()
        ES.pop().close()

    gpost = sb("gpost", [128, 2, D], F32); B_gpost = Buf("gpost")
    P.dma(gpost[:], grows[:, :, :], writes=[B_gpost], sem=B_gpost.sem)

    def post_norm(psA, B_psA, psB_, B_psB, gi, res_ap, B_res, out_ap, B_out, tmp, B_tmp, ss2, B_ss2, junk, B_junk):
        P.op("act", R.activation(out=junk[:, 0:512], in_=psA[:, :], func=AF.Square, accum_out=ss2[:, 0:1]),
             reads=[B_psA], writes=[B_junk], acc=[B_ss2])
        P.op("act", R.activation(out=junk[:, 0:512], in_=psB_[:, :], func=AF.Square, accum_out=ss2[:, 1:2]),
             reads=[B_psB], writes=[B_junk], acc=[B_ss2])
        P.op("dve", R.tensor_tensor(out=ss2[:, 2:3], in0=ss2[:, 0:1], in1=ss2[:, 1:2], op=ALU.add),
             reads=[B_ss2], writes=[B_ss2])
        P.op("act", R.activation(out=ss2[:, 3:4], in_=ss2[:, 2:3], func=AF.Sqrt, bias=EPS_T[:, 0:1],
                                           scale=1.0 / D), reads=[B_ss2, B_eps], writes=[B_ss2])
        P.op("dve", R.reciprocal(out=ss2[:, 3:4], in_=ss2[:, 3:4]), reads=[B_ss2], writes=[B_ss2])
        for half, (pp, B_pp) in enumerate(((psA, B_psA), (psB_, B_psB))):
            hs = slice(half * 512, (half + 1) * 512)
            P.op("dve", R.scalar_tensor_tensor(
                out=tmp[:, hs], in0=pp[:, :], scalar=ss2[:, 3:4], in1=gpost[:, gi, hs], op0=ALU.mult, op1=ALU.mult),
                reads=[B_pp, B_ss2, B_gpost], acc=[B_tmp])
        P.op("pool", R.tensor_tensor(out=out_ap, in0=tmp[:, :], in1=res_ap, op=ALU.add),
             reads=[B_tmp, B_res], acc=[B_out])

    if stop_after >= 6:
        ES.append(ExitStack())
        Wup = sb("Wup", [128, 12, D], BF16); B_Wup = Buf("Wup")
        Wo = sb("Wo", [128, KC, D], BF16); B_Wo = Buf("Wo")
        stg = [sb("wstg6%d" % i, [128, 2048], F32) for i in range(2)]
        B_stg = [Buf("wstg6%d" % i) for i in range(2)]
        prep_weight(Wup, B_Wup, w_up.rearrange("r k n -> (r k) n"), 12, [(0, 1024, 0, 1.0)], None, stg, B_stg)
        prep_weight(Wo, B_Wo, w_out, KC, [(0, 1024, 0, 1.0)], None, stg, B_stg)
        OT = [sb("OT%d" % r, [128, 4, 512], BF16) for r in range(3)]; B_OT = [Buf("OT%d" % r) for r in range(3)]
        gts = sb("gts", [128, 24, 512], BF16); B_gts = Buf("gts")
        xo = sb("xo", [128, 4, D], F32); B_xo = Buf("xo")
        mT = sb("mT", [128, KC, 512], BF16); B_mT = Buf("mT")
        mt_ = [sb("mtmp%d" % i, [128, 512], F32) for i in range(4)]; B_mt = [Buf("mtmp%d" % i) for i in range(4)]
        tmp6 = sb("tmp6", [128, D], F32); B_tmp6 = Buf("tmp6")
        ss6 = sb("ss6", [128, 4], F32); B_ss6 = Buf("ss6")
        junk6 = sb("junk6", [128, 512], BF16); B_junk6 = Buf("junk6")
        x1st = sb("x1st", [128, 4, D], F32); B_x1st = Buf("x1st")
        srcs = [OaT, ObT, OcT]
        for G in range(NSB):
            tsl = slice(G * 512, (G + 1) * 512)
            for r in range(3):
                P.dma(OT[r][:], srcs[r][:, tsl].rearrange("(c p) t -> p c t", p=128), writes=[B_OT[r]], sem=B_OT[r].sem)
            P.dma(gts[:], gT[:, :, tsl].rearrange("c p t -> p c t"), writes=[B_gts], sem=B_gts.sem)
            P.dma(xo[:], xq[tsl, :].rearrange("(n p) d -> p n d", p=128), writes=[B_xo], sem=B_xo.sem)
            for oc in range(KC):
                pys = []
                for r in range(3):
                    pt, B_pt = next_ps()
                    for kc in range(4):
                        P.op("pe", R.matmul(
                            pt[:, :], lhsT=Wup[:, r * 4 + kc, oc * 128:(oc + 1) * 128], rhs=OT[r][:, kc, :],
                            start=(kc == 0), stop=(kc == 3)), reads=[B_Wup, B_OT[r]], acc=[B_pt], inc=(kc == 3))
                    pys.append((pt, B_pt))
                for r in range(3):
                    pt, B_pt = pys[r]
                    P.op("dve", R.tensor_tensor(
                        out=mt_[r][:], in0=pt[:, :], in1=gts[:, r * 8 + oc, :], op=ALU.mult),
                        reads=[B_pt, B_gts], writes=[B_mt[r]])
                P.op("pool", R.tensor_tensor(out=mt_[3][:], in0=mt_[0][:], in1=mt_[1][:], op=ALU.add),
                     reads=[B_mt[0], B_mt[1]], writes=[B_mt[3]])
                P.op("pool", R.tensor_tensor(out=mT[:, oc, :], in0=mt_[3][:], in1=mt_[2][:], op=ALU.add),
                     reads=[B_mt[3], B_mt[2]], acc=[B_mT])
            for j in range(4):
                pp = []
                for half in range(2):
                    pt, B_pt = next_ps()
                    for kc in range(KC):
                        P.op("pe", R.matmul(
                            pt[:, :], lhsT=mT[:, kc, j * 128:(j + 1) * 128], rhs=Wo[:, kc, half * 512:(half + 1) * 512],
                            start=(kc == 0), stop=(kc == KC - 1)), reads=[B_mT, B_Wo], acc=[B_pt], inc=(kc == KC - 1))
                    pp.append((pt, B_pt))
                post_norm(pp[0][0], pp[0][1], pp[1][0], pp[1][1], 0, xo[:, j, :], B_xo, x1st[:, j, :], B_x1st,
                          tmp6, B_tmp6, ss6, B_ss6, junk6, B_junk6)
            store(x1[tsl, :].rearrange("(n p) d -> p n d", p=128), x1st[:], B_x1st)
        P.barrier()
        ES.pop().close()

    if stop_after >= 7:
        ES.append(ExitStack())
        Wfi = sb("Wfi", [128, KC, 2 * DFF], BF16); B_Wfi = Buf("Wfi")
        Wfo = sb("Wfo", [128, FC, D], BF16); B_Wfo = Buf("Wfo")
        ES.append(ExitStack())
        stg = [sb("wstg7%d" % i, [128, 2048], F32) for i in range(2)]
        B_stg = [Buf("wstg7%d" % i) for i in range(2)]
        prep_weight(Wfi, B_Wfi, w_ffn_in, KC, [(0, 2 * DFF, 0, 1.0)], 2, stg, B_stg)
        prep_weight(Wfo, B_Wfo, w_ffn_out, FC, [(0, D, 0, 1.0)], None, stg, B_stg)
        P.barrier()
        ES.pop().close()
        fe = make_front("f", nbm=2)
        aT = sb("aT", [128, FC, 256], BF16); B_aT = Buf("aT")
        sg_ = [sb("sg%d" % i, [128, 256], F32) for i in range(2)]; B_sg = [Buf("sg%d" % i) for i in range(2)]
        tmp7 = sb("tmp7", [128, D], F32); B_tmp7 = Buf("tmp7")
        ss7 = sb("ss7", [128, 4], F32); B_ss7 = Buf("ss7")
        junk7 = sb("junk7", [128, 512], BF16); B_junk7 = Buf("junk7")
        ost = [sb("ost%d" % i, [128, 2, D], F32) for i in range(1)] * 2; B_ost = [Buf("ost%d" % i) for i in range(1)] * 2
        NG7 = SO // 256
        front_load(fe, 0, x1[0:256, :], nb=2)
        for g in range(NG7):
            b = g % 2
            if g + 1 < NG7:
                front_load(fe, g + 1, x1[(g + 1) * 256:(g + 2) * 256, :], nb=2)
            hT, B_hT = front(fe, g, None, nb=2, loaded=True)
            for fc in range(FC):
                pg, B_pg = proj_fm(Wfi, B_Wfi, hT, B_hT, fc * 128, 128, n=256)
                pu, B_pu = proj_fm(Wfi, B_Wfi, hT, B_hT, DFF + fc * 128, 128, n=256)
                k = fc % 2
                P.op("act", R.activation(out=sg_[k][:], in_=pg[:, 0:256], func=AF.Silu),
                     reads=[B_pg], writes=[B_sg[k]])
                P.op("dve", R.tensor_tensor(out=aT[:, fc, :], in0=sg_[k][:], in1=pu[:, 0:256],
                                                                         op=ALU.mult),
                     reads=[B_sg[k], B_pu], acc=[B_aT])
            for j in range(2):
                pp = []
                for half in range(2):
                    pt, B_pt = next_ps()
                    for fc in range(FC):
                        P.op("pe", R.matmul(
                            pt[:, :], lhsT=aT[:, fc, j * 128:(j + 1) * 128], rhs=Wfo[:, fc, half * 512:(half + 1) * 512],
                            start=(fc == 0), stop=(fc == FC - 1)), reads=[B_aT, B_Wfo], acc=[B_pt], inc=(fc == FC - 1))
                    pp.append((pt, B_pt))
                post_norm(pp[0][0], pp[0][1], pp[1][0], pp[1][1], 1, fe["xt"][b][:, j, :], fe["B_xt"][b],
                          ost[b][:, j, :], B_ost[b], tmp7, B_tmp7, ss7, B_ss7, junk7, B_junk7)
            store(out[g * 256:(g + 1) * 256, :].rearrange("(n p) d -> p n d", p=128), ost[b][:], B_ost[b])
        P.barrier()
        ES.pop().close()


    final = [(k, v) for k, v in P.cnt.items() if k not in P.ENG]
    P.run(nc, final_waits=final)
    while ES:
        ES.pop().close()
    return nc


def _rel_bucket(rel):
    nb = 16
    max_exact = 8
    n = np.abs(rel)
    large = max_exact + (np.log(np.maximum(n, 1).astype(np.float32) / max_exact)
                         / np.float32(np.log(128 / max_exact)) * (nb - max_exact)).astype(np.int32)
    large = np.minimum(large, nb - 1)
    return np.where(rel > 0, nb, 0) + np.where(n < max_exact, n, large)


def make_consts():
    c = np.zeros((128, 512), np.float32)
    c[:, 0:128] = np.eye(128)
    j = np.arange(128)[:, None]
    s = np.arange(128)[None, :]
    c[:, 128:256] = -(j >= s).astype(np.float32)
    c[:, 256:384] = 1.0
    c[:, 384:512] = -1.0
    return c.astype(ml_dtypes.bfloat16)


def core_consts(hf, rel_bias):
    p = np.arange(128)[:, None, None]
    j = np.arange(8)[None, :, None]
    t = np.arange(512)[None, None, :]
    krel = j * 128 + p
    qrel = hf * 512 + t
    maskc = (krel < qrel).astype(np.float32).astype(ml_dtypes.bfloat16)
    qp = np.arange(128)[:, None, None]
    qb = np.arange(4)[None, :, None]
    kr = np.arange(1024)[None, None, :]
    qpos = hf * 512 + qb * 128 + qp
    adm = (kr // 64) <= (qpos // 64)
    negadm = np.where(adm, 0.0, -1e30).astype(np.float32).astype(ml_dtypes.bfloat16)
    jj = np.arange(9)[None, :, None]
    rel = ((jj - 1) * 128 + p) - qrel
    bidx = _rel_bucket(rel)
    biasT = np.ascontiguousarray(np.transpose(rel_bias[bidx], (3, 0, 1, 2))).astype(np.float32)
    biasc = np.ascontiguousarray(np.broadcast_to(rel_bias[15][None, :], (128, 8))).astype(np.float32)
    return maskc, negadm, biasT, biasc


def make_in_maps(S, x, mem, rel_bias, g_mix_pre, w_in, b_gate, g_mem, w_mem_kv, w_up_a, w_up_b,
                 w_up_c, w_out, g_mix_post, g_ffn_pre, w_ffn_in, w_ffn_out, g_ffn_post):
    f = lambda a: np.ascontiguousarray(np.asarray(a, dtype=np.float32))
    x = f(x); mem = f(mem); rel_bias = f(rel_bias)
    B = x.shape[0]
    SO = S // 2
    NSB = SO // 512
    col = lambda g: f(g)[0].reshape(KC, 128).T
    gcols = np.ascontiguousarray(np.concatenate([col(g_mix_pre), col(g_mem), col(g_ffn_pre), col(g_ffn_pre)], axis=1))
    grows = np.ascontiguousarray(np.broadcast_to(np.stack([f(g_mix_post)[0], f(g_ffn_post)[0]])[None], (128, 2, D)))
    bg = np.ascontiguousarray(f(b_gate)[0].reshape(24, 128).T)
    w_up = np.ascontiguousarray(np.stack([f(w_up_a)[0], f(w_up_b)[0], f(w_up_c)[0]]))
    consts = make_consts()
    cc = [core_consts(hf, rel_bias) for hf in range(2)]
    shared = dict(w_in=f(w_in)[0], b_gate=bg, gcols=gcols, grows=grows, w_mem_kv=f(w_mem_kv)[0], w_up=w_up,
                  w_out=f(w_out)[0], w_ffn_in=f(w_ffn_in)[0], w_ffn_out=f(w_ffn_out)[0], consts=consts)
    in_maps = []
    for c in range(2 * B):
        b, hf = c // 2, c % 2
        xb = x[b]
        xq = np.ascontiguousarray(xb.reshape(NSB, 2, 512, D)[:, hf].reshape(SO, D))
        maskc, negadm, biasT, biasc = cc[hf]
        m = dict(shared)
        m.update(xf=xb, xq=xq, mem=mem[b], maskc=maskc, negadm=negadm, biasT=biasT, biasc=biasc)
        in_maps.append(m)
    return in_maps


_NC_CACHE = {}


def kernel(**inputs):
    x = np.asarray(inputs["x"])
    B, S, _ = x.shape
    SO = S // 2
    NSB = SO // 512
    if S not in _NC_CACHE:
        _NC_CACHE[S] = build(S)
    nc = _NC_CACHE[S]
    in_maps = make_in_maps(S, **inputs)
    res = run_bass_kernel_spmd(nc, in_maps, core_ids=list(range(2 * B)))
    outp = np.empty((B, S, D), np.float32)
    for c in range(2 * B):
        b, hf = c // 2, c % 2
        outp[b].reshape(NSB, 2, 512, D)[:, hf] = np.asarray(res.results[c]["out"]).reshape(NSB, 512, D)
    return outp
```
